# Optimizing a Trainium2 kernel written in Bass

```python
import jax
import jax.numpy as jnp
from jax import lax
import numpy as np


D_MODEL = 1024
BATCH = 2
SEQ = 8192
DEPTH = 1

GRID_W = 64
CTX_LEN = 256
GLA_HEADS = 4
GLA_DK = 64
GLA_DV = 128
GLA_KEY = GLA_HEADS * GLA_DK
GLA_VAL = GLA_HEADS * GLA_DV
GLA_RANK = 16
GLA_CHUNK = 64
GATE_NORMALIZER = 16.0
POOL_WINDOWS = (2, 4, 8, 16)
POOL_GROUPS = 4
POOL_CH = 128
POOL_WIDTH = POOL_GROUPS * POOL_CH
N_BRANCHES = 2
N_EXPERTS = 16
D_EXPERT = 1024
EC_CAPACITY = 2
EPS = 1e-6
CTX_COLS = (GLA_KEY, GLA_VAL, GLA_RANK, GLA_RANK)
N_CTX_COLS = GLA_KEY + GLA_VAL + 2 * GLA_RANK
ALL_COLS = (N_CTX_COLS, GLA_KEY, GLA_VAL, POOL_WIDTH, N_BRANCHES * D_MODEL)
D_IN = N_CTX_COLS + GLA_KEY + GLA_VAL + POOL_WIDTH + N_BRANCHES * D_MODEL

kernel_name = 'hybrid_gla_pool_ecmoe_diffusion_block'


def split_cols(u, sizes):
    points, acc = [], 0
    for s in sizes[:-1]:
        acc += s
        points.append(acc)
    return jnp.split(u, points, axis=-1)


def rms_norm(x, g):
    xf = x.astype(jnp.float32)
    y = xf * lax.rsqrt(jnp.mean(xf * xf, axis=-1, keepdims=True) + EPS)
    return (y * g.astype(jnp.float32)).astype(x.dtype)


def modulate(x, g, shift, scale):
    return rms_norm(x, g) * (1 + scale) + shift


def to_heads(u, d):
    b, l, _ = u.shape
    return u.reshape(b, l, GLA_HEADS, d).transpose(0, 2, 1, 3)


def chunked(u):
    b, h, l, d = u.shape
    return u.reshape(b, h, l // GLA_CHUNK, GLA_CHUNK, d)


def gla_scan(q, k, v, log_a, h0, with_output):
    kc = chunked(k.astype(jnp.float32))
    vc = chunked(v.astype(jnp.float32))
    b = jnp.cumsum(chunked(log_a.astype(jnp.float32)), axis=3)
    b_last = b[:, :, :, -1:, :]
    kv = jnp.einsum('bhncd,bhnce->bhnde', kc * jnp.exp(b_last - b), vc)
    decay = jnp.exp(b_last[:, :, :, 0, :])

    def step(state, inp):
        dec_n, kv_n = inp
        return dec_n[..., None] * state + kv_n, state

    final, starts = lax.scan(step, h0, (jnp.moveaxis(decay, 2, 0), jnp.moveaxis(kv, 2, 0)))
    if not with_output:
        return None, final
    starts = jnp.moveaxis(starts, 0, 2)
    qc = chunked(q.astype(jnp.float32))
    inter = jnp.einsum('bhncd,bhnde->bhnce', qc * jnp.exp(b), starts)
    mid = b[:, :, :, GLA_CHUNK // 2 - 1:GLA_CHUNK // 2, :]
    scores = jnp.einsum('bhnid,bhnjd->bhnij', qc * jnp.exp(b - mid), kc * jnp.exp(mid - b))
    lower = jnp.tril(jnp.ones((GLA_CHUNK, GLA_CHUNK), dtype=bool))
    intra = jnp.einsum('bhnij,bhnje->bhnie', jnp.where(lower, scores, 0.0), vc)
    bsz, h, n, cl, dv = intra.shape
    return (inter + intra).reshape(bsz, h, n * cl, dv), final


def gla_bidirectional(q, k, v, la_f, la_b, h0_f, h0_b, with_output):
    rev = lambda u: jnp.flip(u, axis=2)
    o_f, h_f = gla_scan(q, k, v, la_f, h0_f, with_output)
    q_rev = rev(q) if with_output else None
    o_b, h_b = gla_scan(q_rev, rev(k), rev(v), rev(la_b), h0_b, with_output)
    if not with_output:
        return None, h_f, h_b
    return o_f + rev(o_b), h_f, h_b


def kv_and_decay(proj_kvd, w_decay_up, b_decay):
    k, v, r_f, r_b = split_cols(proj_kvd, CTX_COLS)
    la_f = jax.nn.log_sigmoid((r_f @ w_decay_up[0] + b_decay[0]).astype(jnp.float32)) / GATE_NORMALIZER
    la_b = jax.nn.log_sigmoid((r_b @ w_decay_up[1] + b_decay[1]).astype(jnp.float32)) / GATE_NORMALIZER
    return to_heads(k, GLA_DK), to_heads(v, GLA_DV), to_heads(la_f, GLA_DK), to_heads(la_b, GLA_DK)


def context_states(ctx, g1, shift, scale, w_in, w_decay_up, b_decay):
    h = modulate(ctx, g1, shift, scale)
    k, v, la_f, la_b = kv_and_decay(h @ w_in[:, :N_CTX_COLS], w_decay_up, b_decay)
    zero = jnp.zeros((ctx.shape[0], GLA_HEADS, GLA_DK, GLA_DV), jnp.float32)
    _, h_f, h_b = gla_bidirectional(None, k, v, la_f, la_b, zero, zero, False)
    return h_f, h_b


def box_sum(u, w, axis):
    n = u.shape[axis]
    pad = [(0, 0)] * u.ndim
    pad[axis] = (1, 0)
    cs = jnp.pad(jnp.cumsum(u, axis=axis), pad)
    pos = jnp.arange(n)
    lo = jnp.clip(pos - w // 2, 0, n)
    hi = jnp.clip(pos + w // 2, 0, n)
    s = jnp.take(cs, hi, axis=axis) - jnp.take(cs, lo, axis=axis)
    return s, (hi - lo).astype(jnp.float32)


def pool_latent(u):
    bsz, l, _ = u.shape
    rows = l // GRID_W
    ug = u.astype(jnp.float32).reshape(bsz, rows, GRID_W, POOL_GROUPS, POOL_CH)
    outs = []
    for gi, w in enumerate(POOL_WINDOWS):
        ugi = ug[:, :, :, gi, :]
        s, cnt_c = box_sum(ugi, w, 2)
        s, cnt_r = box_sum(s, w, 1)
        outs.append(s / (cnt_r[:, None] * cnt_c[None, :])[None, :, :, None] - ugi)
    return jnp.stack(outs, axis=3).reshape(bsz, l, POOL_GROUPS, POOL_CH)


def pool_context(u):
    bsz, l, _ = u.shape
    ug = u.astype(jnp.float32).reshape(bsz, l, POOL_GROUPS, POOL_CH)
    outs = []
    for gi, w in enumerate(POOL_WINDOWS):
        ugi = ug[:, :, gi, :]
        s, cnt = box_sum(ugi, w, 1)
        outs.append(s / cnt[None, :, None] - ugi)
    return jnp.stack(outs, axis=2)


def mixer_sublayer(xs, g1, shift, scale, gate, h0_f, h0_b, pool_fn, w_in, w_decay_up, b_decay,
                   gla_norm_g, w_gla_proj, pool_w, pool_scale, w_pool_proj, w_out):
    bsz, l, _ = xs.shape
    h = modulate(xs, g1, shift, scale)
    proj_kvd, q, g, pool_in, merge = split_cols(h @ w_in, ALL_COLS)
    k, v, la_f, la_b = kv_and_decay(proj_kvd, w_decay_up, b_decay)
    q = to_heads(q, GLA_DK) * (GLA_DK ** -0.5)
    o, h_f, h_b = gla_bidirectional(q, k, v, la_f, la_b, h0_f, h0_b, True)
    o = rms_norm(o, gla_norm_g).transpose(0, 2, 1, 3).reshape(bsz, l, GLA_VAL).astype(xs.dtype)
    branch_gla = (o * jax.nn.silu(g)) @ w_gla_proj
    pooled = pool_fn(pool_in)
    mixed = jnp.einsum('blgc,gcd->blgd', pooled, pool_w.astype(jnp.float32)).reshape(bsz, l, POOL_WIDTH)
    branch_pool = (mixed * pool_scale.astype(jnp.float32)).astype(xs.dtype) @ w_pool_proj
    gate_gla, gate_pool = split_cols(jax.nn.sigmoid(merge), (D_MODEL, D_MODEL))
    y = (gate_gla * branch_gla + gate_pool * branch_pool) @ w_out
    return xs + gate * y, h_f, h_b


def ec_moe(h, w_router, w_gate_e, w_up_e, w_down_e):
    l = h.shape[1]
    cap = EC_CAPACITY * l // N_EXPERTS
    aff = jax.nn.softmax((h @ w_router).astype(jnp.float32), axis=-1)
    top_aff, top_idx = lax.top_k(jnp.swapaxes(aff, 1, 2), cap)

    def per_set(hs, idx, wgt):
        xs = hs[idx]
        a = jax.nn.silu(jnp.einsum('ecd,edf->ecf', xs, w_gate_e)) * jnp.einsum('ecd,edf->ecf', xs, w_up_e)
        y = jnp.einsum('ecf,efd->ecd', a, w_down_e) * wgt[..., None].astype(hs.dtype)
        return jnp.zeros_like(hs).at[idx.reshape(-1)].add(y.reshape(-1, hs.shape[-1]))

    return jax.vmap(per_set)(h, top_idx, top_aff)


def ffn_sublayer(xs, g2, shift, scale, gate, w_router, w_gate_e, w_up_e, w_down_e):
    return xs + gate * ec_moe(modulate(xs, g2, shift, scale), w_router, w_gate_e, w_up_e, w_down_e)


def setup_inputs(seed: int = 0) -> dict:
    key = jax.random.key(seed)
    ks = jax.random.split(key, 24)
    nrm = lambda k, shape, s: jax.random.normal(k, shape, jnp.float32) * s
    return {
        'x': nrm(ks[0], (BATCH, SEQ, D_MODEL), 1.0),
        'c': nrm(ks[1], (BATCH, D_MODEL), 1.0),
        'ctx': nrm(ks[2], (BATCH, CTX_LEN, D_MODEL), 1.0),
        'c_ctx': nrm(ks[3], (D_MODEL,), 1.0),
        'ada_w': nrm(ks[4], (DEPTH, D_MODEL, 6 * D_MODEL), 0.5 * D_MODEL ** -0.5),
        'ada_b': nrm(ks[5], (DEPTH, 6 * D_MODEL), 0.02),
        'norm1_g': 1.0 + nrm(ks[6], (DEPTH, D_MODEL), 0.02),
        'norm2_g': 1.0 + nrm(ks[7], (DEPTH, D_MODEL), 0.02),
        'w_in': nrm(ks[8], (DEPTH, D_MODEL, D_IN), D_MODEL ** -0.5),
        'w_decay_up': nrm(ks[9], (DEPTH, 2, GLA_RANK, GLA_KEY), GLA_RANK ** -0.5),
        'b_decay': nrm(ks[10], (DEPTH, 2, GLA_KEY), 0.1),
        'gla_norm_g': 1.0 + nrm(ks[11], (DEPTH, GLA_DV), 0.02),
        'w_gla_proj': nrm(ks[12], (DEPTH, GLA_VAL, D_MODEL), GLA_VAL ** -0.5),
        'pool_w': nrm(ks[13], (DEPTH, POOL_GROUPS, POOL_CH, POOL_CH), POOL_CH ** -0.5),
        'pool_scale': 1.0 + nrm(ks[14], (DEPTH, POOL_WIDTH), 0.1),
        'w_pool_proj': nrm(ks[15], (DEPTH, POOL_WIDTH, D_MODEL), POOL_WIDTH ** -0.5),
        'w_out': nrm(ks[16], (DEPTH, D_MODEL, D_MODEL), D_MODEL ** -0.5),
        'w_router': nrm(ks[17], (DEPTH, D_MODEL, N_EXPERTS), D_MODEL ** -0.5),
        'w_gate_e': nrm(ks[18], (DEPTH, N_EXPERTS, D_MODEL, D_EXPERT), D_MODEL ** -0.5),
        'w_up_e': nrm(ks[19], (DEPTH, N_EXPERTS, D_MODEL, D_EXPERT), D_MODEL ** -0.5),
        'w_down_e': nrm(ks[20], (DEPTH, N_EXPERTS, D_EXPERT, D_MODEL), D_EXPERT ** -0.5),
        'final_norm_g': 1.0 + nrm(ks[21], (D_MODEL,), 0.02),
    }


def reference(x, c, ctx, c_ctx, ada_w, ada_b, norm1_g, norm2_g, w_in, w_decay_up, b_decay,
              gla_norm_g, w_gla_proj, pool_w, pool_scale, w_pool_proj, w_out, w_router,
              w_gate_e, w_up_e, w_down_e, final_norm_g):
    zero_state = jnp.zeros((ctx.shape[0], GLA_HEADS, GLA_DK, GLA_DV), jnp.float32)
    for i in range(DEPTH):
        sh1, sc1, gt1, sh2, sc2, gt2 = jnp.split((jax.nn.silu(c) @ ada_w[i] + ada_b[i])[:, None, :], 6, axis=-1)
        csh1, csc1, cgt1, csh2, csc2, cgt2 = jnp.split(jax.nn.silu(c_ctx) @ ada_w[i] + ada_b[i], 6, axis=-1)
        mix_w = (w_in[i], w_decay_up[i], b_decay[i], gla_norm_g[i], w_gla_proj[i],
                 pool_w[i], pool_scale[i], w_pool_proj[i], w_out[i])
        moe_w = (w_router[i], w_gate_e[i], w_up_e[i], w_down_e[i])
        if i == DEPTH - 1:
            h_f, h_b = context_states(ctx, norm1_g[i], csh1, csc1, w_in[i], w_decay_up[i], b_decay[i])
        else:
            ctx_mixed, h_f, h_b = mixer_sublayer(ctx, norm1_g[i], csh1, csc1, cgt1, zero_state, zero_state,
                                                 pool_context, *mix_w)
            ctx = ffn_sublayer(ctx_mixed, norm2_g[i], csh2, csc2, cgt2, *moe_w)
        x, _, _ = mixer_sublayer(x, norm1_g[i], sh1, sc1, gt1, h_f, h_b, pool_latent, *mix_w)
        x = ffn_sublayer(x, norm2_g[i], sh2, sc2, gt2, *moe_w)
    return rms_norm(x, final_norm_g)
```

```python
import numpy as np
import concourse.bass as bass
import concourse.mybir as mybir
from concourse.bass_utils import run_bass_kernel_spmd

F32 = mybir.dt.float32
BF16 = mybir.dt.bfloat16
AF = mybir.ActivationFunctionType
ALU = mybir.AluOpType
AX = mybir.AxisListType

ENGS = ("pe", "act", "dve", "pool", "sp")


class Region:
    __slots__ = ("name", "last_write", "readers", "excl")

    def __init__(self, name, excl=False):
        self.name = name
        self.last_write = None
        self.readers = []
        self.excl = excl


class _Rec:
    def __getattr__(self, name):
        return lambda *a, **k: (name, a, k)


_REC = _Rec()


_BCREG = {}


def _replay(rec):
    name, a, k = rec
    if name != "indirect_dma_start":
        return lambda engine: getattr(engine, name)(*a, **k)

    def f(engine):
        key = id(engine)
        if key not in _BCREG:
            _BCREG[key] = engine.to_reg(k["bounds_check"])
        return getattr(engine, name)(*a, **dict(k, bounds_check=_BCREG[key]))
    return f


class Sched:
    def __init__(self, nc):
        self.nc = nc
        self.eng = {"pe": nc.tensor, "act": nc.scalar, "dve": nc.vector,
                    "pool": nc.gpsimd, "sp": nc.sync}
        self.items = {e: [] for e in ENGS}
        self.count = {e: 0 for e in ENGS}
        self.pending = {e: False for e in ENGS}
        self.seen = {e: {} for e in ENGS}
        self.clk = {}
        self.nsem = 0
        self.dma_count = {}
        self.streams = list(ENGS)

    def region(self, name):
        return Region(name)

    def regions(self, name, n, excl=False):
        return [Region(f"{name}{i}", excl) for i in range(n)]

    def dma_stream(self, name):
        s = f"dma:{name}:{len(self.streams)}"
        self.streams.append(s)
        self.dma_count[s] = 0
        return s

    def _need(self, e, dep):
        st, val = dep
        if self.seen[e].get(st, 0) >= val:
            return
        if st == e and val > self.count[e]:
            return
        self.items[e].append(("wait", st, val))
        ck = self.clk.get(dep)
        se = self.seen[e]
        if ck:
            for k, v in ck.items():
                if se.get(k, 0) < v:
                    se[k] = v
        if se.get(st, 0) < val:
            se[st] = val

    def _deps(self, e, reads, writes):
        for r in reads:
            if r.last_write is not None:
                self._need(e, r.last_write)
            if r.excl:
                for rd in r.readers:
                    if rd[0] != e:
                        self._need(e, rd)
        for w in writes:
            if w.last_write is not None:
                self._need(e, w.last_write)
            for rd in w.readers:
                self._need(e, rd)

    def _record(self, tok, reads, writes):
        for r in reads:
            r.readers.append(tok)
        for w in writes:
            w.last_write = tok
            w.readers = []

    def op(self, e, fn, reads=(), writes=(), inc=True):
        fn = _replay(fn(_REC))
        self._deps(e, reads, writes)
        val = self.count[e] + 1
        tok = (e, val)
        if inc:
            self.count[e] = val
            self.items[e].append(("op", fn, e, 1))
            ck = dict(self.seen[e])
            ck[e] = val
            self.clk[tok] = ck
            self.pending[e] = False
        else:
            self.items[e].append(("op", fn, None, 0))
            self.pending[e] = True
        self._record(tok, reads, writes)
        return tok

    def dma(self, q, stream, fn, reads=(), writes=()):
        fn = _replay(fn(_REC))
        self._deps(q, reads, writes)
        if self.dma_count[stream] > 0:
            self._need(q, (stream, self.dma_count[stream]))
        self.dma_count[stream] += 16
        val = self.dma_count[stream]
        tok = (stream, val)
        self.items[q].append(("op", fn, stream, 16))
        self.clk[tok] = dict(self.seen[q])
        self._record(tok, reads, writes)
        return tok

    def wait_all(self, e, toks):
        for t in toks:
            self._need(e, t)

    def emit(self):
        nc = self.nc
        for e in ENGS:
            assert not self.pending[e], f"engine {e} ends with a non-inc op"
        sems = {}
        import contextlib
        with contextlib.ExitStack() as es:
            for st in self.streams:
                nm = st.replace(":", "_")
                sems[st] = es.enter_context(nc.semaphore("s_" + nm))
            block = es.enter_context(nc.Block())

            def run(e, engine):
                for it in self.items[e]:
                    if it[0] == "wait":
                        engine.wait_ge(sems[it[1]], it[2])
                    else:
                        _, fn, st, iv = it
                        ins = fn(engine)
                        if st is not None:
                            ins.then_inc(sems[st], iv)

            @block.tensor
            def _(t):
                run("pe", t)

            @block.scalar
            def _(t):
                run("act", t)

            @block.vector
            def _(t):
                run("dve", t)

            @block.gpsimd
            def _(t):
                run("pool", t)

            @block.sync
            def _(t):
                run("sp", t)

    def barrier(self):
        toks = [(e, self.count[e]) for e in ENGS if self.count[e] > 0]
        toks += [(s, v) for s, v in self.dma_count.items() if v > 0]
        for e in ENGS:
            for t in toks:
                if t[0] != e:
                    self._need(e, t)


D = 1024
NTOK = 2048
NEXT = 3072
NT = 16
CH = 64
WINS = (2, 4, 8, 16)
NE = 16
CAP = 1024
EPS = 1e-6
U8 = mybir.dt.uint8
C_K, C_V, C_R, C_Q, C_G, C_P, C_M = 0, 256, 768, 800, 1056, 1568, 2080


def build_program():
    nc = bass.Bass("TRN2", target_bir_lowering=False)
    S = Sched(nc)

    def din(name, shape):
        return nc.dram_tensor(name, list(shape), F32, kind="ExternalInput").ap()

    xext = din("xext", [NEXT, D]); ctx = din("ctx", [256, D]); cvec = din("cvec", [128, 16])
    ada_w = din("ada_w", [D, 6 * D]); ada_bT = din("ada_bT", [128, 48]); ada_b = din("ada_b", [1, 6 * D])
    vecs = din("vecs", [128, 32]); b_decay = din("b_decay", [1, 512]); fin_g = din("fin_g", [1, D])
    w_in = din("w_in", [D, 4128]); w_upP = din("w_upP", [32, 512])
    w_glap = din("w_glap", [512, D]); pool_w = din("pool_w", [4, 128, 128]); w_poolp = din("w_poolp", [512, D])
    w_out = din("w_out", [D, D]); w_router = din("w_router", [D, 16])
    import os as _os
    NEW = 1 if _os.environ.get("KSTOP", "") else NE
    w_gate = din("w_gate", [NEW, D, D]); w_up = din("w_up", [NEW, D, D]); w_down = din("w_down", [NEW, D, D])
    consts = din("consts", [128, 9 * 128]); percore = din("percore", [128, 16])
    pmask = din("pmask", [1, NEXT]); invcnt = din("invcnt", [1, 4 * NTOK])
    sel = din("sel", [64, 16]); gmat = din("gmat", [64, 64]); consts2 = din("consts2", [128, 256])
    out = nc.dram_tensor("out", [NTOK, D], F32, kind="ExternalOutput").ap()
    xmid = nc.dram_tensor("xmid", [NTOK, D], F32).ap()
    h2rows = nc.dram_tensor("h2rows", [NTOK, D], BF16).ap()
    xsel = [nc.dram_tensor(f"xsel{i}", [CAP, D], BF16).ap() for i in range(NE)]
    ysel = [nc.dram_tensor(f"ysel{i}", [CAP, D], F32).ap() for i in range(NE)]
    U32 = mybir.dt.uint32
    ag1_in = nc.dram_tensor("ag1_in", [64, 1032], F32); ag1_out = nc.dram_tensor("ag1_out", [256, 1032], F32)
    ag2_in = nc.dram_tensor("ag2_in", [16, NTOK], F32); ag2_out = nc.dram_tensor("ag2_out", [64, NTOK], F32)
    GROUPS = [[0, 1, 2, 3], [4, 5, 6, 7]]

    def sb(name, shape, dt=F32):
        return nc.alloc_sbuf_tensor(name, list(shape), dt)

    CONS = sb("cons", [128, 9, 128])
    IDB = sb("idb", [128, 128], BF16); IDF = sb("idf", [128, 128])
    PC = sb("pc", [128, 16]); VEC = sb("vec", [128, 32]); CV = sb("cv", [128, 16]); SC = sb("scv", [128, 16])
    ABT = sb("abt", [128, 48]); MODF = sb("modf", [128, 64]); AB = sb("ab", [128, 48])
    GT1 = sb("gt1", [128, D]); GT2 = sb("gt2", [128, D]); BDEC = sb("bdec", [128, 512])
    WUP = sb("wup", [32, 512]); WRT = sb("wrt", [128, 8, 16], BF16)
    AFF = sb("aff", [128, NT, 16]); WGT = sb("wgt", [128, NT, 16])
    SEL = sb("selm", [64, 16]); GM = sb("gm", [64, 64])
    HT = sb("ht", [128, 8, NTOK], BF16)
    SMALL = sb("small", [128, 64])
    HF = sb("hf", [64, 2, 4, 128]); SINIT = sb("sinit", [64, 2, 4, 128])
    ARENA_BYTES = 146 * 1024
    ARENA = sb("arena", [128, ARENA_BYTES], U8)
    print("sbuf remaining", nc.sbuf_bytes_remaining)

    def view(off, shape, dt=F32, parts=128):
        esz = 4 if dt == F32 else 2
        n = 1
        for s_ in shape:
            n *= s_
        assert off % 32 == 0 and off + n * esz <= ARENA_BYTES, (off, n * esz)
        v = ARENA[0:parts, off:off + n * esz].bitcast(dt)
        if len(shape) == 2:
            return v.rearrange("p (a b) -> p a b", b=shape[1])
        if len(shape) == 3:
            return v.rearrange("p (a b c) -> p a b c", b=shape[1], c=shape[2])
        if len(shape) == 4:
            return v.rearrange("p (a b c d) -> p a b c d", b=shape[1], c=shape[2], d=shape[3])
        return v

    KB = 1024
    PTB = [nc.alloc_psum_tensor(f"ptb{i}", [128, 8, 128], BF16) for i in range(2)]
    PTBr = S.regions("ptb", 2, True)
    PSB = [nc.alloc_psum_tensor(f"psb{i}", [128, 512], F32) for i in range(6)]
    PSBr = S.regions("psb", 6, True)
    cnt = {"pt": 0, "ps": 0, "sm": 0}

    def ptb():
        i = cnt["pt"] % 2; cnt["pt"] += 1
        return PTB[i], PTBr[i]

    def psb():
        i = cnt["ps"] % 6; cnt["ps"] += 1
        return PSB[i], PSBr[i]

    SMr = S.regions("sm", 64)

    def small():
        i = cnt["sm"] % 64; cnt["sm"] += 1
        return SMALL[:, i:i + 1], SMr[i]

    R = {}

    def rg(name):
        if name not in R:
            R[name] = S.region(name)
        return R[name]

    dcount = [0]

    lstreams = {"sp": [S.dma_stream(f"lsp{i}") for i in range(24)], "pool": [S.dma_stream(f"lpl{i}") for i in range(12)]}
    lidx = {"sp": 0, "pool": 0}

    def load(q, dst, src, wr, rd=()):
        st = lstreams[q][lidx[q] % len(lstreams[q])]; lidx[q] += 1
        return S.dma(q, st, lambda e: e.dma_start(out=dst, in_=src), reads=list(rd), writes=list(wr))

    def op(e, fn, reads=(), writes=(), inc=True):
        return S.op(e, fn, reads=list(reads), writes=list(writes), inc=inc)

    import os
    STOP = os.environ.get("KSTOP", "")
    KSUB = int(os.environ.get("KSUB", "0"))

    def finish():
        S.barrier()
        S.emit()
        return nc

    load("sp", CONS[:], consts.rearrange("p (a b) -> p a b", b=128), [rg("cons")])
    load("sp", PC[:], percore, [rg("pc")]); load("sp", VEC[:], vecs, [rg("vec")]); load("sp", CV[:], cvec, [rg("cv")])
    load("sp", ABT[:], ada_bT, [rg("abt")]); load("sp", WUP[:], w_upP, [rg("wup")])
    load("sp", SEL[:], sel, [rg("sel")]); load("sp", GM[:], gmat, [rg("gm")])
    load("sp", BDEC[:], b_decay.partition_broadcast(128), [rg("bdec")])
    load("sp", GT1[:], ada_b[:, 2 * D:3 * D].partition_broadcast(128), [rg("gt1")])
    load("sp", GT2[:], ada_b[:, 5 * D:6 * D].partition_broadcast(128), [rg("gt2")])
    load("pool", WRT[:], w_router.rearrange("(k p) n -> p k n", p=128), [rg("wrt")])
    Uf, Ub, Lf, Lb, Mf, Mb, MKF, MKB = [CONS[:, i, :] for i in range(8)]
    BLK = CONS[:, 8, 0:2]; ONEC = CONS[:, 8, 2:3]
    op("pool", lambda e: e.memset(IDF[:], 0.0), [], [rg("idf")])
    op("pool", lambda e: e.affine_select(out=IDF[:], in_=IDF[:], pattern=[[-1, 128]], compare_op=ALU.not_equal,
                                         fill=1.0, base=0, channel_multiplier=1), [rg("idf")], [rg("idf")])
    op("dve", lambda e: e.tensor_copy(out=IDB[:], in_=IDF[:]), [rg("idf")], [rg("idb")])
    op("act", lambda e: e.activation(out=SC[:], in_=CV[:], func=AF.Silu), [rg("cv")], [rg("sc")])
    SCREP = view(0, [8, 128])
    op("dve", lambda e: e.memset(SCREP[:], 1.0), [], [rg("screp")])
    for k in range(8):
        op("dve", lambda e, k=k: e.tensor_scalar(out=SCREP[:, k, :], in0=SCREP[:, k, :], scalar1=SC[:, k:k + 1], scalar2=None, op0=ALU.mult),
           [rg("sc"), rg("screp")], [rg("screp")])
    ADW = [view(8 * KB + i * 32 * KB, [8, D]) for i in range(2)]
    ADWr = S.regions("adw", 2)
    modp, modpr = psb()
    grp_fm = [0, 1, 3, 4]
    modp_v = modp[:, 0:64].rearrange("p (a b) -> p a b", b=2)
    order = [0, 1, 3, 4, 2, 5]
    for n_, g in enumerate(order):
        slot = n_ % 2
        load("sp", ADW[slot][:], ada_w[:, g * D:(g + 1) * D].rearrange("(k p) n -> p k n", p=128), [ADWr[slot]])
        if g in grp_fm:
            gi = grp_fm.index(g)
            for cc in range(8):
                for k in range(8):
                    op("pe", lambda e, gi=gi, cc=cc, k=k, slot=slot: e.matmul(
                        modp_v[:, gi * 8 + cc, :], lhsT=ADW[slot][:, k, cc * 128:(cc + 1) * 128], rhs=SC[:, k:16:8],
                        start=(k == 0), stop=(k == 7)), [ADWr[slot], rg("sc")], [modpr], inc=(k == 7 and cc == 7))
        else:
            GT = GT1 if g == 2 else GT2
            gr = rg("gt1") if g == 2 else rg("gt2")
            for half in range(2):
                ps, psr = psb()
                for k in range(8):
                    op("pe", lambda e, k=k, slot=slot, half=half, ps=ps: e.matmul(
                        ps[:], lhsT=SCREP[:, k, :], rhs=ADW[slot][:, k, half * 512:(half + 1) * 512],
                        start=(k == 0), stop=(k == 7)), [ADWr[slot], rg("screp")], [psr], inc=(k == 7))
                op("dve", lambda e, ps=ps, GT=GT, half=half: e.tensor_tensor(
                    out=GT[:, half * 512:(half + 1) * 512], in0=ps[:], in1=GT[:, half * 512:(half + 1) * 512], op=ALU.add),
                   [psr, gr], [gr])
    MODFv = MODF[:].rearrange("p (a b) -> p a b", b=2)
    for gi, g in enumerate(grp_fm):
        for j in range(2):
            op("dve", lambda e, gi=gi, g=g, j=j: e.tensor_tensor(
                out=MODFv[:, gi * 8:(gi + 1) * 8, j], in0=modp_v[:, gi * 8:(gi + 1) * 8, j], in1=ABT[:, g * 8:(g + 1) * 8], op=ALU.add),
               [modpr, rg("abt")], [rg("modf")])
    A1, B1, ACX, BCX, A2, B2 = [AB[:, i * 8:(i + 1) * 8] for i in range(6)]

    def mkAB(A, B, gi_sh, gi_sc, j, gvec):
        op("dve", lambda e: e.scalar_tensor_tensor(out=A, in0=MODFv[:, gi_sc * 8:(gi_sc + 1) * 8, j], scalar=1.0, in1=gvec,
                                                    op0=ALU.add, op1=ALU.mult), [rg("modf"), rg("vec")], [rg("ab")])
        op("dve", lambda e: e.tensor_copy(out=B, in_=MODFv[:, gi_sh * 8:(gi_sh + 1) * 8, j]), [rg("modf")], [rg("ab")])

    mkAB(A1, B1, 0, 1, 0, VEC[:, 0:8]); mkAB(ACX, BCX, 0, 1, 1, VEC[:, 0:8]); mkAB(A2, B2, 2, 3, 0, VEC[:, 8:16])
    S.barrier()

    if STOP == "0":
        return finish()
    XT = [view(0 * KB + i * 4 * KB, [D]) for i in range(2)]
    XS = [view(8 * KB + i * 2 * KB, [D], BF16) for i in range(2)]
    JUNK = view(12 * KB, [D], BF16)
    XTr = S.regions("xt", 2); XSr = S.regions("xs", 2); JUNKr = rg("junk")
    tcount = [0]

    def norm_tile(src_dram, A, B, dst, dst_r, xt_pre=None):
        i = tcount[0] % 2; tcount[0] += 1
        xt, xs = XT[i], XS[i]
        if xt_pre is None:
            load("sp", xt, src_dram, [XTr[i]])
            xr = XTr[i]
        else:
            xt, xr = xt_pre
        ssq, ssqr = small(); sd, sdr = small(); rs, rsr = small()
        op("act", lambda e: e.activation(out=JUNK, in_=xt, func=AF.Square, accum_out=ssq), [xr], [JUNKr, ssqr])
        op("act", lambda e: e.activation(out=sd, in_=ssq, func=AF.Sqrt, scale=1.0 / D, bias=EPS), [ssqr], [sdr])
        op("dve", lambda e: e.reciprocal(out=rs, in_=sd), [sdr], [rsr])
        op("dve", lambda e: e.tensor_scalar(out=xs, in0=xt, scalar1=rs, scalar2=None, op0=ALU.mult), [xr, rsr], [XSr[i]])
        pt, ptr = ptb()
        for k in range(8):
            op("pe", lambda e, k=k: e.transpose(out=pt[:, k, :], in_=xs[:, k * 128:(k + 1) * 128], identity=IDB[:]),
               [XSr[i], rg("idb")], [ptr], inc=(k == 7))
        for k in range(8):
            if i == 0:
                op("act", lambda e, k=k: e.activation(out=dst[:, k, :], in_=pt[:, k, :], func=AF.Identity,
                                                      scale=A[:, k:k + 1], bias=B[:, k:k + 1]), [ptr, rg("ab")], [dst_r])
            else:
                op("dve", lambda e, k=k: e.tensor_scalar(out=dst[:, k, :], in0=pt[:, k, :], scalar1=A[:, k:k + 1], scalar2=B[:, k:k + 1],
                                                         op0=ALU.mult, op1=ALU.add), [ptr, rg("ab")], [dst_r])

    def wload(dst, cols, ncols, r):
        load("pool", dst, w_in[:, cols:cols + ncols].rearrange("(k p) n -> p k n", p=128), [r])

    HTr = S.regions("ht", NT)
    o_ = 14 * KB + 24 * KB + 512
    HTH = view(o_, [8, 1024], BF16); o_ += 16 * KB
    WP = view(o_, [8, 512], BF16); o_ += 8 * KB
    PD = view(o_, [48, 80]); o_ += 15 * KB
    TA = view(o_, [48, 80]); o_ += 15 * KB
    TB = view(o_, [48, 80]); o_ += 15 * KB
    INV = view(o_, [32, 64]); INVF = view(o_, [NTOK]); o_ += 8 * KB
    POOLED = view(o_, [NTOK], BF16); o_ += 4 * KB
    PW = view(o_, [4, 128], BF16); o_ += 1 * KB
    MIXT = view(130 * KB, [4, NTOK], BF16)
    assert o_ <= 130 * KB, o_
    HTHr = S.regions("hth", 8)
    wload(WP[:], C_P, 512, rg("wp"))
    load("pool", PW[:], pool_w.rearrange("g p c -> p g c"), [rg("pw")])
    go_ = 14 * KB
    WK = view(go_, [8, 256], BF16); go_ += 4 * KB
    WV = view(go_, [8, 512], BF16); go_ += 8 * KB
    WR = view(go_, [8, 32], BF16); go_ += 512
    WQ = view(go_, [8, 256], BF16); go_ += 4 * KB
    WG = view(go_, [8, 512], BF16); go_ += 8 * KB
    wload(WK[:], C_K, 256, rg("wk")); wload(WV[:], C_V, 512, rg("wv")); wload(WR[:], C_R, 32, rg("wr"))
    wload(WQ[:], C_Q, 256, rg("wq")); wload(WG[:], C_G, 512, rg("wg"))
    op("pool", lambda e: e.memset(PD[:], 0.0), [], [rg("pd")])
    for t in range(24):
        if t < 4:
            dst, dr = HTH[:, :, t * 128:(t + 1) * 128], HTHr[t]
        elif t < 20:
            dst, dr = HT[:, :, (t - 4) * 128:(t - 3) * 128], HTr[t - 4]
        else:
            dst, dr = HTH[:, :, (t - 16) * 128:(t - 15) * 128], HTHr[t - 16]
        norm_tile(xext[t * 128:(t + 1) * 128, :], A1, B1, dst, dr)

    ZT0 = view(122 * KB, [D], BF16)
    op("pool", lambda e: e.memset(ZT0, 0.0), [], [rg("zt0")])
    XSELr = S.regions("xsel", NE)
    for ex in range(NE):
        for st in range(8):
            load("sp", xsel[ex][st * 128:(st + 1) * 128, :], ZT0, [XSELr[ex]], [rg("zt0")])

    def ht_ext_block(bk):
        if bk == 0:
            return HTH[:, :, 0:512], HTHr[0:4]
        if bk == 5:
            return HTH[:, :, 512:1024], HTHr[4:8]
        return HT[:, :, (bk - 1) * 512:bk * 512], HTr[(bk - 1) * 4:bk * 4]

    for g in range(4):
        w = WINS[g]
        load("sp", INVF, invcnt[:, g * NTOK:(g + 1) * NTOK].partition_broadcast(128), [rg("inv")])
        for bk in range(6):
            ps, psr = psb()
            hsrc, hr = ht_ext_block(bk)
            for k in range(8):
                op("pe", lambda e, k=k, ps=ps, hsrc=hsrc: e.matmul(ps[:], lhsT=WP[:, k, g * 128:(g + 1) * 128], rhs=hsrc[:, k, :],
                                                              start=(k == 0), stop=(k == 7)), [rg("wp")] + hr, [psr], inc=(k == 7))
            vcol = 2 if bk == 0 else (3 if bk == 5 else 12)
            op("dve", lambda e, ps=ps, bk=bk, vcol=vcol: e.tensor_scalar(
                out=PD[:, bk * 8:(bk + 1) * 8, 8:72], in0=ps[:].rearrange("p (r c) -> p r c", c=64),
                scalar1=PC[:, vcol:vcol + 1], scalar2=None, op0=ALU.mult), [psr, rg("pc")], [rg("pd")])
        src, srr, m = PD, rg("pd"), 1
        bufs = [(TA, rg("ta")), (TB, rg("tb"))]
        bi = 0
        while m < w:
            dstb, dbr = bufs[bi]; bi ^= 1
            wd = 80 - 2 * m + 1
            op("dve", lambda e, src=src, dstb=dstb, m=m, wd=wd: e.tensor_tensor(
                out=dstb[:, :, 0:wd], in0=src[:, :, 0:wd], in1=src[:, :, m:m + wd], op=ALU.add), [srr], [dbr])
            src, srr, m = dstb, dbr, 2 * m
        coff = 8 - w // 2
        m = 1
        first = True
        while m < w:
            dstb, dbr = bufs[bi]; bi ^= 1
            nr = 48 - 2 * m + 1
            c0 = coff if first else 0
            op("dve", lambda e, src=src, dstb=dstb, m=m, nr=nr, c0=c0: e.tensor_tensor(
                out=dstb[:, 0:nr, 0:64], in0=src[:, 0:nr, c0:c0 + 64], in1=src[:, m:m + nr, c0:c0 + 64], op=ALU.add), [srr], [dbr])
            src, srr, m, first = dstb, dbr, 2 * m, False
        roff = 8 - w // 2
        dstb, dbr = bufs[bi]; bi ^= 1
        op("dve", lambda e, src=src, dstb=dstb: e.tensor_tensor(
            out=dstb[:, 0:32, 0:64], in0=src[:, roff:roff + 32, 0:64], in1=INV[:, :, :], op=ALU.mult), [srr, rg("inv")], [dbr])
        op("dve", lambda e, dstb=dstb: e.tensor_tensor(
            out=POOLED.rearrange("p (r c) -> p r c", c=64), in0=dstb[:, 0:32, 0:64], in1=PD[:, 8:40, 8:72], op=ALU.subtract),
           [dbr, rg("pd")], [rg("pooled")])
        for bk in range(4):
            ps, psr = psb()
            op("pe", lambda e, ps=ps, bk=bk: e.matmul(ps[:], lhsT=PW[:, g, :], rhs=POOLED[:, bk * 512:(bk + 1) * 512], start=True, stop=True),
               [rg("pw"), rg("pooled")], [psr])
            op("act", lambda e, ps=ps, bk=bk: e.activation(out=MIXT[:, g, bk * 512:(bk + 1) * 512], in_=ps[:], func=AF.Copy,
                                                           scale=VEC[:, 16 + g:17 + g]), [psr, rg("vec")], [rg("mixt")])
    S.barrier()

    if STOP == "P":
        return finish()
    o_ = 14 * KB
    o_ = go_
    KVB = view(o_, [32, 4, 128], BF16); o_ += 32 * KB
    KVC = view(o_, [4, 4, 128], BF16); o_ += 4 * KB
    DEC = view(o_, [4, 2, 32]); o_ += 1 * KB
    DECC = view(o_, [4, 2, 4]); o_ += 128
    RTS = view(o_, [128]); o_ += 512
    Z = view(o_, [512]); o_ += 2 * KB
    LL = view(o_, [512]); o_ += 2 * KB
    EKS = view(o_, [512]); o_ += 2 * KB
    KD = view(o_, [2, 256], BF16); o_ += 1 * KB
    VT = view(o_, [512], BF16); o_ += 1 * KB
    EBS2 = [view(o_ + i * KB, [256]) for i in range(2)]; o_ += 2 * KB
    EMS2 = [view(o_ + i * KB, [256]) for i in range(2)]; o_ += 2 * KB
    EMN2 = [view(o_ + i * KB, [256]) for i in range(2)]; o_ += 2 * KB
    QDH2 = [view(o_ + i * 512, [2, 128], BF16) for i in range(2)]; o_ += 1 * KB
    QM2 = [view(o_ + i * 512, [2, 128], BF16) for i in range(2)]; o_ += 1 * KB
    KM2 = [view(o_ + i * 512, [2, 128], BF16) for i in range(2)]; o_ += 1 * KB
    T1 = view(o_, [128]); o_ += 512
    T2 = view(o_, [128]); o_ += 512
    PTH = view(o_, [128], BF16); o_ += 256
    SF = view(o_, [4, 128]); o_ += 2 * KB
    SB2 = [view(o_ + i * 2 * KB, [4, 128]) for i in range(2)]; o_ += 4 * KB
    SFB = view(o_, [4, 128], BF16); o_ += 1 * KB
    TMPB = view(o_, [4, 128], BF16); o_ += 1 * KB
    ON = view(o_, [512], BF16); o_ += 1 * KB
    SG = view(o_, [4, 128], BF16); o_ += 1 * KB
    AGS = view(116224, [4, 1032])
    AGI_OFF = o_
    AGI = view(o_, [1032]); o_ += 4128 + 32 - 4128 % 32
    DM = view(o_, [8]); o_ += 32
    LTS = view(o_, [8]); o_ += 32
    OSUM = view(o_, [512]); o_ += 2 * KB
    KVM = view(o_, [4, 128]); o_ += 2 * KB
    OG = view(114 * KB, [4, NTOK], BF16)
    assert o_ <= 114 * KB, o_
    R['ags'] = rg('og')
    gts = ["rts", "z", "ll", "eks", "kd", "vt", "ebs", "ems", "emn", "qdh", "qm", "km", "t1", "t2", "pth", "sf", "sbk", "sfb", "tmpb",
           "on", "sg", "ags", "agi", "dm", "kvm", "dec", "decc", "kvb", "kvc", "og", "ltot"]
    psb5 = [0]
    pspool = [[0, 1, 2, 3, 4, 5]]

    def psb():
        pl = pspool[0]
        i = pl[psb5[0] % len(pl)]; psb5[0] += 1
        return PSB[i], PSBr[i]

    def gla_common(hsrc, hr, own_first, own_last, ltot_col=True):
        pv, pvr = psb()
        for k in range(8):
            op("pe", lambda e, k=k: e.matmul(pv[:], lhsT=hsrc[:, k, :], rhs=WV[:, k, :], start=(k == 0), stop=(k == 7)), hr + [rg("wv")], [pvr], inc=(k == 7))
        op("act", lambda e: e.copy(out=VT, in_=pv[:]), [pvr], [rg("vt")])
        pk, pkr = psb()
        for k in range(8):
            op("pe", lambda e, k=k: e.matmul(pk[:, 0:256], lhsT=hsrc[:, k, :], rhs=WK[:, k, :], start=(k == 0), stop=(k == 7)), hr + [rg("wk")], [pkr], inc=(k == 7))
        pr, prr = psb()
        for k in range(8):
            op("pe", lambda e, k=k: e.matmul(pr[0:32, 0:128], lhsT=WR[:, k, :], rhs=hsrc[:, k, :], start=(k == 0), stop=(k == 7)), hr + [rg("wr")], [prr], inc=(k == 7))
        op("act", lambda e: e.copy(out=RTS[0:32, :], in_=pr[0:32, 0:128]), [prr], [rg("rts")])
        op("pe", lambda e: e.matmul(pr[:, :], lhsT=RTS[0:32, :], rhs=WUP[:], start=True, stop=True), [rg("rts"), rg("wup")], [prr])
        op("dve", lambda e: e.tensor_tensor(out=Z, in0=pr[:], in1=BDEC[:], op=ALU.add), [prr, rg("bdec")], [rg("z")])
        op("act", lambda e: e.activation(out=Z, in_=Z, func=AF.Exp, scale=-1.0), [rg("z")], [rg("z")])
        op("act", lambda e: e.activation(out=LL, in_=Z, func=AF.Ln, bias=1.0), [rg("z")], [rg("ll")])
        pe_, per = psb()
        op("pe", lambda e: e.matmul(pe_[:, 0:256], lhsT=Lf, rhs=LL[:, 0:256], start=True, stop=True), [rg("cons"), rg("ll")], [per], inc=False)
        op("pe", lambda e: e.matmul(pe_[:, 256:512], lhsT=Lb, rhs=LL[:, 256:512], start=True, stop=True), [rg("cons"), rg("ll")], [per])
        op("act", lambda e: e.activation(out=EKS, in_=pe_[:], func=AF.Exp), [per], [rg("eks")])
        for d in range(2):
            op("dve", lambda e, d=d: e.tensor_tensor(out=KD[:, d, :], in0=pk[:, 0:256], in1=EKS[:, d * 256:(d + 1) * 256], op=ALU.mult),
               [pkr, rg("eks")], [rg("kd")])

    def kv_mm(h):
        res = []
        for c in range(2):
            pkv, pkvr = psb()
            for d in range(2):
                op("pe", lambda e, c=c, d=d: e.matmul(pkv[0:64, d * 128:(d + 1) * 128],
                                                      lhsT=KD[c * 64:(c + 1) * 64, d, h * 64:(h + 1) * 64],
                                                      rhs=VT[c * 64:(c + 1) * 64, h * 128:(h + 1) * 128], start=True, stop=True),
                   [rg("kd"), rg("vt")], [pkvr], inc=(d == 1))
            res.append((pkv, pkvr))
        return res

    def g1_tile(j, hsrc, hr, DECs, decr, KVs, kvr, ntl, seg_tot):
        gla_common(hsrc, hr, j == 0, j == ntl - 1)
        for h in range(4):
            pd_, pdr = psb()
            for d in range(2):
                op("pe", lambda e, d=d: e.matmul(pd_[0:64, d * 2:(d + 1) * 2], lhsT=LL[:, d * 256 + h * 64:d * 256 + (h + 1) * 64], rhs=BLK,
                                                 start=True, stop=True), [rg("ll"), rg("cons")], [pdr], inc=(d == 1))
            op("act", lambda e, h=h: e.activation(out=DECs[0:64, h, :, 2 * j:2 * j + 2], in_=pd_[0:64, 0:4].rearrange("p (d c) -> p d c", c=2),
                                                  func=AF.Exp), [pdr], [decr])
            if KSUB == 1:
                continue
            if seg_tot:
                pt_, ptr_ = psb()
                for d in range(2):
                    op("pe", lambda e, d=d: e.matmul(pt_[0:64, d:d + 1], lhsT=LL[:, d * 256 + h * 64:d * 256 + (h + 1) * 64], rhs=ONEC,
                                                     start=True, stop=True), [rg("ll"), rg("cons")], [ptr_], inc=(d == 1))
                op("dve", lambda e: e.tensor_tensor(out=LTS[0:64, h * 2:h * 2 + 2], in0=LTS[0:64, h * 2:h * 2 + 2], in1=pt_[0:64, 0:2], op=ALU.add),
                   [ptr_, rg("lts")], [rg("lts")])
            kvs = kv_mm(h)
            if KSUB == 2:
                continue
            for c in range(2):
                pkv, pkvr = kvs[c]
                if KSUB == 3 and c == 1:
                    continue
                op("dve", lambda e, c=c, h=h: e.scalar_tensor_tensor(out=SF[0:64, h, :], in0=SF[0:64, h, :], scalar=DECs[0:64, h, 0, 2 * j + c:2 * j + c + 1],
                                                                     in1=pkv[0:64, 0:128], op0=ALU.mult, op1=ALU.add),
                   [rg("sf"), decr, pkvr], [rg("sf")])
                op("act", lambda e, h=h, c=c: e.copy(out=KVs[0:64, 2 * j + c, h, :], in_=pkv[0:64, 128:256]), [pkvr], [kvr])

    SBr = S.regions("sbk", 2)

    def bwd_scan(nch, DECs, decr, KVs, kvr, store, cur=0):
        for n in range(nch - 1, -1, -1):
            nxt = cur ^ 1
            for h in range(4):
                op("dve", lambda e, h=h, n=n, cur=cur, nxt=nxt: e.scalar_tensor_tensor(out=SB2[nxt][0:64, h, :], in0=SB2[cur][0:64, h, :], scalar=DECs[0:64, h, 1, n:n + 1],
                                                                                       in1=KVs[0:64, n, h, :], op0=ALU.mult, op1=ALU.add),
                   [SBr[cur], decr, kvr], [SBr[nxt]])
            if store:
                op("act", lambda e, n=n, cur=cur: e.copy(out=KVs[0:64, n, :, :], in_=SB2[cur][0:64]), [SBr[cur]], [kvr])
            cur = nxt
        return cur

    CTXH = view(0, [8, 256], BF16)
    CTXH = AGS[:, :, :].bitcast(BF16) if False else view(AGI_OFF, [8, 256], BF16)
    CTXHr = S.regions("ctxh", 2)
    op("dve", lambda e: e.memset(SF[0:64], 0.0), [], [rg("sf")])
    op("dve", lambda e: e.memset(SB2[0][0:64], 0.0), [], [SBr[0]])
    if STOP == "W":
        return finish()
    for t in range(2):
        norm_tile(ctx[t * 128:(t + 1) * 128, :], ACX, BCX, CTXH[:, :, t * 128:(t + 1) * 128], CTXHr[t])
        if STOP == "C1":
            gla_common(CTXH[:, :, t * 128:(t + 1) * 128], [CTXHr[t]], True, True)
            return finish()
        g1_tile(t, CTXH[:, :, t * 128:(t + 1) * 128], [CTXHr[t]], DECC, rg("decc"), KVC, rg("kvc"), 2, False)
        if STOP == "C2":
            return finish()
    cb = bwd_scan(4, DECC, rg("decc"), KVC, rg("kvc"), False, 0)
    op("act", lambda e: e.copy(out=HF[:, 0], in_=SF[0:64]), [rg("sf")], [rg("hf")])
    op("act", lambda e: e.copy(out=HF[:, 1], in_=SB2[cb][0:64]), [SBr[cb]], [rg("hf")])
    if STOP == "C":
        return finish()
    op("dve", lambda e: e.memset(LTS[0:64], 0.0), [], [rg("lts")])
    op("dve", lambda e: e.memset(SF[0:64], 0.0), [rg("sf")], [rg("sf")])
    op("dve", lambda e: e.memset(SB2[0][0:64], 0.0), [SBr[0]], [SBr[0]])
    for j in range(NT):
        g1_tile(j, HT[:, :, j * 128:(j + 1) * 128], [HTr[j]], DEC, rg("dec"), KVB, rg("kvb"), NT, True)
    cb = bwd_scan(32, DEC, rg("dec"), KVB, rg("kvb"), False, 0)
    if STOP == "G1":
        return finish()
    op("act", lambda e: e.copy(out=AGI[0:64, 0:512], in_=SF[0:64].rearrange("p h v -> p (h v)")), [rg("sf")], [rg("agi")])
    op("act", lambda e: e.copy(out=AGI[0:64, 512:1024], in_=SB2[cb][0:64].rearrange("p h v -> p (h v)")), [SBr[cb]], [rg("agi")])
    op("act", lambda e: e.activation(out=AGI[0:64, 1024:1032].rearrange("p (d h) -> p d h", d=2),
                                     in_=LTS[0:64, 0:8].rearrange("p (h d) -> p d h", d=2), func=AF.Exp), [rg("lts")], [rg("agi")])
    load("pool", ag1_in.ap(), AGI[0:64, :], [rg("ag1in")], [rg("agi")])
    ccs = S.dma_stream("cc1")
    S.dma_count[ccs] = 0

    def cc_dma(stream, fn, reads, writes):
        fn = _replay(fn(_REC))
        S._deps("pool", reads, writes)
        S.dma_count[stream] += 1
        tok = (stream, S.dma_count[stream])
        S.items["pool"].append(("op", fn, stream, 1))
        S.clk[tok] = dict(S.seen["pool"])
        S._record(tok, reads, writes)

    cc_dma(ccs, lambda e: e.collective_compute("AllGather", ALU.bypass, replica_groups=GROUPS,
                                               ins=[ag1_in.ap().opt()], outs=[ag1_out.ap().opt()]), [rg("ag1in")], [rg("ag1out")])
    load("pool", AGS[0:64], ag1_out.ap().rearrange("(r p) f -> p r f", p=64), [rg("ags")], [rg("ag1out")])
    op("dve", lambda e: e.tensor_copy(out=SINIT[:], in_=HF[:]), [rg("hf")], [rg("sinit")])
    for d in range(2):
        for r in (range(4) if d == 0 else range(3, -1, -1)):
            mcol = PC[0:64, 4 + d * 4 + r:5 + d * 4 + r]
            op("dve", lambda e, r=r, d=d, mcol=mcol: e.tensor_scalar(out=DM[0:64, 0:4], in0=AGS[0:64, r, 1024 + d * 4:1028 + d * 4], scalar1=-1.0, scalar2=mcol,
                                                                     op0=ALU.add, op1=ALU.mult), [rg("ags"), rg("pc")], [rg("dm")])
            op("dve", lambda e: e.tensor_scalar(out=DM[0:64, 4:8], in0=DM[0:64, 0:4], scalar1=1.0, scalar2=None, op0=ALU.add), [rg("dm")], [rg("dm")])
            op("dve", lambda e, r=r, d=d, mcol=mcol: e.tensor_scalar(out=KVM[0:64].rearrange("p h v -> p (h v)"), in0=AGS[0:64, r, d * 512:(d + 1) * 512],
                                                                     scalar1=mcol, scalar2=None, op0=ALU.mult), [rg("ags"), rg("pc")], [rg("kvm")])
            for h in range(4):
                op("dve", lambda e, h=h, d=d: e.scalar_tensor_tensor(out=SINIT[:, d, h, :], in0=SINIT[:, d, h, :], scalar=DM[0:64, 4 + h:5 + h],
                                                                     in1=KVM[0:64, h, :], op0=ALU.mult, op1=ALU.add), [rg("sinit"), rg("dm"), rg("kvm")], [rg("sinit")])
    op("dve", lambda e: e.tensor_copy(out=SB2[0][0:64], in_=SINIT[:, 1]), [rg("sinit"), SBr[0]], [SBr[0]])
    bwd_scan(32, DEC, rg("dec"), KVB, rg("kvb"), True, 0)
    op("dve", lambda e: e.tensor_copy(out=SF[0:64], in_=SINIT[:, 0]), [rg("sinit"), rg("sf")], [rg("sf")])

    if STOP == "X":
        return finish()
    def g2_tile(j):
        hsrc, hr = HT[:, :, j * 128:(j + 1) * 128], [HTr[j]]
        gla_common(hsrc, hr, False, False)
        pO = [(PSB[3], PSBr[3]), (PSB[4], PSBr[4])]
        pI1, pI1r = PSB[5], PSBr[5]
        def H1(h):
            b_ = h % 2
            EBS, EMS, EMN, QDH, QM, KM = EBS2[b_], EMS2[b_], EMN2[b_], QDH2[b_], QM2[b_], KM2[b_]
            ebr, emr, enr, qdr, qmr, kmr = (rg(f"ebs{b_}"), rg(f"ems{b_}"), rg(f"emn{b_}"), rg(f"qdh{b_}"), rg(f"qm{b_}"), rg(f"km{b_}"))
            pf, pfr = psb()
            for qi, W_ in enumerate((WK, WQ)):
                for k in range(8):
                    op("pe", lambda e, k=k, qi=qi, W_=W_: e.matmul(pf[0:64, qi * 128:(qi + 1) * 128], lhsT=W_[:, k, h * 64:(h + 1) * 64], rhs=hsrc[:, k, :],
                                                                  start=(k == 0), stop=(k == 7)), hr + [rg("wk"), rg("wq")], [pfr], inc=(k == 7 and qi == 1))
            pb, pbr = psb()
            for ci, (d, Cm) in enumerate(((0, Uf), (1, Ub), (0, Mf), (1, Mb))):
                op("pe", lambda e, ci=ci, d=d, Cm=Cm: e.matmul(pb[0:64, ci * 128:(ci + 1) * 128], lhsT=LL[:, d * 256 + h * 64:d * 256 + (h + 1) * 64], rhs=Cm,
                                                               start=True, stop=True), [rg("ll"), rg("cons")], [pbr], inc=(ci == 3))
            op("act", lambda e: e.activation(out=EBS[0:64], in_=pb[0:64, 0:256], func=AF.Exp), [pbr], [ebr])
            op("act", lambda e: e.activation(out=EMS[0:64], in_=pb[0:64, 256:512], func=AF.Exp), [pbr], [emr])
            op("act", lambda e: e.activation(out=EMN[0:64], in_=pb[0:64, 256:512], func=AF.Exp, scale=-1.0), [pbr], [enr])
            for d in range(2):
                op("dve", lambda e, d=d: e.scalar_tensor_tensor(out=QDH[0:64, d, :], in0=pf[0:64, 128:256], scalar=0.125, in1=EBS[0:64, d * 128:(d + 1) * 128],
                                                                op0=ALU.mult, op1=ALU.mult), [pfr, ebr], [qdr])
                op("dve", lambda e, d=d: e.scalar_tensor_tensor(out=QM[0:64, d, :], in0=pf[0:64, 128:256], scalar=0.125, in1=EMS[0:64, d * 128:(d + 1) * 128],
                                                                op0=ALU.mult, op1=ALU.mult), [pfr, emr], [qmr])
                op("dve", lambda e, d=d: e.tensor_tensor(out=KM[0:64, d, :], in0=pf[0:64, 0:128], in1=EMN[0:64, d * 128:(d + 1) * 128], op=ALU.mult),
                   [pfr, enr], [kmr])

        def H2(h):
            b_ = h % 2
            EBS, QDH, QM, KM = EBS2[b_], QDH2[b_], QM2[b_], KM2[b_]
            ebr, qdr, qmr, kmr = rg(f"ebs{b_}"), rg(f"qdh{b_}"), rg(f"qm{b_}"), rg(f"km{b_}")
            psc, pscr = psb()
            for d in range(2):
                op("pe", lambda e, d=d: e.matmul(psc[:, d * 128:(d + 1) * 128], lhsT=KM[0:64, d, :], rhs=QM[0:64, d, :], start=True, stop=True),
                   [kmr, qmr], [pscr], inc=(d == 1))
            op("dve", lambda e: e.tensor_tensor(out=T1, in0=psc[:, 0:128], in1=MKF, op=ALU.mult), [pscr, rg("cons")], [rg("t1")])
            op("dve", lambda e: e.tensor_tensor(out=T2, in0=psc[:, 128:256], in1=MKB, op=ALU.mult), [pscr, rg("cons")], [rg("t2")])
            op("dve", lambda e: e.tensor_tensor(out=PTH, in0=T1, in1=T2, op=ALU.add), [rg("t1"), rg("t2")], [rg("pth")])
            kvs = kv_mm(h)
            for c in range(2):
                po, por = pO[c]
                pkv, pkvr = kvs[c]
                op("act", lambda e, h=h: e.copy(out=SFB[0:64, h, :], in_=SF[0:64, h, :]), [rg("sf")], [rg("sfb")])
                op("pe", lambda e, c=c, h=h, po=po: e.matmul(po[0:64, h * 128:(h + 1) * 128], lhsT=QDH[0:64, 0, c * 64:(c + 1) * 64], rhs=SFB[0:64, h, :],
                                                             start=True, stop=False), [qdr, rg("sfb")], [por], inc=False)
                op("pe", lambda e, c=c, h=h, po=po: e.matmul(po[0:64, h * 128:(h + 1) * 128], lhsT=QDH[0:64, 1, c * 64:(c + 1) * 64], rhs=KVB[0:64, 2 * j + c, h, :],
                                                             start=False, stop=(c == 1)), [qdr, rg("kvb")], [por], inc=(c == 1))
                pin, pinr = (po, por) if c == 0 else (pI1, pI1r)
                op("pe", lambda e, c=c, h=h, pin=pin: e.matmul(pin[0:64, h * 128:(h + 1) * 128], lhsT=PTH[c * 64:(c + 1) * 64, c * 64:(c + 1) * 64],
                                                               rhs=VT[c * 64:(c + 1) * 64, h * 128:(h + 1) * 128], start=(c == 1), stop=True),
                   [rg("pth"), rg("vt")], [pinr], inc=True)
                op("dve", lambda e, c=c, h=h: e.scalar_tensor_tensor(out=SF[0:64, h, :], in0=SF[0:64, h, :], scalar=EBS[0:64, c * 64 + 63:c * 64 + 64],
                                                                     in1=pkv[0:64, 0:128], op0=ALU.mult, op1=ALU.add),
                   [rg("sf"), ebr, pkvr], [rg("sf")])

        H1(0)
        for h in range(4):
            if h < 3:
                H1(h + 1)
            H2(h)
        ptp, ptpr = PTB[0], PTBr[0]
        for c in range(2):
            po, por = pO[c]
            if c == 0:
                op("act", lambda e, po=po: e.copy(out=OSUM[0:64, :], in_=po[0:64, :]), [por], [rg("osum")])
            else:
                op("act", lambda e: e.copy(out=OSUM[0:64, :], in_=pI1[0:64, :]), [pI1r], [rg("osum")])
                op("dve", lambda e, po=po: e.tensor_tensor(out=OSUM[0:64, :], in0=po[0:64, :], in1=OSUM[0:64, :], op=ALU.add), [por, rg("osum")], [rg("osum")])
            po, por = OSUM, rg("osum")
            for h in range(4):
                op("act", lambda e, h=h, po=po: e.activation(out=T1[0:64, :], in_=po[0:64, h * 128:(h + 1) * 128], func=AF.Square,
                                                             accum_out=DM[0:64, h:h + 1]), [por], [rg("t1"), rg("dm")])
            op("act", lambda e: e.activation(out=DM[0:64, 4:8], in_=DM[0:64, 0:4], func=AF.Sqrt, scale=1.0 / 128, bias=EPS), [rg("dm")], [rg("dm")])
            op("dve", lambda e: e.reciprocal(out=DM[0:64, 0:4], in_=DM[0:64, 4:8]), [rg("dm")], [rg("dm")])
            for h in range(4):
                op("dve", lambda e, h=h, po=po: e.tensor_scalar(out=ON[0:64, h * 128:(h + 1) * 128], in0=po[0:64, h * 128:(h + 1) * 128],
                                                                scalar1=DM[0:64, h:h + 1], scalar2=None, op0=ALU.mult), [por, rg("dm")], [rg("on")])
            for h in range(4):
                op("pe", lambda e, h=h, c=c: e.transpose(out=ptp[:, h, c * 64:(c + 1) * 64], in_=ON[0:64, h * 128:(h + 1) * 128], identity=IDB[0:64, 0:64]),
                   [rg("on"), rg("idb")], [ptpr], inc=(h == 3))
        pg, pgr = psb()
        for h in range(4):
            for k in range(8):
                op("pe", lambda e, h=h, k=k: e.matmul(pg[:, h * 128:(h + 1) * 128], lhsT=WG[:, k, h * 128:(h + 1) * 128], rhs=hsrc[:, k, :],
                                                      start=(k == 0), stop=(k == 7)), hr + [rg("wg")], [pgr], inc=(k == 7 and h == 3))
        op("act", lambda e: e.activation(out=SG[:].rearrange("p h t -> p (h t)"), in_=pg[:], func=AF.Silu), [pgr], [rg("sg")])
        op("dve", lambda e: e.scalar_tensor_tensor(out=OG[:, :, j * 128:(j + 1) * 128], in0=ptp[:, 0:4, :], scalar=VEC[:, 20:21], in1=SG[:],
                                                   op0=ALU.mult, op1=ALU.mult), [ptpr, rg("vec"), rg("sg")], [rg("og")])

    PSB.append(PTB[1][:].rearrange("p a b -> p (a b)").bitcast(F32)); PSBr.append(PTBr[1])
    pspool[0] = [0, 1, 2, 6]
    for j in range(NT):
        g2_tile(j)
    pspool[0] = [0, 1, 2, 3, 4, 5]
    S.barrier()

    if STOP == "G2":
        return finish()
    o_ = 14 * KB
    WGP = view(o_, [4, D], BF16); o_ += 8 * KB
    WPP = view(o_, [4, D], BF16); o_ += 8 * KB
    WO = view(o_, [8, D], BF16); o_ += 16 * KB
    WM = view(o_, [8, 2048], BF16); o_ += 32 * KB
    ZT = view(o_, [8, 512], BF16); o_ += 8 * KB
    SGA = view(o_, [512], BF16); o_ += 1 * KB
    SGB = view(o_, [512], BF16); o_ += 1 * KB
    TM1 = view(o_, [512]); o_ += 2 * KB
    TM2 = view(o_, [512]); o_ += 2 * KB
    XMs = [view(o_ + i * 4 * KB, [D]) for i in range(2)]; o_ += 8 * KB
    XRs = XT; XRr = XTr
    H2TK = [view(o_ + i * 2 * KB, [D], BF16) for i in range(2)]; o_ += 4 * KB
    H2TKr = S.regions("h2tk", 2)
    XMr = S.regions("xm", 2)
    EXPT = view(o_, [16]); o_ += 64
    AFTS = view(o_, [NTOK]); o_ += 8 * KB
    assert o_ <= 114 * KB, o_
    load("pool", WGP[:], w_glap.rearrange("(k p) n -> p k n", p=128), [rg("wgp")])
    load("pool", WPP[:], w_poolp.rearrange("(k p) n -> p k n", p=128), [rg("wpp")])
    load("pool", WO[:], w_out.rearrange("(k p) n -> p k n", p=128), [rg("wo")])
    wload(WM[:], C_M, 2048, rg("wm"))
    for bk in range(4):
        hsrc, hr = HT[:, :, bk * 512:(bk + 1) * 512], HTr[bk * 4:(bk + 1) * 4]
        for dc in range(8):
            pbg, pbgr = psb(); pbp, pbpr = psb(); pmg, pmgr = psb(); pmp, pmpr = psb()
            for h in range(4):
                op("pe", lambda e, h=h, dc=dc: e.matmul(pbg[:], lhsT=WGP[:, h, dc * 128:(dc + 1) * 128], rhs=OG[:, h, bk * 512:(bk + 1) * 512],
                                                        start=(h == 0), stop=(h == 3)), [rg("wgp"), rg("og")], [pbgr], inc=(h == 3))
            for g in range(4):
                op("pe", lambda e, g=g, dc=dc: e.matmul(pbp[:], lhsT=WPP[:, g, dc * 128:(dc + 1) * 128], rhs=MIXT[:, g, bk * 512:(bk + 1) * 512],
                                                        start=(g == 0), stop=(g == 3)), [rg("wpp"), rg("mixt")], [pbpr], inc=(g == 3))
            for k in range(8):
                op("pe", lambda e, k=k, dc=dc: e.matmul(pmg[:], lhsT=WM[:, k, dc * 128:(dc + 1) * 128], rhs=hsrc[:, k, :], start=(k == 0), stop=(k == 7)),
                   hr + [rg("wm")], [pmgr], inc=(k == 7))
            for k in range(8):
                op("pe", lambda e, k=k, dc=dc: e.matmul(pmp[:], lhsT=WM[:, k, 1024 + dc * 128:1024 + (dc + 1) * 128], rhs=hsrc[:, k, :], start=(k == 0), stop=(k == 7)),
                   hr + [rg("wm")], [pmpr], inc=(k == 7))
            op("act", lambda e: e.activation(out=SGA, in_=pmg[:], func=AF.Sigmoid), [pmgr], [rg("sga")])
            op("act", lambda e: e.activation(out=SGB, in_=pmp[:], func=AF.Sigmoid), [pmpr], [rg("sgb")])
            op("dve", lambda e: e.tensor_tensor(out=TM1, in0=pbg[:], in1=SGA, op=ALU.mult), [pbgr, rg("sga")], [rg("tm1")])
            op("dve", lambda e: e.tensor_tensor(out=TM2, in0=pbp[:], in1=SGB, op=ALU.mult), [pbpr, rg("sgb")], [rg("tm2")])
            op("dve", lambda e, dc=dc: e.tensor_tensor(out=ZT[:, dc, :], in0=TM1, in1=TM2, op=ALU.add), [rg("tm1"), rg("tm2")], [rg("zt")])
        def y_stage(tt):
            j = bk * 4 + tt
            b_ = j % 2
            load("sp", XRs[b_], xext[512 + j * 128:512 + (j + 1) * 128, :], [XRr[b_]])
            for half in range(2):
                py, pyr = psb()
                for dc in range(8):
                    op("pe", lambda e, dc=dc, half=half, tt=tt: e.matmul(py[:], lhsT=ZT[:, dc, tt * 128:(tt + 1) * 128], rhs=WO[:, dc, half * 512:(half + 1) * 512],
                                                                        start=(dc == 0), stop=(dc == 7)), [rg("zt"), rg("wo")], [pyr], inc=(dc == 7))
                op("dve", lambda e, half=half: e.tensor_tensor(out=XMs[b_][:, half * 512:(half + 1) * 512], in0=py[:], in1=GT1[:, half * 512:(half + 1) * 512], op=ALU.mult),
                   [pyr, rg("gt1")], [XMr[b_]])
            op("dve", lambda e: e.tensor_tensor(out=XMs[b_], in0=XMs[b_], in1=XRs[b_], op=ALU.add), [XMr[b_], XRr[b_]], [XMr[b_]])
            load("sp", xmid[j * 128:(j + 1) * 128, :], XMs[b_], [rg("xmid")], [XMr[b_]])

        def n_stage(tt):
            j = bk * 4 + tt
            b_ = j % 2
            norm_tile(None, A2, B2, HT[:, :, j * 128:(j + 1) * 128], HTr[j], xt_pre=(XMs[b_], XMr[b_]))
            pt2, pt2r = ptb()
            for k in range(8):
                op("pe", lambda e, k=k, j=j: e.transpose(out=pt2[:, k, :], in_=HT[:, k, j * 128:(j + 1) * 128], identity=IDB[:]),
                   [HTr[j], rg("idb")], [pt2r], inc=(k == 7))
            op("act", lambda e: e.copy(out=H2TK[b_].rearrange("p (k d) -> p k d", k=8), in_=pt2[:]), [pt2r], [H2TKr[b_]])
            load("sp", h2rows[j * 128:(j + 1) * 128, :], H2TK[b_], [rg("h2rows")], [H2TKr[b_]])
            pl, plr = psb()
            for k in range(8):
                op("pe", lambda e, k=k, j=j: e.matmul(pl[:, 0:16], lhsT=HT[:, k, j * 128:(j + 1) * 128], rhs=WRT[:, k, :], start=(k == 0), stop=(k == 7)),
                   [HTr[j], rg("wrt")], [plr], inc=(k == 7))
            mx, mxr = small(); sm, smr = small(); rsm, rsmr = small()
            op("dve", lambda e: e.reduce_max(out=mx, in_=pl[:, 0:16], axis=AX.X), [plr], [mxr])
            op("dve", lambda e: e.tensor_scalar(out=mx, in0=mx, scalar1=-1.0, scalar2=None, op0=ALU.mult), [mxr], [mxr])
            op("act", lambda e: e.activation(out=EXPT, in_=pl[:, 0:16], func=AF.Exp, bias=mx, accum_out=sm), [plr, mxr], [rg("expt"), smr])
            op("dve", lambda e: e.reciprocal(out=rsm, in_=sm), [smr], [rsmr])
            op("dve", lambda e, j=j: e.tensor_scalar(out=AFF[:, j, :], in0=EXPT, scalar1=rsm, scalar2=None, op0=ALU.mult), [rg("expt"), rsmr], [rg("aff")])
            pa, par = psb()
            op("pe", lambda e, j=j: e.transpose(out=pa[0:16, 0:128], in_=AFF[:, j, :], identity=IDF[:]), [rg("aff"), rg("idf")], [par])
            op("act", lambda e, j=j: e.copy(out=AFTS[0:16, j * 128:(j + 1) * 128], in_=pa[0:16, 0:128]), [par], [rg("afts")])

        y_stage(0)
        for tt in range(4):
            if tt + 1 < 4:
                y_stage(tt + 1)
            n_stage(tt)
    load("pool", ag2_in.ap(), AFTS[0:16, :], [rg("ag2in")], [rg("afts")])
    cc2 = S.dma_stream("cc2")
    cc_dma(cc2, lambda e: e.collective_compute("AllGather", ALU.bypass, replica_groups=GROUPS,
                                               ins=[ag2_in.ap().opt()], outs=[ag2_out.ap().opt()]), [rg("ag2in")], [rg("ag2out")])
    S.barrier()
    AG = view(14 * KB, [NTOK]); CMP = view(22 * KB, [NTOK]); o_ = 30 * KB
    WGE = view(96 * KB, [8, D], BF16); WUE = view(112 * KB, [8, D], BF16); WDE = view(128 * KB, [8, D], BF16)
    wst = [S.dma_stream(f"we{i}") for i in range(3)]

    wst += [S.dma_stream(f"we{i}") for i in range(3, 5)]

    def load_expert(ex):
        load_gu(ex); load_d(ex)

    def load_d(ex):
        S.dma("pool", wst[4], lambda e: e.dma_start(out=WDE[:], in_=w_down[ex].rearrange("(k p) n -> p k n", p=128)), writes=[rg("wde")])

    def load_gu(ex):
        for hf in range(2):
            cs = slice(hf * 512, (hf + 1) * 512)
            S.dma("pool", wst[hf * 2], lambda e, cs=cs: e.dma_start(out=WGE[:, :, cs], in_=w_gate[ex][:, cs].rearrange("(k p) n -> p k n", p=128)), writes=[rg(f"wge{hf}")])
            S.dma("pool", wst[hf * 2 + 1], lambda e, cs=cs: e.dma_start(out=WUE[:, :, cs], in_=w_up[ex][:, cs].rearrange("(k p) n -> p k n", p=128)), writes=[rg(f"wue{hf}")])
    LO = view(o_, [4]); o_ += 32
    load("pool", AG[0:64, :], ag2_out.ap(), [rg("ag")], [rg("ag2out")])
    load_expert(0)
    op("dve", lambda e: e.memset(LO[0:64, 0:1], 0.0), [], [rg("lo")])
    for it in range(27):
        wdt = 2.0 ** -(it + 1)
        op("dve", lambda e, wdt=wdt: e.tensor_scalar(out=CMP[0:64, :], in0=AG[0:64, :], scalar1=LO[0:64, 0:1], scalar2=wdt, op0=ALU.subtract, op1=ALU.is_gt),
           [rg("ag"), rg("lo")], [rg("cmp")])
        op("dve", lambda e: e.reduce_sum(out=LO[0:64, 2:3], in_=CMP[0:64, :], axis=AX.X), [rg("cmp")], [rg("lo")])
        pc_, pcr = psb()
        op("pe", lambda e, pc_=pc_: e.matmul(pc_[0:64, 0:1], lhsT=GM[:], rhs=LO[0:64, 2:3], start=True, stop=True), [rg("gm"), rg("lo")], [pcr])
        op("dve", lambda e, pc_=pc_: e.tensor_scalar(out=LO[0:64, 3:4], in0=pc_[0:64, 0:1], scalar1=CAP - 0.5, scalar2=None, op0=ALU.is_ge), [pcr], [rg("lo")])
        op("dve", lambda e, wdt=wdt: e.scalar_tensor_tensor(out=LO[0:64, 0:1], in0=LO[0:64, 3:4], scalar=wdt, in1=LO[0:64, 0:1], op0=ALU.mult, op1=ALU.add),
           [rg("lo")], [rg("lo")])
    op("dve", lambda e: e.scalar_tensor_tensor(out=CMP[0:64, :], in0=AG[0:64, :], scalar=LO[0:64, 0:1], in1=AG[0:64, :], op0=ALU.is_gt, op1=ALU.mult),
       [rg("ag"), rg("lo")], [rg("cmp")])
    for j in range(NT):
        pw_, pwr = psb()
        op("pe", lambda e, j=j, pw_=pw_: e.matmul(pw_[:, 0:16], lhsT=CMP[0:64, j * 128:(j + 1) * 128], rhs=SEL[:], start=True, stop=True), [rg("cmp"), rg("sel")], [pwr])
        op("act", lambda e, j=j, pw_=pw_: e.copy(out=WGT[:, j, :], in_=pw_[:, 0:16]), [pwr], [rg("wgt")])
    o_ = 0
    C2 = view(o_, [2, 128]); o_ += 1 * KB
    MK = view(o_, [256]); o_ += 1 * KB
    CSM = view(o_, [NT, 16]); o_ += 1 * KB
    SLF = view(o_, [256]); o_ += 1 * KB
    SLI = ARENA[:, 94 * KB:95 * KB].bitcast(U32)
    RST = [view(64 * KB + (4 + i) * 2 * KB, [D], BF16) for i in range(3)]
    RSTr = S.regions("rst", 3)
    rs_cnt = [0]
    load("sp", C2[:], consts2.rearrange("p (a b) -> p a b", b=128), [rg("c2")])
    op("dve", lambda e: e.tensor_scalar(out=MK, in0=WGT[:].rearrange("p j e -> p (j e)"), scalar1=0.0, scalar2=None, op0=ALU.is_gt), [rg("wgt")], [rg("mk")])
    op("dve", lambda e: e.memset(CSM[:, 0, :], 0.0), [], [rg("csm")])
    for j in range(1, NT):
        op("dve", lambda e, j=j: e.tensor_tensor(out=CSM[:, j, :], in0=CSM[:, j - 1, :], in1=MK[:, (j - 1) * 16:j * 16], op=ALU.add), [rg("csm"), rg("mk")], [rg("csm")])
    prk, prkr = psb()
    op("pe", lambda e: e.matmul(prk[:, 0:256], lhsT=C2[:, 0, :], rhs=MK, start=True, stop=False), [rg("c2"), rg("mk")], [prkr], inc=False)
    op("pe", lambda e: e.matmul(prk[:, 0:256], lhsT=C2[:, 1, :], rhs=CSM[:].rearrange("p j e -> p (j e)"), start=False, stop=True), [rg("c2"), rg("csm")], [prkr])
    op("dve", lambda e: e.scalar_tensor_tensor(out=SLF, in0=prk[:, 0:256], scalar=-4096.0, in1=MK, op0=ALU.add, op1=ALU.mult), [prkr, rg("mk")], [rg("slf")])
    op("dve", lambda e: e.tensor_scalar(out=SLI, in0=SLF, scalar1=4096.0, scalar2=None, op0=ALU.add), [rg("slf")], [rg("sli")])
    YSELr = S.regions("ysel", NE)

    def pdma(fn, reads, writes):
        st = lstreams["pool"][lidx["pool"] % len(lstreams["pool"])]; lidx["pool"] += 1
        return S.dma("pool", st, fn, reads=list(reads), writes=list(writes))

    SXr = [S.regions(f"sx{ex}_", NT) for ex in range(NE)]

    def scatter_expert(ex):
        for j in range(NT):
            i = rs_cnt[0] % 3; rs_cnt[0] += 1
            load("sp", RST[i], h2rows[j * 128:(j + 1) * 128, :], [RSTr[i]], [rg("h2rows")])
            pdma(lambda e, j=j, i=i: e.indirect_dma_start(
                out=xsel[ex], out_offset=bass.IndirectOffsetOnAxis(SLI[:, j * 16 + ex:j * 16 + ex + 1], 0),
                in_=RST[i], in_offset=None, bounds_check=CAP - 1, oob_is_err=False), [RSTr[i], rg("sli"), XSELr[ex]], [SXr[ex][j]])

    scatter_expert(0)
    scatter_expert(1)

    if STOP == "R":
        return finish()
    XACC = view(0, [NT, D]); o_ = 64 * KB
    NXS = 4
    XSE = [view(o_ + i * 2 * KB, [D], BF16) for i in range(NXS)]; o_ += 7 * 2 * KB
    YS = [view(o_ + i * 4 * KB, [D]) for i in range(2)]; o_ += 8 * KB
    GTL = [view(o_ + i * 4 * KB, [D]) for i in range(2)]; o_ += 8 * KB
    assert o_ <= 94 * KB
    o_ = 144 * KB
    SGT = [view(o_ + i * KB, [512], BF16) for i in range(2)]; o_ += 2 * KB
    assert o_ <= ARENA_BYTES, o_
    SGTr = S.regions("sgt", 2); XSEr = S.regions("xse", NXS); YSr = S.regions("ys", 2); GTLr = S.regions("gtl", 2)
    XST = HT[:, :, 0:CAP]; HTE = HT[:, :, CAP:2 * CAP]
    XSTr = S.regions("xst", 2); HTEr = S.regions("hte", 2)
    XAr = S.regions("xacc", NT)
    for i in range(2):
        op("dve", lambda e, i=i: e.memset(GTL[i], 0.0), [], [GTLr[i]])
    xs_cnt = [0]

    def prep_expert(ex):
        for st in range(8):
            i = xs_cnt[0] % NXS; xs_cnt[0] += 1
            extra = []
            S.dma("sp", lstreams["sp"][lidx["sp"] % 24], lambda e, i=i, st=st: e.dma_start(out=XSE[i], in_=xsel[ex][st * 128:(st + 1) * 128, :]),
                  reads=list(SXr[ex]), writes=[XSEr[i]] + extra)
            lidx["sp"] += 1
            pt3, pt3r = ptb()
            for k in range(8):
                op("pe", lambda e, k=k, i=i: e.transpose(out=pt3[:, k, :], in_=XSE[i][:, k * 128:(k + 1) * 128], identity=IDB[:]),
                   [XSEr[i], rg("idb")], [pt3r], inc=(k == 7))
            if st % 2 == 0:
                op("act", lambda e, st=st: e.copy(out=XST[:, :, st * 128:(st + 1) * 128], in_=pt3[:]), [pt3r], [XSTr[st // 4]])
            else:
                op("dve", lambda e, st=st: e.tensor_copy(out=XST[:, :, st * 128:(st + 1) * 128], in_=pt3[:]), [pt3r], [XSTr[st // 4]])

    def combine(ex):
        for j in range(NT):
            i = j % 2
            pdma(lambda e, j=j, i=i: e.indirect_dma_start(
                out=GTL[i], out_offset=None, in_=ysel[ex],
                in_offset=bass.IndirectOffsetOnAxis(SLI[:, j * 16 + ex:j * 16 + ex + 1], 0), bounds_check=CAP - 1, oob_is_err=False),
                [YSELr[ex], rg("sli")], [GTLr[i]])
            if ex == 0:
                op("dve", lambda e, j=j, i=i: e.tensor_scalar(out=XACC[:, j, :], in0=GTL[i], scalar1=WGT[:, j, ex:ex + 1], scalar2=None, op0=ALU.mult),
                   [GTLr[i], rg("wgt")], [XAr[j]])
            else:
                op("dve", lambda e, j=j, i=i: e.scalar_tensor_tensor(out=XACC[:, j, :], in0=GTL[i], scalar=WGT[:, j, ex:ex + 1], in1=XACC[:, j, :],
                                                                     op0=ALU.mult, op1=ALU.add), [GTLr[i], rg("wgt"), XAr[j]], [XAr[j]])

    for ex in range(NE):
        if ex + 2 < NE:
            scatter_expert(ex + 2)
        if ex == 0:
            prep_expert(0)
        for fc in range(8):
            for bk in range(2):
                hf = fc // 4
                pg_, pgr_ = psb(); pu_, pur_ = psb()
                for k in range(8):
                    op("pe", lambda e, k=k, fc=fc, pg_=pg_: e.matmul(pg_[:], lhsT=WGE[:, k, fc * 128:(fc + 1) * 128], rhs=XST[:, k, bk * 512:(bk + 1) * 512], start=(k == 0), stop=(k == 7)),
                       [XSTr[bk], rg(f"wge{hf}")], [pgr_], inc=(k == 7))
                for k in range(8):
                    op("pe", lambda e, k=k, fc=fc, pu_=pu_: e.matmul(pu_[:], lhsT=WUE[:, k, fc * 128:(fc + 1) * 128], rhs=XST[:, k, bk * 512:(bk + 1) * 512], start=(k == 0), stop=(k == 7)),
                       [XSTr[bk], rg(f"wue{hf}")], [pur_], inc=(k == 7))
                si = (bk * 8 + fc) % 2
                op("act", lambda e, pg_=pg_, si=si: e.activation(out=SGT[si], in_=pg_[:], func=AF.Silu), [pgr_], [SGTr[si]])
                op("dve", lambda e, pu_=pu_, si=si, fc=fc, bk=bk: e.tensor_tensor(out=HTE[:, fc, bk * 512:(bk + 1) * 512], in0=pu_[:], in1=SGT[si], op=ALU.mult),
                   [pur_, SGTr[si]], [HTEr[bk]])
        if ex + 1 < NE:
            load_gu(ex + 1)
            prep_expert(ex + 1)
        for st in range(8):
            i = st % 2
            for half in range(2):
                py, pyr = psb()
                for fc in range(8):
                    op("pe", lambda e, fc=fc, st=st, half=half, py=py: e.matmul(py[:], lhsT=HTE[:, fc, st * 128:(st + 1) * 128], rhs=WDE[:, fc, half * 512:(half + 1) * 512],
                                                                              start=(fc == 0), stop=(fc == 7)), [HTEr[st // 4], rg("wde")], [pyr], inc=(fc == 7))
                op("act", lambda e, i=i, half=half, py=py: e.copy(out=YS[i][:, half * 512:(half + 1) * 512], in_=py[:]), [pyr], [YSr[i]])
            load("sp", ysel[ex][st * 128:(st + 1) * 128, :], YS[i], [YSELr[ex]], [YSr[i]])
        if ex > 0:
            combine(ex - 1)
        if ex + 1 < NE:
            load_d(ex + 1)
    S.barrier()
    if STOP == "E":
        return finish()
    FGT = view(96 * KB, [D]); XF = [view(100 * KB + i * 4 * KB, [D]) for i in range(4)]; OT = [view(116 * KB + i * 4 * KB, [D]) for i in range(4)]
    JK = view(132 * KB, [D])
    XFr = S.regions("xf", 4); OTr = S.regions("ot", 4)
    load("sp", FGT, fin_g.partition_broadcast(128), [rg("fgt")])
    ost = [S.dma_stream(f"o{i}") for i in range(4)]
    for j in range(NT):
        load("sp", XF[j % 4], xmid[j * 128:(j + 1) * 128, :], [XFr[j % 4]], [rg("xmid")]) if j < 4 else None
    ex = NE - 1
    for j in range(NT):
        i = j % 4
        g_ = j % 2
        pdma(lambda e, j=j, g_=g_: e.indirect_dma_start(
            out=GTL[g_], out_offset=None, in_=ysel[ex],
            in_offset=bass.IndirectOffsetOnAxis(SLI[:, j * 16 + ex:j * 16 + ex + 1], 0), bounds_check=CAP - 1, oob_is_err=False),
            [YSELr[ex], rg("sli")], [GTLr[g_]])
        op("dve", lambda e, j=j, g_=g_: e.scalar_tensor_tensor(out=XACC[:, j, :], in0=GTL[g_], scalar=WGT[:, j, ex:ex + 1], in1=XACC[:, j, :],
                                                               op0=ALU.mult, op1=ALU.add), [GTLr[g_], rg("wgt"), XAr[j]], [XAr[j]])
        if j >= 4:
            load("sp", XF[i], xmid[j * 128:(j + 1) * 128, :], [XFr[i]], [rg("xmid")])
        op("dve", lambda e, j=j: e.tensor_tensor(out=XACC[:, j, :], in0=XACC[:, j, :], in1=GT2[:], op=ALU.mult), [XAr[j], rg("gt2")], [XAr[j]])
        op("dve", lambda e, j=j, i=i: e.tensor_tensor(out=XF[i], in0=XF[i], in1=XACC[:, j, :], op=ALU.add), [XAr[j], XFr[i]], [XFr[i]])
        ssq, ssqr = small(); sd, sdr = small(); rs, rsr = small()
        op("act", lambda e, i=i: e.activation(out=JK, in_=XF[i], func=AF.Square, accum_out=ssq), [XFr[i]], [rg("jk"), ssqr])
        op("act", lambda e: e.activation(out=sd, in_=ssq, func=AF.Sqrt, scale=1.0 / D, bias=EPS), [ssqr], [sdr])
        op("dve", lambda e: e.reciprocal(out=rs, in_=sd), [sdr], [rsr])
        op("dve", lambda e, i=i: e.scalar_tensor_tensor(out=OT[i], in0=XF[i], scalar=rs, in1=FGT, op0=ALU.mult, op1=ALU.mult), [XFr[i], rsr, rg("fgt")], [OTr[i]])
        S.dma("sp", ost[i], lambda e, j=j, i=i: e.dma_start(out=out[j * 128:(j + 1) * 128, :], in_=OT[i]), reads=[OTr[i]], writes=[rg("outd")])
    S.wait_all("sp", [(o_s, S.dma_count[o_s]) for o_s in ost])
    S.emit()
    return nc


_NC = [None]


def _NEW():
    import os
    return 1 if os.environ.get("KSTOP", "") else NE


def _consts():
    j = np.arange(128)[:, None]; i = np.arange(128)[None, :]
    same = (j // 64) == (i // 64)
    lj = j % 64
    sc = -1.0 / 16.0
    C = np.zeros((128, 9, 128), np.float32)
    C[:, 0] = sc * (same & (j <= i)); C[:, 1] = sc * (same & (j >= i))
    C[:, 2] = sc * (same & (j > i)); C[:, 3] = sc * (same & (j < i))
    C[:, 4] = sc * same * ((j <= i).astype(np.float32) - (lj <= 31).astype(np.float32))
    C[:, 5] = sc * same * ((j >= i).astype(np.float32) - (lj >= 32).astype(np.float32))
    C[:, 6] = (same & (i >= j)); C[:, 7] = (same & (j >= i))
    C[:, 8, 0] = sc * (np.arange(128) // 64 == 0); C[:, 8, 1] = sc * (np.arange(128) // 64 == 1); C[:, 8, 2] = sc
    return C.reshape(128, 9 * 128)


def kernel(x, c, ctx, c_ctx, ada_w, ada_b, norm1_g, norm2_g, w_in, w_decay_up, b_decay,
           gla_norm_g, w_gla_proj, pool_w, pool_scale, w_pool_proj, w_out, w_router,
           w_gate_e, w_up_e, w_down_e, final_norm_g):
    f = lambda a: np.ascontiguousarray(np.asarray(a, dtype=np.float32))
    x = f(x); c = f(c); ctx = f(ctx); c_ctx = f(c_ctx)
    fm = lambda v: f(v).reshape(-1, 128).T
    if _NC[0] is None:
        _NC[0] = build_program()
    nc = _NC[0]
    vecs = np.zeros((128, 32), np.float32)
    vecs[:, 0:8] = fm(norm1_g[0]); vecs[:, 8:16] = fm(norm2_g[0]); vecs[:, 16:20] = fm(pool_scale[0]); vecs[:, 20] = f(gla_norm_g[0])
    w_upP = np.zeros((32, 512), np.float32)
    w_upP[0:16, 0:256] = f(w_decay_up[0, 0]); w_upP[16:32, 256:512] = f(w_decay_up[0, 1])
    consts = _consts()
    pp = np.arange(128)
    consts2 = np.concatenate([(pp[:, None] < pp[None, :]).astype(np.float32), np.ones((128, 128), np.float32)], axis=1)
    gmat = np.zeros((64, 64), np.float32)
    for r in range(4):
        for r2 in range(4):
            gmat[r * 16:(r + 1) * 16, r2 * 16:(r2 + 1) * 16] = np.eye(16, dtype=np.float32)
    shared = {
        "ada_w": f(ada_w[0]), "ada_bT": fm(ada_b[0]), "ada_b": f(ada_b[0]).reshape(1, -1), "vecs": vecs,
        "b_decay": f(b_decay[0]).reshape(1, 512), "fin_g": f(final_norm_g).reshape(1, D), "w_in": f(w_in[0]), "w_upP": w_upP,
        "w_glap": f(w_gla_proj[0]), "pool_w": f(pool_w[0]), "w_poolp": f(w_pool_proj[0]), "w_out": f(w_out[0]), "w_router": f(w_router[0]),
        "w_gate": f(w_gate_e[0])[:_NEW()], "w_up": f(w_up_e[0])[:_NEW()], "w_down": f(w_down_e[0])[:_NEW()], "consts": consts, "consts2": consts2, "gmat": gmat,
        "pmask": np.zeros((1, NEXT), np.float32),
    }
    in_maps = []
    for core in range(8):
        b, s_ = core // 4, core % 4
        t0 = 2048 * s_
        xe = np.zeros((NEXT, D), np.float32)
        lo, hi = max(t0 - 512, 0), min(t0 + 2560, 8192)
        xe[lo - (t0 - 512):hi - (t0 - 512)] = x[b, lo:hi]
        cv = np.zeros((128, 16), np.float32)
        cv[:, 0:8] = fm(c[b]); cv[:, 8:16] = fm(c_ctx)
        pcz = np.zeros((128, 16), np.float32)
        pcz[:, 2] = 1.0 if s_ > 0 else 0.0; pcz[:, 3] = 1.0 if s_ < 3 else 0.0; pcz[:, 12] = 1.0
        for r in range(4):
            pcz[:, 4 + r] = 1.0 if r < s_ else 0.0
            pcz[:, 8 + r] = 1.0 if r > s_ else 0.0
        inv = np.zeros((4, 32, 64), np.float32)
        for g, w in enumerate(WINS):
            rows = 32 * s_ + np.arange(32)
            cr = np.minimum(rows + w // 2, 128) - np.maximum(rows - w // 2, 0)
            cols = np.arange(64)
            cc_ = np.minimum(cols + w // 2, 64) - np.maximum(cols - w // 2, 0)
            inv[g] = 1.0 / (cr[:, None] * cc_[None, :]).astype(np.float32)
        selm = np.zeros((64, 16), np.float32)
        selm[s_ * 16:(s_ + 1) * 16] = np.eye(16, dtype=np.float32)
        m = dict(shared)
        m.update({"xext": xe, "ctx": f(ctx[b]), "cvec": cv, "percore": pcz, "invcnt": inv.reshape(1, -1), "sel": selm})
        in_maps.append(m)
    res = run_bass_kernel_spmd(nc, in_maps, core_ids=list(range(8)))
    outp = np.zeros((2, 8192, D), np.float32)
    for core in range(8):
        b, s_ = core // 4, core % 4
        outp[b, 2048 * s_:2048 * (s_ + 1)] = res.results[core]["out"]
    return outp
```

```python
import numpy as np
import concourse.bass as bass
import concourse.mybir as mybir
from concourse.bass_utils import run_bass_kernel_spmd

F32 = mybir.dt.float32
BF16 = mybir.dt.bfloat16
AF = mybir.ActivationFunctionType
ALU = mybir.AluOpType
AX = mybir.AxisListType

ENGS = ("pe", "act", "dve", "pool", "sp")


class Region:
    __slots__ = ("name", "last_write", "readers", "excl")

    def __init__(self, name, excl=False):
        self.name = name
        self.last_write = None
        self.readers = []
        self.excl = excl


class _Rec:
    def __getattr__(self, name):
        return lambda *a, **k: (name, a, k)


_REC = _Rec()


_BCREG = {}


def _replay(rec):
    name, a, k = rec
    if name != "indirect_dma_start":
        return lambda engine: getattr(engine, name)(*a, **k)

    def f(engine):
        key = id(engine)
        if key not in _BCREG:
            _BCREG[key] = engine.to_reg(k["bounds_check"])
        return getattr(engine, name)(*a, **dict(k, bounds_check=_BCREG[key]))
    return f


class Sched:
    def __init__(self, nc):
        self.nc = nc
        self.eng = {"pe": nc.tensor, "act": nc.scalar, "dve": nc.vector,
                    "pool": nc.gpsimd, "sp": nc.sync}
        self.items = {e: [] for e in ENGS}
        self.count = {e: 0 for e in ENGS}
        self.pending = {e: False for e in ENGS}
        self.seen = {e: {} for e in ENGS}
        self.clk = {}
        self.nsem = 0
        self.dma_count = {}
        self.streams = list(ENGS)

    def region(self, name):
        return Region(name)

    def regions(self, name, n, excl=False):
        return [Region(f"{name}{i}", excl) for i in range(n)]

    def dma_stream(self, name):
        s = f"dma:{name}:{len(self.streams)}"
        self.streams.append(s)
        self.dma_count[s] = 0
        return s

    def _need(self, e, dep):
        st, val = dep
        if self.seen[e].get(st, 0) >= val:
            return
        if st == e and val > self.count[e]:
            return
        self.items[e].append(("wait", st, val))
        ck = self.clk.get(dep)
        se = self.seen[e]
        if ck:
            for k, v in ck.items():
                if se.get(k, 0) < v:
                    se[k] = v
        if se.get(st, 0) < val:
            se[st] = val

    def _deps(self, e, reads, writes):
        for r in reads:
            if r.last_write is not None:
                self._need(e, r.last_write)
            if r.excl:
                for rd in r.readers:
                    if rd[0] != e:
                        self._need(e, rd)
        for w in writes:
            if w.last_write is not None:
                self._need(e, w.last_write)
            for rd in w.readers:
                self._need(e, rd)

    def _record(self, tok, reads, writes):
        for r in reads:
            r.readers.append(tok)
        for w in writes:
            w.last_write = tok
            w.readers = []

    def op(self, e, fn, reads=(), writes=(), inc=True):
        fn = _replay(fn(_REC))
        self._deps(e, reads, writes)
        val = self.count[e] + 1
        tok = (e, val)
        if inc:
            self.count[e] = val
            self.items[e].append(("op", fn, e, 1))
            ck = dict(self.seen[e])
            ck[e] = val
            self.clk[tok] = ck
            self.pending[e] = False
        else:
            self.items[e].append(("op", fn, None, 0))
            self.pending[e] = True
        self._record(tok, reads, writes)
        return tok

    def dma(self, q, stream, fn, reads=(), writes=()):
        fn = _replay(fn(_REC))
        self._deps(q, reads, writes)
        if self.dma_count[stream] > 0:
            self._need(q, (stream, self.dma_count[stream]))
        self.dma_count[stream] += 16
        val = self.dma_count[stream]
        tok = (stream, val)
        self.items[q].append(("op", fn, stream, 16))
        self.clk[tok] = dict(self.seen[q])
        self._record(tok, reads, writes)
        return tok

    def wait_all(self, e, toks):
        for t in toks:
            self._need(e, t)

    def emit(self):
        nc = self.nc
        for e in ENGS:
            assert not self.pending[e], f"engine {e} ends with a non-inc op"
        sems = {}
        import contextlib
        with contextlib.ExitStack() as es:
            for st in self.streams:
                nm = st.replace(":", "_")
                sems[st] = es.enter_context(nc.semaphore("s_" + nm))
            block = es.enter_context(nc.Block())

            def run(e, engine):
                for it in self.items[e]:
                    if it[0] == "wait":
                        engine.wait_ge(sems[it[1]], it[2])
                    else:
                        _, fn, st, iv = it
                        ins = fn(engine)
                        if st is not None:
                            ins.then_inc(sems[st], iv)

            @block.tensor
            def _(t):
                run("pe", t)

            @block.scalar
            def _(t):
                run("act", t)

            @block.vector
            def _(t):
                run("dve", t)

            @block.gpsimd
            def _(t):
                run("pool", t)

            @block.sync
            def _(t):
                run("sp", t)

    def barrier(self):
        toks = [(e, self.count[e]) for e in ENGS if self.count[e] > 0]
        toks += [(s, v) for s, v in self.dma_count.items() if v > 0]
        for e in ENGS:
            for t in toks:
                if t[0] != e:
                    self._need(e, t)


D = 1024
NTOK = 2048
NEXT = 3072
NT = 16
CH = 64
WINS = (2, 4, 8, 16)
NE = 16
CAP = 1024
EPS = 1e-6
U8 = mybir.dt.uint8
C_K, C_V, C_R, C_Q, C_G, C_P, C_M = 0, 256, 768, 800, 1056, 1568, 2080


def build_program():
    nc = bass.Bass("TRN2", target_bir_lowering=False)
    S = Sched(nc)

    def din(name, shape):
        return nc.dram_tensor(name, list(shape), F32, kind="ExternalInput").ap()

    xext = din("xext", [NEXT, D]); ctx = din("ctx", [256, D]); cvec = din("cvec", [128, 16])
    ada_w = din("ada_w", [D, 6 * D]); ada_bT = din("ada_bT", [128, 48]); ada_b = din("ada_b", [1, 6 * D])
    vecs = din("vecs", [128, 32]); b_decay = din("b_decay", [1, 512]); fin_g = din("fin_g", [1, D])
    w_in = din("w_in", [D, 4128]); w_upP = din("w_upP", [32, 512])
    w_glap = din("w_glap", [512, D]); pool_w = din("pool_w", [4, 128, 128]); w_poolp = din("w_poolp", [512, D])
    w_out = din("w_out", [D, D]); w_router = din("w_router", [D, 16])
    import os as _os
    NEW = 1 if _os.environ.get("KSTOP", "") else NE
    w_gate = din("w_gate", [NEW, D, D]); w_up = din("w_up", [NEW, D, D]); w_down = din("w_down", [NEW, D, D])
    consts = din("consts", [128, 9 * 128]); percore = din("percore", [128, 16])
    pmask = din("pmask", [1, NEXT]); invcnt = din("invcnt", [1, 4 * NTOK])
    sel = din("sel", [64, 16]); gmat = din("gmat", [64, 64]); consts2 = din("consts2", [128, 256])
    out = nc.dram_tensor("out", [NTOK, D], F32, kind="ExternalOutput").ap()
    xmid = nc.dram_tensor("xmid", [NTOK, D], F32).ap()
    h2rows = nc.dram_tensor("h2rows", [NTOK, D], BF16).ap()
    xsel = [nc.dram_tensor(f"xsel{i}", [CAP, D], BF16).ap() for i in range(NE)]
    ysel = [nc.dram_tensor(f"ysel{i}", [CAP, D], F32).ap() for i in range(NE)]
    U32 = mybir.dt.uint32
    ag1_in = nc.dram_tensor("ag1_in", [64, 1032], F32); ag1_out = nc.dram_tensor("ag1_out", [256, 1032], F32)
    ag2_in = nc.dram_tensor("ag2_in", [16, NTOK], F32); ag2_out = nc.dram_tensor("ag2_out", [64, NTOK], F32)
    GROUPS = [[0, 1, 2, 3], [4, 5, 6, 7]]

    def sb(name, shape, dt=F32):
        return nc.alloc_sbuf_tensor(name, list(shape), dt)

    CONS = sb("cons", [128, 9, 128])
    IDB = sb("idb", [128, 128], BF16); IDF = sb("idf", [128, 128])
    PC = sb("pc", [128, 16]); VEC = sb("vec", [128, 32]); CV = sb("cv", [128, 16]); SC = sb("scv", [128, 16])
    ABT = sb("abt", [128, 48]); MODF = sb("modf", [128, 64]); AB = sb("ab", [128, 48])
    GT1 = sb("gt1", [128, D]); GT2 = sb("gt2", [128, D]); BDEC = sb("bdec", [128, 512])
    WUP = sb("wup", [32, 512]); WRT = sb("wrt", [128, 8, 16], BF16)
    AFF = sb("aff", [128, NT, 16]); WGT = sb("wgt", [128, NT, 16])
    SEL = sb("selm", [64, 16]); GM = sb("gm", [64, 64])
    HT = sb("ht", [128, 8, NTOK], BF16)
    SMALL = sb("small", [128, 64])
    HF = sb("hf", [64, 2, 4, 128]); SINIT = sb("sinit", [64, 2, 4, 128])
    ARENA_BYTES = 146 * 1024
    ARENA = sb("arena", [128, ARENA_BYTES], U8)
    print("sbuf remaining", nc.sbuf_bytes_remaining)

    def view(off, shape, dt=F32, parts=128):
        esz = 4 if dt == F32 else 2
        n = 1
        for s_ in shape:
            n *= s_
        assert off % 32 == 0 and off + n * esz <= ARENA_BYTES, (off, n * esz)
        v = ARENA[0:parts, off:off + n * esz].bitcast(dt)
        if len(shape) == 2:
            return v.rearrange("p (a b) -> p a b", b=shape[1])
        if len(shape) == 3:
            return v.rearrange("p (a b c) -> p a b c", b=shape[1], c=shape[2])
        if len(shape) == 4:
            return v.rearrange("p (a b c d) -> p a b c d", b=shape[1], c=shape[2], d=shape[3])
        return v

    KB = 1024
    PTB = [nc.alloc_psum_tensor(f"ptb{i}", [128, 8, 128], BF16) for i in range(2)]
    PTBr = S.regions("ptb", 2, True)
    PSB = [nc.alloc_psum_tensor(f"psb{i}", [128, 512], F32) for i in range(6)]
    PSBr = S.regions("psb", 6, True)
    cnt = {"pt": 0, "ps": 0, "sm": 0}

    def ptb():
        i = cnt["pt"] % 2; cnt["pt"] += 1
        return PTB[i], PTBr[i]

    def psb():
        i = cnt["ps"] % 6; cnt["ps"] += 1
        return PSB[i], PSBr[i]

    SMr = S.regions("sm", 64)

    def small():
        i = cnt["sm"] % 64; cnt["sm"] += 1
        return SMALL[:, i:i + 1], SMr[i]

    R = {}

    def rg(name):
        if name not in R:
            R[name] = S.region(name)
        return R[name]

    dcount = [0]

    lstreams = {"sp": [S.dma_stream(f"lsp{i}") for i in range(24)], "pool": [S.dma_stream(f"lpl{i}") for i in range(24)]}
    lidx = {"sp": 0, "pool": 0}

    def load(q, dst, src, wr, rd=()):
        st = lstreams[q][lidx[q] % len(lstreams[q])]; lidx[q] += 1
        return S.dma(q, st, lambda e: e.dma_start(out=dst, in_=src), reads=list(rd), writes=list(wr))

    def op(e, fn, reads=(), writes=(), inc=True):
        return S.op(e, fn, reads=list(reads), writes=list(writes), inc=inc)

    import os
    STOP = os.environ.get("KSTOP", "")
    KSUB = int(os.environ.get("KSUB", "0"))

    def finish():
        S.barrier()
        S.emit()
        return nc

    load("sp", CONS[:], consts.rearrange("p (a b) -> p a b", b=128), [rg("cons")])
    load("sp", PC[:], percore, [rg("pc")]); load("sp", VEC[:], vecs, [rg("vec")]); load("sp", CV[:], cvec, [rg("cv")])
    load("sp", ABT[:], ada_bT, [rg("abt")]); load("sp", WUP[:], w_upP, [rg("wup")])
    load("sp", SEL[:], sel, [rg("sel")]); load("sp", GM[:], gmat, [rg("gm")])
    load("sp", BDEC[:], b_decay.partition_broadcast(128), [rg("bdec")])
    load("sp", GT1[:], ada_b[:, 2 * D:3 * D].partition_broadcast(128), [rg("gt1")])
    load("sp", GT2[:], ada_b[:, 5 * D:6 * D].partition_broadcast(128), [rg("gt2")])
    load("pool", WRT[:], w_router.rearrange("(k p) n -> p k n", p=128), [rg("wrt")])
    Uf, Ub, Lf, Lb, Mf, Mb, MKF, MKB = [CONS[:, i, :] for i in range(8)]
    BLK = CONS[:, 8, 0:2]; ONEC = CONS[:, 8, 2:3]
    op("pool", lambda e: e.memset(IDF[:], 0.0), [], [rg("idf")])
    op("pool", lambda e: e.affine_select(out=IDF[:], in_=IDF[:], pattern=[[-1, 128]], compare_op=ALU.not_equal,
                                         fill=1.0, base=0, channel_multiplier=1), [rg("idf")], [rg("idf")])
    op("dve", lambda e: e.tensor_copy(out=IDB[:], in_=IDF[:]), [rg("idf")], [rg("idb")])
    op("act", lambda e: e.activation(out=SC[:], in_=CV[:], func=AF.Silu), [rg("cv")], [rg("sc")])
    SCREP = view(0, [8, 128])
    op("dve", lambda e: e.memset(SCREP[:], 1.0), [], [rg("screp")])
    for k in range(8):
        op("dve", lambda e, k=k: e.tensor_scalar(out=SCREP[:, k, :], in0=SCREP[:, k, :], scalar1=SC[:, k:k + 1], scalar2=None, op0=ALU.mult),
           [rg("sc"), rg("screp")], [rg("screp")])
    ADW = [view(8 * KB + i * 32 * KB, [8, D]) for i in range(2)]
    ADWr = S.regions("adw", 2)
    modp, modpr = psb()
    grp_fm = [0, 1, 3, 4]
    modp_v = modp[:, 0:64].rearrange("p (a b) -> p a b", b=2)
    order = [0, 1, 3, 4, 2, 5]
    for n_, g in enumerate(order):
        slot = n_ % 2
        load("sp", ADW[slot][:], ada_w[:, g * D:(g + 1) * D].rearrange("(k p) n -> p k n", p=128), [ADWr[slot]])
        if g in grp_fm:
            gi = grp_fm.index(g)
            for cc in range(8):
                for k in range(8):
                    op("pe", lambda e, gi=gi, cc=cc, k=k, slot=slot: e.matmul(
                        modp_v[:, gi * 8 + cc, :], lhsT=ADW[slot][:, k, cc * 128:(cc + 1) * 128], rhs=SC[:, k:16:8],
                        start=(k == 0), stop=(k == 7)), [ADWr[slot], rg("sc")], [modpr], inc=(k == 7 and cc == 7))
        else:
            GT = GT1 if g == 2 else GT2
            gr = rg("gt1") if g == 2 else rg("gt2")
            for half in range(2):
                ps, psr = psb()
                for k in range(8):
                    op("pe", lambda e, k=k, slot=slot, half=half, ps=ps: e.matmul(
                        ps[:], lhsT=SCREP[:, k, :], rhs=ADW[slot][:, k, half * 512:(half + 1) * 512],
                        start=(k == 0), stop=(k == 7)), [ADWr[slot], rg("screp")], [psr], inc=(k == 7))
                op("dve", lambda e, ps=ps, GT=GT, half=half: e.tensor_tensor(
                    out=GT[:, half * 512:(half + 1) * 512], in0=ps[:], in1=GT[:, half * 512:(half + 1) * 512], op=ALU.add),
                   [psr, gr], [gr])
    MODFv = MODF[:].rearrange("p (a b) -> p a b", b=2)
    for gi, g in enumerate(grp_fm):
        for j in range(2):
            op("dve", lambda e, gi=gi, g=g, j=j: e.tensor_tensor(
                out=MODFv[:, gi * 8:(gi + 1) * 8, j], in0=modp_v[:, gi * 8:(gi + 1) * 8, j], in1=ABT[:, g * 8:(g + 1) * 8], op=ALU.add),
               [modpr, rg("abt")], [rg("modf")])
    A1, B1, ACX, BCX, A2, B2 = [AB[:, i * 8:(i + 1) * 8] for i in range(6)]

    def mkAB(A, B, gi_sh, gi_sc, j, gvec):
        op("dve", lambda e: e.scalar_tensor_tensor(out=A, in0=MODFv[:, gi_sc * 8:(gi_sc + 1) * 8, j], scalar=1.0, in1=gvec,
                                                    op0=ALU.add, op1=ALU.mult), [rg("modf"), rg("vec")], [rg("ab")])
        op("dve", lambda e: e.tensor_copy(out=B, in_=MODFv[:, gi_sh * 8:(gi_sh + 1) * 8, j]), [rg("modf")], [rg("ab")])

    mkAB(A1, B1, 0, 1, 0, VEC[:, 0:8]); mkAB(ACX, BCX, 0, 1, 1, VEC[:, 0:8]); mkAB(A2, B2, 2, 3, 0, VEC[:, 8:16])
    S.barrier()

    if STOP == "0":
        return finish()
    XT = [view(0 * KB + i * 4 * KB, [D]) for i in range(2)]
    XS = [view(8 * KB + i * 2 * KB, [D], BF16) for i in range(2)]
    JUNK = view(12 * KB, [D], BF16)
    XTr = S.regions("xt", 2); XSr = S.regions("xs", 2); JUNKr = rg("junk")
    tcount = [0]

    def norm_tile(src_dram, A, B, dst, dst_r, xt_pre=None):
        i = tcount[0] % 2; tcount[0] += 1
        xt, xs = XT[i], XS[i]
        if xt_pre is None:
            load("sp", xt, src_dram, [XTr[i]])
            xr = XTr[i]
        else:
            xt, xr = xt_pre
        ssq, ssqr = small(); sd, sdr = small(); rs, rsr = small()
        op("act", lambda e: e.activation(out=JUNK, in_=xt, func=AF.Square, accum_out=ssq), [xr], [JUNKr, ssqr])
        op("act", lambda e: e.activation(out=sd, in_=ssq, func=AF.Sqrt, scale=1.0 / D, bias=EPS), [ssqr], [sdr])
        op("dve", lambda e: e.reciprocal(out=rs, in_=sd), [sdr], [rsr])
        op("dve", lambda e: e.tensor_scalar(out=xs, in0=xt, scalar1=rs, scalar2=None, op0=ALU.mult), [xr, rsr], [XSr[i]])
        pt, ptr = ptb()
        for k in range(8):
            op("pe", lambda e, k=k: e.transpose(out=pt[:, k, :], in_=xs[:, k * 128:(k + 1) * 128], identity=IDB[:]),
               [XSr[i], rg("idb")], [ptr], inc=(k == 7))
        for k in range(8):
            if i == 0:
                op("act", lambda e, k=k: e.activation(out=dst[:, k, :], in_=pt[:, k, :], func=AF.Identity,
                                                      scale=A[:, k:k + 1], bias=B[:, k:k + 1]), [ptr, rg("ab")], [dst_r])
            else:
                op("dve", lambda e, k=k: e.tensor_scalar(out=dst[:, k, :], in0=pt[:, k, :], scalar1=A[:, k:k + 1], scalar2=B[:, k:k + 1],
                                                         op0=ALU.mult, op1=ALU.add), [ptr, rg("ab")], [dst_r])

    def wload(dst, cols, ncols, r):
        load("pool", dst, w_in[:, cols:cols + ncols].rearrange("(k p) n -> p k n", p=128), [r])

    HTr = S.regions("ht", NT)
    o_ = 14 * KB + 24 * KB + 512
    HTH = view(o_, [8, 1024], BF16); o_ += 16 * KB
    WP = view(o_, [8, 512], BF16); o_ += 8 * KB
    PD = view(o_, [48, 80]); o_ += 15 * KB
    TA = view(o_, [48, 80]); o_ += 15 * KB
    TB = view(o_, [48, 80]); o_ += 15 * KB
    INV = view(o_, [32, 64]); INVF = view(o_, [NTOK]); o_ += 8 * KB
    POOLED = view(o_, [NTOK], BF16); o_ += 4 * KB
    PW = view(o_, [4, 128], BF16); o_ += 1 * KB
    MIXT = view(130 * KB, [4, NTOK], BF16)
    assert o_ <= 130 * KB, o_
    HTHr = S.regions("hth", 8)
    wload(WP[:], C_P, 512, rg("wp"))
    load("pool", PW[:], pool_w.rearrange("g p c -> p g c"), [rg("pw")])
    go_ = 14 * KB
    WK = view(go_, [8, 256], BF16); go_ += 4 * KB
    WV = view(go_, [8, 512], BF16); go_ += 8 * KB
    WR = view(go_, [8, 32], BF16); go_ += 512
    WQ = view(go_, [8, 256], BF16); go_ += 4 * KB
    WG = view(go_, [8, 512], BF16); go_ += 8 * KB
    wload(WK[:], C_K, 256, rg("wk")); wload(WV[:], C_V, 512, rg("wv")); wload(WR[:], C_R, 32, rg("wr"))
    wload(WQ[:], C_Q, 256, rg("wq")); wload(WG[:], C_G, 512, rg("wg"))
    op("pool", lambda e: e.memset(PD[:], 0.0), [], [rg("pd")])
    for t in range(24):
        if t < 4:
            dst, dr = HTH[:, :, t * 128:(t + 1) * 128], HTHr[t]
        elif t < 20:
            dst, dr = HT[:, :, (t - 4) * 128:(t - 3) * 128], HTr[t - 4]
        else:
            dst, dr = HTH[:, :, (t - 16) * 128:(t - 15) * 128], HTHr[t - 16]
        norm_tile(xext[t * 128:(t + 1) * 128, :], A1, B1, dst, dr)

    ZT0 = view(122 * KB, [D], BF16)
    op("pool", lambda e: e.memset(ZT0, 0.0), [], [rg("zt0")])
    XSELr = S.regions("xsel", NE)
    for ex in range(NE):
        for st in range(8):
            load("sp", xsel[ex][st * 128:(st + 1) * 128, :], ZT0, [XSELr[ex]], [rg("zt0")])

    def ht_ext_block(bk):
        if bk == 0:
            return HTH[:, :, 0:512], HTHr[0:4]
        if bk == 5:
            return HTH[:, :, 512:1024], HTHr[4:8]
        return HT[:, :, (bk - 1) * 512:bk * 512], HTr[(bk - 1) * 4:bk * 4]

    for g in range(4):
        w = WINS[g]
        load("sp", INVF, invcnt[:, g * NTOK:(g + 1) * NTOK].partition_broadcast(128), [rg("inv")])
        for bk in range(6):
            ps, psr = psb()
            hsrc, hr = ht_ext_block(bk)
            for k in range(8):
                op("pe", lambda e, k=k, ps=ps, hsrc=hsrc: e.matmul(ps[:], lhsT=WP[:, k, g * 128:(g + 1) * 128], rhs=hsrc[:, k, :],
                                                              start=(k == 0), stop=(k == 7)), [rg("wp")] + hr, [psr], inc=(k == 7))
            vcol = 2 if bk == 0 else (3 if bk == 5 else 12)
            op("dve", lambda e, ps=ps, bk=bk, vcol=vcol: e.tensor_scalar(
                out=PD[:, bk * 8:(bk + 1) * 8, 8:72], in0=ps[:].rearrange("p (r c) -> p r c", c=64),
                scalar1=PC[:, vcol:vcol + 1], scalar2=None, op0=ALU.mult), [psr, rg("pc")], [rg("pd")])
        src, srr, m = PD, rg("pd"), 1
        bufs = [(TA, rg("ta")), (TB, rg("tb"))]
        bi = 0
        while m < w:
            dstb, dbr = bufs[bi]; bi ^= 1
            wd = 80 - 2 * m + 1
            op("dve", lambda e, src=src, dstb=dstb, m=m, wd=wd: e.tensor_tensor(
                out=dstb[:, :, 0:wd], in0=src[:, :, 0:wd], in1=src[:, :, m:m + wd], op=ALU.add), [srr], [dbr])
            src, srr, m = dstb, dbr, 2 * m
        coff = 8 - w // 2
        m = 1
        first = True
        while m < w:
            dstb, dbr = bufs[bi]; bi ^= 1
            nr = 48 - 2 * m + 1
            c0 = coff if first else 0
            op("dve", lambda e, src=src, dstb=dstb, m=m, nr=nr, c0=c0: e.tensor_tensor(
                out=dstb[:, 0:nr, 0:64], in0=src[:, 0:nr, c0:c0 + 64], in1=src[:, m:m + nr, c0:c0 + 64], op=ALU.add), [srr], [dbr])
            src, srr, m, first = dstb, dbr, 2 * m, False
        roff = 8 - w // 2
        dstb, dbr = bufs[bi]; bi ^= 1
        op("dve", lambda e, src=src, dstb=dstb: e.tensor_tensor(
            out=dstb[:, 0:32, 0:64], in0=src[:, roff:roff + 32, 0:64], in1=INV[:, :, :], op=ALU.mult), [srr, rg("inv")], [dbr])
        op("dve", lambda e, dstb=dstb: e.tensor_tensor(
            out=POOLED.rearrange("p (r c) -> p r c", c=64), in0=dstb[:, 0:32, 0:64], in1=PD[:, 8:40, 8:72], op=ALU.subtract),
           [dbr, rg("pd")], [rg("pooled")])
        for bk in range(4):
            ps, psr = psb()
            op("pe", lambda e, ps=ps, bk=bk: e.matmul(ps[:], lhsT=PW[:, g, :], rhs=POOLED[:, bk * 512:(bk + 1) * 512], start=True, stop=True),
               [rg("pw"), rg("pooled")], [psr])
            op("act", lambda e, ps=ps, bk=bk: e.activation(out=MIXT[:, g, bk * 512:(bk + 1) * 512], in_=ps[:], func=AF.Copy,
                                                           scale=VEC[:, 16 + g:17 + g]), [psr, rg("vec")], [rg("mixt")])
    S.barrier()

    if STOP == "P":
        return finish()
    o_ = 14 * KB
    o_ = go_
    KVB = view(o_, [32, 4, 128], BF16); o_ += 32 * KB
    KVC = view(o_, [4, 4, 128], BF16); o_ += 4 * KB
    DEC = view(o_, [4, 2, 32]); o_ += 1 * KB
    DECC = view(o_, [4, 2, 4]); o_ += 128
    RTS = view(o_, [128]); o_ += 512
    Z = view(o_, [512]); o_ += 2 * KB
    LL = view(o_, [512]); o_ += 2 * KB
    EKS = view(o_, [512]); o_ += 2 * KB
    KD = view(o_, [2, 256], BF16); o_ += 1 * KB
    VT = view(o_, [512], BF16); o_ += 1 * KB
    EBS2 = [view(o_ + i * KB, [256]) for i in range(2)]; o_ += 2 * KB
    EMS2 = [view(o_ + i * KB, [256]) for i in range(2)]; o_ += 2 * KB
    EMN2 = [view(o_ + i * KB, [256]) for i in range(2)]; o_ += 2 * KB
    QDH2 = [view(o_ + i * 512, [2, 128], BF16) for i in range(2)]; o_ += 1 * KB
    QM2 = [view(o_ + i * 512, [2, 128], BF16) for i in range(2)]; o_ += 1 * KB
    KM2 = [view(o_ + i * 512, [2, 128], BF16) for i in range(2)]; o_ += 1 * KB
    T1 = view(o_, [128]); o_ += 512
    T2 = view(o_, [128]); o_ += 512
    PTH = view(o_, [128], BF16); o_ += 256
    SF = view(o_, [4, 128]); o_ += 2 * KB
    SB2 = [view(o_ + i * 2 * KB, [4, 128]) for i in range(2)]; o_ += 4 * KB
    SFB = view(o_, [4, 128], BF16); o_ += 1 * KB
    TMPB = view(o_, [4, 128], BF16); o_ += 1 * KB
    ON = view(o_, [512], BF16); o_ += 1 * KB
    SG = view(o_, [4, 128], BF16); o_ += 1 * KB
    AGS = view(116224, [4, 1032])
    AGI_OFF = o_
    AGI = view(o_, [1032]); o_ += 4128 + 32 - 4128 % 32
    DM = view(o_, [8]); o_ += 32
    LTS = view(o_, [8]); o_ += 32
    OSUM = view(o_, [512]); o_ += 2 * KB
    KVM = view(o_, [4, 128]); o_ += 2 * KB
    OG = view(114 * KB, [4, NTOK], BF16)
    assert o_ <= 114 * KB, o_
    R['ags'] = rg('og')
    gts = ["rts", "z", "ll", "eks", "kd", "vt", "ebs", "ems", "emn", "qdh", "qm", "km", "t1", "t2", "pth", "sf", "sbk", "sfb", "tmpb",
           "on", "sg", "ags", "agi", "dm", "kvm", "dec", "decc", "kvb", "kvc", "og", "ltot"]
    psb5 = [0]
    pspool = [[0, 1, 2, 3, 4, 5]]

    def psb():
        pl = pspool[0]
        i = pl[psb5[0] % len(pl)]; psb5[0] += 1
        return PSB[i], PSBr[i]

    def gla_common(hsrc, hr, own_first, own_last, ltot_col=True):
        pv, pvr = psb()
        for k in range(8):
            op("pe", lambda e, k=k: e.matmul(pv[:], lhsT=hsrc[:, k, :], rhs=WV[:, k, :], start=(k == 0), stop=(k == 7)), hr + [rg("wv")], [pvr], inc=(k == 7))
        op("act", lambda e: e.copy(out=VT, in_=pv[:]), [pvr], [rg("vt")])
        pk, pkr = psb()
        for k in range(8):
            op("pe", lambda e, k=k: e.matmul(pk[:, 0:256], lhsT=hsrc[:, k, :], rhs=WK[:, k, :], start=(k == 0), stop=(k == 7)), hr + [rg("wk")], [pkr], inc=(k == 7))
        pr, prr = psb()
        for k in range(8):
            op("pe", lambda e, k=k: e.matmul(pr[0:32, 0:128], lhsT=WR[:, k, :], rhs=hsrc[:, k, :], start=(k == 0), stop=(k == 7)), hr + [rg("wr")], [prr], inc=(k == 7))
        op("act", lambda e: e.copy(out=RTS[0:32, :], in_=pr[0:32, 0:128]), [prr], [rg("rts")])
        op("pe", lambda e: e.matmul(pr[:, :], lhsT=RTS[0:32, :], rhs=WUP[:], start=True, stop=True), [rg("rts"), rg("wup")], [prr])
        op("dve", lambda e: e.tensor_tensor(out=Z, in0=pr[:], in1=BDEC[:], op=ALU.add), [prr, rg("bdec")], [rg("z")])
        op("act", lambda e: e.activation(out=Z, in_=Z, func=AF.Exp, scale=-1.0), [rg("z")], [rg("z")])
        op("act", lambda e: e.activation(out=LL, in_=Z, func=AF.Ln, bias=1.0), [rg("z")], [rg("ll")])
        pe_, per = psb()
        op("pe", lambda e: e.matmul(pe_[:, 0:256], lhsT=Lf, rhs=LL[:, 0:256], start=True, stop=True), [rg("cons"), rg("ll")], [per], inc=False)
        op("pe", lambda e: e.matmul(pe_[:, 256:512], lhsT=Lb, rhs=LL[:, 256:512], start=True, stop=True), [rg("cons"), rg("ll")], [per])
        op("act", lambda e: e.activation(out=EKS, in_=pe_[:], func=AF.Exp), [per], [rg("eks")])
        for d in range(2):
            op("dve", lambda e, d=d: e.tensor_tensor(out=KD[:, d, :], in0=pk[:, 0:256], in1=EKS[:, d * 256:(d + 1) * 256], op=ALU.mult),
               [pkr, rg("eks")], [rg("kd")])

    def kv_mm(h):
        res = []
        for c in range(2):
            pkv, pkvr = psb()
            for d in range(2):
                op("pe", lambda e, c=c, d=d: e.matmul(pkv[0:64, d * 128:(d + 1) * 128],
                                                      lhsT=KD[c * 64:(c + 1) * 64, d, h * 64:(h + 1) * 64],
                                                      rhs=VT[c * 64:(c + 1) * 64, h * 128:(h + 1) * 128], start=True, stop=True),
                   [rg("kd"), rg("vt")], [pkvr], inc=(d == 1))
            res.append((pkv, pkvr))
        return res

    def g1_tile(j, hsrc, hr, DECs, decr, KVs, kvr, ntl, seg_tot):
        gla_common(hsrc, hr, j == 0, j == ntl - 1)
        for h in range(4):
            pd_, pdr = psb()
            for d in range(2):
                op("pe", lambda e, d=d: e.matmul(pd_[0:64, d * 2:(d + 1) * 2], lhsT=LL[:, d * 256 + h * 64:d * 256 + (h + 1) * 64], rhs=BLK,
                                                 start=True, stop=True), [rg("ll"), rg("cons")], [pdr], inc=(d == 1))
            op("act", lambda e, h=h: e.activation(out=DECs[0:64, h, :, 2 * j:2 * j + 2], in_=pd_[0:64, 0:4].rearrange("p (d c) -> p d c", c=2),
                                                  func=AF.Exp), [pdr], [decr])
            if KSUB == 1:
                continue
            if seg_tot:
                pt_, ptr_ = psb()
                for d in range(2):
                    op("pe", lambda e, d=d: e.matmul(pt_[0:64, d:d + 1], lhsT=LL[:, d * 256 + h * 64:d * 256 + (h + 1) * 64], rhs=ONEC,
                                                     start=True, stop=True), [rg("ll"), rg("cons")], [ptr_], inc=(d == 1))
                op("dve", lambda e: e.tensor_tensor(out=LTS[0:64, h * 2:h * 2 + 2], in0=LTS[0:64, h * 2:h * 2 + 2], in1=pt_[0:64, 0:2], op=ALU.add),
                   [ptr_, rg("lts")], [rg("lts")])
            kvs = kv_mm(h)
            if KSUB == 2:
                continue
            for c in range(2):
                pkv, pkvr = kvs[c]
                if KSUB == 3 and c == 1:
                    continue
                op("dve", lambda e, c=c, h=h: e.scalar_tensor_tensor(out=SF[0:64, h, :], in0=SF[0:64, h, :], scalar=DECs[0:64, h, 0, 2 * j + c:2 * j + c + 1],
                                                                     in1=pkv[0:64, 0:128], op0=ALU.mult, op1=ALU.add),
                   [rg("sf"), decr, pkvr], [rg("sf")])
                op("act", lambda e, h=h, c=c: e.copy(out=KVs[0:64, 2 * j + c, h, :], in_=pkv[0:64, 128:256]), [pkvr], [kvr])

    SBr = S.regions("sbk", 2)

    def bwd_scan(nch, DECs, decr, KVs, kvr, store, cur=0):
        for n in range(nch - 1, -1, -1):
            nxt = cur ^ 1
            for h in range(4):
                op("dve", lambda e, h=h, n=n, cur=cur, nxt=nxt: e.scalar_tensor_tensor(out=SB2[nxt][0:64, h, :], in0=SB2[cur][0:64, h, :], scalar=DECs[0:64, h, 1, n:n + 1],
                                                                                       in1=KVs[0:64, n, h, :], op0=ALU.mult, op1=ALU.add),
                   [SBr[cur], decr, kvr], [SBr[nxt]])
            if store:
                op("act", lambda e, n=n, cur=cur: e.copy(out=KVs[0:64, n, :, :], in_=SB2[cur][0:64]), [SBr[cur]], [kvr])
            cur = nxt
        return cur

    CTXH = view(0, [8, 256], BF16)
    CTXH = AGS[:, :, :].bitcast(BF16) if False else view(AGI_OFF, [8, 256], BF16)
    CTXHr = S.regions("ctxh", 2)
    op("dve", lambda e: e.memset(SF[0:64], 0.0), [], [rg("sf")])
    op("dve", lambda e: e.memset(SB2[0][0:64], 0.0), [], [SBr[0]])
    if STOP == "W":
        return finish()
    for t in range(2):
        norm_tile(ctx[t * 128:(t + 1) * 128, :], ACX, BCX, CTXH[:, :, t * 128:(t + 1) * 128], CTXHr[t])
        if STOP == "C1":
            gla_common(CTXH[:, :, t * 128:(t + 1) * 128], [CTXHr[t]], True, True)
            return finish()
        g1_tile(t, CTXH[:, :, t * 128:(t + 1) * 128], [CTXHr[t]], DECC, rg("decc"), KVC, rg("kvc"), 2, False)
        if STOP == "C2":
            return finish()
    cb = bwd_scan(4, DECC, rg("decc"), KVC, rg("kvc"), False, 0)
    op("act", lambda e: e.copy(out=HF[:, 0], in_=SF[0:64]), [rg("sf")], [rg("hf")])
    op("act", lambda e: e.copy(out=HF[:, 1], in_=SB2[cb][0:64]), [SBr[cb]], [rg("hf")])
    if STOP == "C":
        return finish()
    op("dve", lambda e: e.memset(LTS[0:64], 0.0), [], [rg("lts")])
    op("dve", lambda e: e.memset(SF[0:64], 0.0), [rg("sf")], [rg("sf")])
    op("dve", lambda e: e.memset(SB2[0][0:64], 0.0), [SBr[0]], [SBr[0]])
    for j in range(NT):
        g1_tile(j, HT[:, :, j * 128:(j + 1) * 128], [HTr[j]], DEC, rg("dec"), KVB, rg("kvb"), NT, True)
    cb = bwd_scan(32, DEC, rg("dec"), KVB, rg("kvb"), False, 0)
    if STOP == "G1":
        return finish()
    op("act", lambda e: e.copy(out=AGI[0:64, 0:512], in_=SF[0:64].rearrange("p h v -> p (h v)")), [rg("sf")], [rg("agi")])
    op("act", lambda e: e.copy(out=AGI[0:64, 512:1024], in_=SB2[cb][0:64].rearrange("p h v -> p (h v)")), [SBr[cb]], [rg("agi")])
    op("act", lambda e: e.activation(out=AGI[0:64, 1024:1032].rearrange("p (d h) -> p d h", d=2),
                                     in_=LTS[0:64, 0:8].rearrange("p (h d) -> p d h", d=2), func=AF.Exp), [rg("lts")], [rg("agi")])
    load("pool", ag1_in.ap(), AGI[0:64, :], [rg("ag1in")], [rg("agi")])
    ccs = S.dma_stream("cc1")
    S.dma_count[ccs] = 0

    def cc_dma(stream, fn, reads, writes):
        fn = _replay(fn(_REC))
        S._deps("pool", reads, writes)
        S.dma_count[stream] += 1
        tok = (stream, S.dma_count[stream])
        S.items["pool"].append(("op", fn, stream, 1))
        S.clk[tok] = dict(S.seen["pool"])
        S._record(tok, reads, writes)

    cc_dma(ccs, lambda e: e.collective_compute("AllGather", ALU.bypass, replica_groups=GROUPS,
                                               ins=[ag1_in.ap().opt()], outs=[ag1_out.ap().opt()]), [rg("ag1in")], [rg("ag1out")])
    load("pool", AGS[0:64], ag1_out.ap().rearrange("(r p) f -> p r f", p=64), [rg("ags")], [rg("ag1out")])
    op("dve", lambda e: e.tensor_copy(out=SINIT[:], in_=HF[:]), [rg("hf")], [rg("sinit")])
    for d in range(2):
        for r in (range(4) if d == 0 else range(3, -1, -1)):
            mcol = PC[0:64, 4 + d * 4 + r:5 + d * 4 + r]
            op("dve", lambda e, r=r, d=d, mcol=mcol: e.tensor_scalar(out=DM[0:64, 0:4], in0=AGS[0:64, r, 1024 + d * 4:1028 + d * 4], scalar1=-1.0, scalar2=mcol,
                                                                     op0=ALU.add, op1=ALU.mult), [rg("ags"), rg("pc")], [rg("dm")])
            op("dve", lambda e: e.tensor_scalar(out=DM[0:64, 4:8], in0=DM[0:64, 0:4], scalar1=1.0, scalar2=None, op0=ALU.add), [rg("dm")], [rg("dm")])
            op("dve", lambda e, r=r, d=d, mcol=mcol: e.tensor_scalar(out=KVM[0:64].rearrange("p h v -> p (h v)"), in0=AGS[0:64, r, d * 512:(d + 1) * 512],
                                                                     scalar1=mcol, scalar2=None, op0=ALU.mult), [rg("ags"), rg("pc")], [rg("kvm")])
            for h in range(4):
                op("dve", lambda e, h=h, d=d: e.scalar_tensor_tensor(out=SINIT[:, d, h, :], in0=SINIT[:, d, h, :], scalar=DM[0:64, 4 + h:5 + h],
                                                                     in1=KVM[0:64, h, :], op0=ALU.mult, op1=ALU.add), [rg("sinit"), rg("dm"), rg("kvm")], [rg("sinit")])
    op("dve", lambda e: e.tensor_copy(out=SB2[0][0:64], in_=SINIT[:, 1]), [rg("sinit"), SBr[0]], [SBr[0]])
    bwd_scan(32, DEC, rg("dec"), KVB, rg("kvb"), True, 0)
    op("dve", lambda e: e.tensor_copy(out=SF[0:64], in_=SINIT[:, 0]), [rg("sinit"), rg("sf")], [rg("sf")])

    if STOP == "X":
        return finish()
    def g2_tile(j):
        hsrc, hr = HT[:, :, j * 128:(j + 1) * 128], [HTr[j]]
        gla_common(hsrc, hr, False, False)
        pO = [(PSB[3], PSBr[3]), (PSB[4], PSBr[4])]
        pI1, pI1r = PSB[5], PSBr[5]
        def H1(h):
            b_ = h % 2
            EBS, EMS, EMN, QDH, QM, KM = EBS2[b_], EMS2[b_], EMN2[b_], QDH2[b_], QM2[b_], KM2[b_]
            ebr, emr, enr, qdr, qmr, kmr = (rg(f"ebs{b_}"), rg(f"ems{b_}"), rg(f"emn{b_}"), rg(f"qdh{b_}"), rg(f"qm{b_}"), rg(f"km{b_}"))
            pf, pfr = psb()
            for qi, W_ in enumerate((WK, WQ)):
                for k in range(8):
                    op("pe", lambda e, k=k, qi=qi, W_=W_: e.matmul(pf[0:64, qi * 128:(qi + 1) * 128], lhsT=W_[:, k, h * 64:(h + 1) * 64], rhs=hsrc[:, k, :],
                                                                  start=(k == 0), stop=(k == 7)), hr + [rg("wk"), rg("wq")], [pfr], inc=(k == 7 and qi == 1))
            pb, pbr = psb()
            for ci, (d, Cm) in enumerate(((0, Uf), (1, Ub), (0, Mf), (1, Mb))):
                op("pe", lambda e, ci=ci, d=d, Cm=Cm: e.matmul(pb[0:64, ci * 128:(ci + 1) * 128], lhsT=LL[:, d * 256 + h * 64:d * 256 + (h + 1) * 64], rhs=Cm,
                                                               start=True, stop=True), [rg("ll"), rg("cons")], [pbr], inc=(ci == 3))
            op("act", lambda e: e.activation(out=EBS[0:64], in_=pb[0:64, 0:256], func=AF.Exp), [pbr], [ebr])
            op("act", lambda e: e.activation(out=EMS[0:64], in_=pb[0:64, 256:512], func=AF.Exp), [pbr], [emr])
            op("act", lambda e: e.activation(out=EMN[0:64], in_=pb[0:64, 256:512], func=AF.Exp, scale=-1.0), [pbr], [enr])
            for d in range(2):
                op("dve", lambda e, d=d: e.scalar_tensor_tensor(out=QDH[0:64, d, :], in0=pf[0:64, 128:256], scalar=0.125, in1=EBS[0:64, d * 128:(d + 1) * 128],
                                                                op0=ALU.mult, op1=ALU.mult), [pfr, ebr], [qdr])
                op("dve", lambda e, d=d: e.scalar_tensor_tensor(out=QM[0:64, d, :], in0=pf[0:64, 128:256], scalar=0.125, in1=EMS[0:64, d * 128:(d + 1) * 128],
                                                                op0=ALU.mult, op1=ALU.mult), [pfr, emr], [qmr])
                op("dve", lambda e, d=d: e.tensor_tensor(out=KM[0:64, d, :], in0=pf[0:64, 0:128], in1=EMN[0:64, d * 128:(d + 1) * 128], op=ALU.mult),
                   [pfr, enr], [kmr])

        def H2(h):
            b_ = h % 2
            EBS, QDH, QM, KM = EBS2[b_], QDH2[b_], QM2[b_], KM2[b_]
            ebr, qdr, qmr, kmr = rg(f"ebs{b_}"), rg(f"qdh{b_}"), rg(f"qm{b_}"), rg(f"km{b_}")
            psc, pscr = psb()
            for d in range(2):
                op("pe", lambda e, d=d: e.matmul(psc[:, d * 128:(d + 1) * 128], lhsT=KM[0:64, d, :], rhs=QM[0:64, d, :], start=True, stop=True),
                   [kmr, qmr], [pscr], inc=(d == 1))
            op("dve", lambda e: e.tensor_tensor(out=T1, in0=psc[:, 0:128], in1=MKF, op=ALU.mult), [pscr, rg("cons")], [rg("t1")])
            op("dve", lambda e: e.tensor_tensor(out=T2, in0=psc[:, 128:256], in1=MKB, op=ALU.mult), [pscr, rg("cons")], [rg("t2")])
            op("dve", lambda e: e.tensor_tensor(out=PTH, in0=T1, in1=T2, op=ALU.add), [rg("t1"), rg("t2")], [rg("pth")])
            kvs = kv_mm(h)
            for c in range(2):
                po, por = pO[c]
                pkv, pkvr = kvs[c]
                op("act", lambda e, h=h: e.copy(out=SFB[0:64, h, :], in_=SF[0:64, h, :]), [rg("sf")], [rg("sfb")])
                op("pe", lambda e, c=c, h=h, po=po: e.matmul(po[0:64, h * 128:(h + 1) * 128], lhsT=QDH[0:64, 0, c * 64:(c + 1) * 64], rhs=SFB[0:64, h, :],
                                                             start=True, stop=False), [qdr, rg("sfb")], [por], inc=False)
                op("pe", lambda e, c=c, h=h, po=po: e.matmul(po[0:64, h * 128:(h + 1) * 128], lhsT=QDH[0:64, 1, c * 64:(c + 1) * 64], rhs=KVB[0:64, 2 * j + c, h, :],
                                                             start=False, stop=(c == 1)), [qdr, rg("kvb")], [por], inc=(c == 1))
                pin, pinr = (po, por) if c == 0 else (pI1, pI1r)
                op("pe", lambda e, c=c, h=h, pin=pin: e.matmul(pin[0:64, h * 128:(h + 1) * 128], lhsT=PTH[c * 64:(c + 1) * 64, c * 64:(c + 1) * 64],
                                                               rhs=VT[c * 64:(c + 1) * 64, h * 128:(h + 1) * 128], start=(c == 1), stop=True),
                   [rg("pth"), rg("vt")], [pinr], inc=True)
                op("dve", lambda e, c=c, h=h: e.scalar_tensor_tensor(out=SF[0:64, h, :], in0=SF[0:64, h, :], scalar=EBS[0:64, c * 64 + 63:c * 64 + 64],
                                                                     in1=pkv[0:64, 0:128], op0=ALU.mult, op1=ALU.add),
                   [rg("sf"), ebr, pkvr], [rg("sf")])

        H1(0)
        for h in range(4):
            if h < 3:
                H1(h + 1)
            H2(h)
        ptp, ptpr = PTB[0], PTBr[0]
        for c in range(2):
            po, por = pO[c]
            if c == 0:
                op("act", lambda e, po=po: e.copy(out=OSUM[0:64, :], in_=po[0:64, :]), [por], [rg("osum")])
            else:
                op("act", lambda e: e.copy(out=OSUM[0:64, :], in_=pI1[0:64, :]), [pI1r], [rg("osum")])
                op("dve", lambda e, po=po: e.tensor_tensor(out=OSUM[0:64, :], in0=po[0:64, :], in1=OSUM[0:64, :], op=ALU.add), [por, rg("osum")], [rg("osum")])
            po, por = OSUM, rg("osum")
            for h in range(4):
                op("act", lambda e, h=h, po=po: e.activation(out=T1[0:64, :], in_=po[0:64, h * 128:(h + 1) * 128], func=AF.Square,
                                                             accum_out=DM[0:64, h:h + 1]), [por], [rg("t1"), rg("dm")])
            op("act", lambda e: e.activation(out=DM[0:64, 4:8], in_=DM[0:64, 0:4], func=AF.Sqrt, scale=1.0 / 128, bias=EPS), [rg("dm")], [rg("dm")])
            op("dve", lambda e: e.reciprocal(out=DM[0:64, 0:4], in_=DM[0:64, 4:8]), [rg("dm")], [rg("dm")])
            for h in range(4):
                op("dve", lambda e, h=h, po=po: e.tensor_scalar(out=ON[0:64, h * 128:(h + 1) * 128], in0=po[0:64, h * 128:(h + 1) * 128],
                                                                scalar1=DM[0:64, h:h + 1], scalar2=None, op0=ALU.mult), [por, rg("dm")], [rg("on")])
            for h in range(4):
                op("pe", lambda e, h=h, c=c: e.transpose(out=ptp[:, h, c * 64:(c + 1) * 64], in_=ON[0:64, h * 128:(h + 1) * 128], identity=IDB[0:64, 0:64]),
                   [rg("on"), rg("idb")], [ptpr], inc=(h == 3))
        pg, pgr = psb()
        for h in range(4):
            for k in range(8):
                op("pe", lambda e, h=h, k=k: e.matmul(pg[:, h * 128:(h + 1) * 128], lhsT=WG[:, k, h * 128:(h + 1) * 128], rhs=hsrc[:, k, :],
                                                      start=(k == 0), stop=(k == 7)), hr + [rg("wg")], [pgr], inc=(k == 7 and h == 3))
        op("act", lambda e: e.activation(out=SG[:].rearrange("p h t -> p (h t)"), in_=pg[:], func=AF.Silu), [pgr], [rg("sg")])
        op("dve", lambda e: e.scalar_tensor_tensor(out=OG[:, :, j * 128:(j + 1) * 128], in0=ptp[:, 0:4, :], scalar=VEC[:, 20:21], in1=SG[:],
                                                   op0=ALU.mult, op1=ALU.mult), [ptpr, rg("vec"), rg("sg")], [rg("og")])

    PSB.append(PTB[1][:].rearrange("p a b -> p (a b)").bitcast(F32)); PSBr.append(PTBr[1])
    pspool[0] = [0, 1, 2, 6]
    for j in range(NT):
        g2_tile(j)
    pspool[0] = [0, 1, 2, 3, 4, 5]
    S.barrier()

    if STOP == "G2":
        return finish()
    o_ = 14 * KB
    WGP = view(o_, [4, D], BF16); o_ += 8 * KB
    WPP = view(o_, [4, D], BF16); o_ += 8 * KB
    WO = view(o_, [8, D], BF16); o_ += 16 * KB
    WM = view(o_, [8, 2048], BF16); o_ += 32 * KB
    ZT = view(o_, [8, 512], BF16); o_ += 8 * KB
    SGA = view(o_, [512], BF16); o_ += 1 * KB
    SGB = view(o_, [512], BF16); o_ += 1 * KB
    TM1 = view(o_, [512]); o_ += 2 * KB
    TM2 = view(o_, [512]); o_ += 2 * KB
    XMs = [view(o_ + i * 4 * KB, [D]) for i in range(2)]; o_ += 8 * KB
    XRs = XT; XRr = XTr
    H2TK = [view(o_ + i * 2 * KB, [D], BF16) for i in range(2)]; o_ += 4 * KB
    H2TKr = S.regions("h2tk", 2)
    XMr = S.regions("xm", 2)
    EXPT = view(o_, [16]); o_ += 64
    AFTS = view(o_, [NTOK]); o_ += 8 * KB
    assert o_ <= 114 * KB, o_
    load("pool", WGP[:], w_glap.rearrange("(k p) n -> p k n", p=128), [rg("wgp")])
    load("pool", WPP[:], w_poolp.rearrange("(k p) n -> p k n", p=128), [rg("wpp")])
    load("pool", WO[:], w_out.rearrange("(k p) n -> p k n", p=128), [rg("wo")])
    wload(WM[:], C_M, 2048, rg("wm"))
    for bk in range(4):
        hsrc, hr = HT[:, :, bk * 512:(bk + 1) * 512], HTr[bk * 4:(bk + 1) * 4]
        for dc in range(8):
            pbg, pbgr = psb(); pbp, pbpr = psb(); pmg, pmgr = psb(); pmp, pmpr = psb()
            for h in range(4):
                op("pe", lambda e, h=h, dc=dc: e.matmul(pbg[:], lhsT=WGP[:, h, dc * 128:(dc + 1) * 128], rhs=OG[:, h, bk * 512:(bk + 1) * 512],
                                                        start=(h == 0), stop=(h == 3)), [rg("wgp"), rg("og")], [pbgr], inc=(h == 3))
            for g in range(4):
                op("pe", lambda e, g=g, dc=dc: e.matmul(pbp[:], lhsT=WPP[:, g, dc * 128:(dc + 1) * 128], rhs=MIXT[:, g, bk * 512:(bk + 1) * 512],
                                                        start=(g == 0), stop=(g == 3)), [rg("wpp"), rg("mixt")], [pbpr], inc=(g == 3))
            for k in range(8):
                op("pe", lambda e, k=k, dc=dc: e.matmul(pmg[:], lhsT=WM[:, k, dc * 128:(dc + 1) * 128], rhs=hsrc[:, k, :], start=(k == 0), stop=(k == 7)),
                   hr + [rg("wm")], [pmgr], inc=(k == 7))
            for k in range(8):
                op("pe", lambda e, k=k, dc=dc: e.matmul(pmp[:], lhsT=WM[:, k, 1024 + dc * 128:1024 + (dc + 1) * 128], rhs=hsrc[:, k, :], start=(k == 0), stop=(k == 7)),
                   hr + [rg("wm")], [pmpr], inc=(k == 7))
            op("act", lambda e: e.activation(out=SGA, in_=pmg[:], func=AF.Sigmoid), [pmgr], [rg("sga")])
            op("act", lambda e: e.activation(out=SGB, in_=pmp[:], func=AF.Sigmoid), [pmpr], [rg("sgb")])
            op("dve", lambda e: e.tensor_tensor(out=TM1, in0=pbg[:], in1=SGA, op=ALU.mult), [pbgr, rg("sga")], [rg("tm1")])
            op("dve", lambda e: e.tensor_tensor(out=TM2, in0=pbp[:], in1=SGB, op=ALU.mult), [pbpr, rg("sgb")], [rg("tm2")])
            op("dve", lambda e, dc=dc: e.tensor_tensor(out=ZT[:, dc, :], in0=TM1, in1=TM2, op=ALU.add), [rg("tm1"), rg("tm2")], [rg("zt")])
        def y_stage(tt):
            j = bk * 4 + tt
            b_ = j % 2
            load("sp", XRs[b_], xext[512 + j * 128:512 + (j + 1) * 128, :], [XRr[b_]])
            for half in range(2):
                py, pyr = psb()
                for dc in range(8):
                    op("pe", lambda e, dc=dc, half=half, tt=tt: e.matmul(py[:], lhsT=ZT[:, dc, tt * 128:(tt + 1) * 128], rhs=WO[:, dc, half * 512:(half + 1) * 512],
                                                                        start=(dc == 0), stop=(dc == 7)), [rg("zt"), rg("wo")], [pyr], inc=(dc == 7))
                op("dve", lambda e, half=half: e.tensor_tensor(out=XMs[b_][:, half * 512:(half + 1) * 512], in0=py[:], in1=GT1[:, half * 512:(half + 1) * 512], op=ALU.mult),
                   [pyr, rg("gt1")], [XMr[b_]])
            op("dve", lambda e: e.tensor_tensor(out=XMs[b_], in0=XMs[b_], in1=XRs[b_], op=ALU.add), [XMr[b_], XRr[b_]], [XMr[b_]])
            load("sp", xmid[j * 128:(j + 1) * 128, :], XMs[b_], [rg("xmid")], [XMr[b_]])

        def n_stage(tt):
            j = bk * 4 + tt
            b_ = j % 2
            norm_tile(None, A2, B2, HT[:, :, j * 128:(j + 1) * 128], HTr[j], xt_pre=(XMs[b_], XMr[b_]))
            pt2, pt2r = ptb()
            for k in range(8):
                op("pe", lambda e, k=k, j=j: e.transpose(out=pt2[:, k, :], in_=HT[:, k, j * 128:(j + 1) * 128], identity=IDB[:]),
                   [HTr[j], rg("idb")], [pt2r], inc=(k == 7))
            op("act", lambda e: e.copy(out=H2TK[b_].rearrange("p (k d) -> p k d", k=8), in_=pt2[:]), [pt2r], [H2TKr[b_]])
            load("sp", h2rows[j * 128:(j + 1) * 128, :], H2TK[b_], [rg("h2rows")], [H2TKr[b_]])
            pl, plr = psb()
            for k in range(8):
                op("pe", lambda e, k=k, j=j: e.matmul(pl[:, 0:16], lhsT=HT[:, k, j * 128:(j + 1) * 128], rhs=WRT[:, k, :], start=(k == 0), stop=(k == 7)),
                   [HTr[j], rg("wrt")], [plr], inc=(k == 7))
            mx, mxr = small(); sm, smr = small(); rsm, rsmr = small()
            op("dve", lambda e: e.reduce_max(out=mx, in_=pl[:, 0:16], axis=AX.X), [plr], [mxr])
            op("dve", lambda e: e.tensor_scalar(out=mx, in0=mx, scalar1=-1.0, scalar2=None, op0=ALU.mult), [mxr], [mxr])
            op("act", lambda e: e.activation(out=EXPT, in_=pl[:, 0:16], func=AF.Exp, bias=mx, accum_out=sm), [plr, mxr], [rg("expt"), smr])
            op("dve", lambda e: e.reciprocal(out=rsm, in_=sm), [smr], [rsmr])
            op("dve", lambda e, j=j: e.tensor_scalar(out=AFF[:, j, :], in0=EXPT, scalar1=rsm, scalar2=None, op0=ALU.mult), [rg("expt"), rsmr], [rg("aff")])
            pa, par = psb()
            op("pe", lambda e, j=j: e.transpose(out=pa[0:16, 0:128], in_=AFF[:, j, :], identity=IDF[:]), [rg("aff"), rg("idf")], [par])
            op("act", lambda e, j=j: e.copy(out=AFTS[0:16, j * 128:(j + 1) * 128], in_=pa[0:16, 0:128]), [par], [rg("afts")])

        y_stage(0)
        for tt in range(4):
            if tt + 1 < 4:
                y_stage(tt + 1)
            n_stage(tt)
    load("pool", ag2_in.ap(), AFTS[0:16, :], [rg("ag2in")], [rg("afts")])
    cc2 = S.dma_stream("cc2")
    cc_dma(cc2, lambda e: e.collective_compute("AllGather", ALU.bypass, replica_groups=GROUPS,
                                               ins=[ag2_in.ap().opt()], outs=[ag2_out.ap().opt()]), [rg("ag2in")], [rg("ag2out")])
    S.barrier()
    AG = view(14 * KB, [NTOK]); CMP = view(22 * KB, [NTOK]); o_ = 30 * KB
    WGE = view(96 * KB, [8, D], BF16); WUE = view(112 * KB, [8, D], BF16); WDE = view(128 * KB, [8, D], BF16)
    wst = [S.dma_stream(f"we{i}") for i in range(3)]

    wst += [S.dma_stream(f"we{i}") for i in range(3, 5)]

    def load_expert(ex):
        load_gu(ex); load_d(ex)

    def load_d(ex):
        S.dma("pool", wst[4], lambda e: e.dma_start(out=WDE[:], in_=w_down[ex].rearrange("(k p) n -> p k n", p=128)), writes=[rg("wde")])

    def load_gu(ex):
        for hf in range(2):
            cs = slice(hf * 512, (hf + 1) * 512)
            S.dma("pool", wst[hf * 2], lambda e, cs=cs: e.dma_start(out=WGE[:, :, cs], in_=w_gate[ex][:, cs].rearrange("(k p) n -> p k n", p=128)), writes=[rg(f"wge{hf}")])
            S.dma("pool", wst[hf * 2 + 1], lambda e, cs=cs: e.dma_start(out=WUE[:, :, cs], in_=w_up[ex][:, cs].rearrange("(k p) n -> p k n", p=128)), writes=[rg(f"wue{hf}")])
    LO = view(o_, [4]); o_ += 32
    load("pool", AG[0:64, :], ag2_out.ap(), [rg("ag")], [rg("ag2out")])
    load_expert(0)
    op("dve", lambda e: e.memset(LO[0:64, 0:1], 0.0), [], [rg("lo")])
    for it in range(27):
        wdt = 2.0 ** -(it + 1)
        op("dve", lambda e, wdt=wdt: e.tensor_scalar(out=LO[0:64, 1:2], in0=LO[0:64, 0:1], scalar1=wdt, scalar2=None, op0=ALU.add), [rg("lo")], [rg("lo")])
        op("dve", lambda e: e.tensor_scalar(out=CMP[0:64, :], in0=AG[0:64, :], scalar1=LO[0:64, 1:2], scalar2=None, op0=ALU.is_gt), [rg("ag"), rg("lo")], [rg("cmp")])
        op("dve", lambda e: e.reduce_sum(out=LO[0:64, 2:3], in_=CMP[0:64, :], axis=AX.X), [rg("cmp")], [rg("lo")])
        pc_, pcr = psb()
        op("pe", lambda e, pc_=pc_: e.matmul(pc_[0:64, 0:1], lhsT=GM[:], rhs=LO[0:64, 2:3], start=True, stop=True), [rg("gm"), rg("lo")], [pcr])
        op("dve", lambda e, pc_=pc_: e.tensor_scalar(out=LO[0:64, 3:4], in0=pc_[0:64, 0:1], scalar1=CAP - 0.5, scalar2=None, op0=ALU.is_ge), [pcr], [rg("lo")])
        op("dve", lambda e, wdt=wdt: e.scalar_tensor_tensor(out=LO[0:64, 0:1], in0=LO[0:64, 3:4], scalar=wdt, in1=LO[0:64, 0:1], op0=ALU.mult, op1=ALU.add),
           [rg("lo")], [rg("lo")])
    op("dve", lambda e: e.scalar_tensor_tensor(out=CMP[0:64, :], in0=AG[0:64, :], scalar=LO[0:64, 0:1], in1=AG[0:64, :], op0=ALU.is_gt, op1=ALU.mult),
       [rg("ag"), rg("lo")], [rg("cmp")])
    for j in range(NT):
        pw_, pwr = psb()
        op("pe", lambda e, j=j, pw_=pw_: e.matmul(pw_[:, 0:16], lhsT=CMP[0:64, j * 128:(j + 1) * 128], rhs=SEL[:], start=True, stop=True), [rg("cmp"), rg("sel")], [pwr])
        op("act", lambda e, j=j, pw_=pw_: e.copy(out=WGT[:, j, :], in_=pw_[:, 0:16]), [pwr], [rg("wgt")])
    o_ = 0
    C2 = view(o_, [2, 128]); o_ += 1 * KB
    MK = view(o_, [256]); o_ += 1 * KB
    CSM = view(o_, [NT, 16]); o_ += 1 * KB
    SLF = view(o_, [256]); o_ += 1 * KB
    SLI = ARENA[:, 94 * KB:95 * KB].bitcast(U32)
    RST = [view(64 * KB + (4 + i) * 2 * KB, [D], BF16) for i in range(3)]
    RSTr = S.regions("rst", 3)
    rs_cnt = [0]
    load("sp", C2[:], consts2.rearrange("p (a b) -> p a b", b=128), [rg("c2")])
    op("dve", lambda e: e.tensor_scalar(out=MK, in0=WGT[:].rearrange("p j e -> p (j e)"), scalar1=0.0, scalar2=None, op0=ALU.is_gt), [rg("wgt")], [rg("mk")])
    op("dve", lambda e: e.memset(CSM[:, 0, :], 0.0), [], [rg("csm")])
    for j in range(1, NT):
        op("dve", lambda e, j=j: e.tensor_tensor(out=CSM[:, j, :], in0=CSM[:, j - 1, :], in1=MK[:, (j - 1) * 16:j * 16], op=ALU.add), [rg("csm"), rg("mk")], [rg("csm")])
    prk, prkr = psb()
    op("pe", lambda e: e.matmul(prk[:, 0:256], lhsT=C2[:, 0, :], rhs=MK, start=True, stop=False), [rg("c2"), rg("mk")], [prkr], inc=False)
    op("pe", lambda e: e.matmul(prk[:, 0:256], lhsT=C2[:, 1, :], rhs=CSM[:].rearrange("p j e -> p (j e)"), start=False, stop=True), [rg("c2"), rg("csm")], [prkr])
    op("dve", lambda e: e.scalar_tensor_tensor(out=SLF, in0=prk[:, 0:256], scalar=-4096.0, in1=MK, op0=ALU.add, op1=ALU.mult), [prkr, rg("mk")], [rg("slf")])
    op("dve", lambda e: e.tensor_scalar(out=SLI, in0=SLF, scalar1=4096.0, scalar2=None, op0=ALU.add), [rg("slf")], [rg("sli")])
    YSELr = S.regions("ysel", NE)

    def pdma(fn, reads, writes):
        st = lstreams["pool"][lidx["pool"] % len(lstreams["pool"])]; lidx["pool"] += 1
        return S.dma("pool", st, fn, reads=list(reads), writes=list(writes))

    SXr = [S.regions(f"sx{ex}_", NT) for ex in range(NE)]

    def scatter_expert(ex):
        for j in range(NT):
            i = rs_cnt[0] % 3; rs_cnt[0] += 1
            load("sp", RST[i], h2rows[j * 128:(j + 1) * 128, :], [RSTr[i]], [rg("h2rows")])
            pdma(lambda e, j=j, i=i: e.indirect_dma_start(
                out=xsel[ex], out_offset=bass.IndirectOffsetOnAxis(SLI[:, j * 16 + ex:j * 16 + ex + 1], 0),
                in_=RST[i], in_offset=None, bounds_check=CAP - 1, oob_is_err=False), [RSTr[i], rg("sli"), XSELr[ex]], [SXr[ex][j]])

    scatter_expert(0)
    scatter_expert(1)

    if STOP == "R":
        return finish()
    XACC = view(0, [NT, D]); o_ = 64 * KB
    NXS = 4
    XSE = [view(o_ + i * 2 * KB, [D], BF16) for i in range(NXS)]; o_ += 7 * 2 * KB
    YS = [view(o_ + i * 4 * KB, [D]) for i in range(2)]; o_ += 8 * KB
    GTL = [view(o_ + i * 4 * KB, [D]) for i in range(2)]; o_ += 8 * KB
    assert o_ <= 94 * KB
    o_ = 144 * KB
    SGT = [view(o_ + i * KB, [512], BF16) for i in range(2)]; o_ += 2 * KB
    assert o_ <= ARENA_BYTES, o_
    SGTr = S.regions("sgt", 2); XSEr = S.regions("xse", NXS); YSr = S.regions("ys", 2); GTLr = S.regions("gtl", 2)
    XST = HT[:, :, 0:CAP]; HTE = HT[:, :, CAP:2 * CAP]
    XSTr = S.regions("xst", 2); HTEr = S.regions("hte", 2)
    XAr = S.regions("xacc", NT)
    for i in range(2):
        op("dve", lambda e, i=i: e.memset(GTL[i], 0.0), [], [GTLr[i]])
    xs_cnt = [0]

    def prep_expert(ex):
        for st in range(8):
            i = xs_cnt[0] % NXS; xs_cnt[0] += 1
            extra = []
            S.dma("sp", lstreams["sp"][lidx["sp"] % 24], lambda e, i=i, st=st: e.dma_start(out=XSE[i], in_=xsel[ex][st * 128:(st + 1) * 128, :]),
                  reads=list(SXr[ex]), writes=[XSEr[i]] + extra)
            lidx["sp"] += 1
            pt3, pt3r = ptb()
            for k in range(8):
                op("pe", lambda e, k=k, i=i: e.transpose(out=pt3[:, k, :], in_=XSE[i][:, k * 128:(k + 1) * 128], identity=IDB[:]),
                   [XSEr[i], rg("idb")], [pt3r], inc=(k == 7))
            if st % 2 == 0:
                op("act", lambda e, st=st: e.copy(out=XST[:, :, st * 128:(st + 1) * 128], in_=pt3[:]), [pt3r], [XSTr[st // 4]])
            else:
                op("dve", lambda e, st=st: e.tensor_copy(out=XST[:, :, st * 128:(st + 1) * 128], in_=pt3[:]), [pt3r], [XSTr[st // 4]])

    def combine(ex):
        for j in range(NT):
            i = j % 2
            pdma(lambda e, j=j, i=i: e.indirect_dma_start(
                out=GTL[i], out_offset=None, in_=ysel[ex],
                in_offset=bass.IndirectOffsetOnAxis(SLI[:, j * 16 + ex:j * 16 + ex + 1], 0), bounds_check=CAP - 1, oob_is_err=False),
                [YSELr[ex], rg("sli")], [GTLr[i]])
            if ex == 0:
                op("dve", lambda e, j=j, i=i: e.tensor_scalar(out=XACC[:, j, :], in0=GTL[i], scalar1=WGT[:, j, ex:ex + 1], scalar2=None, op0=ALU.mult),
                   [GTLr[i], rg("wgt")], [XAr[j]])
            else:
                op("dve", lambda e, j=j, i=i: e.scalar_tensor_tensor(out=XACC[:, j, :], in0=GTL[i], scalar=WGT[:, j, ex:ex + 1], in1=XACC[:, j, :],
                                                                     op0=ALU.mult, op1=ALU.add), [GTLr[i], rg("wgt"), XAr[j]], [XAr[j]])

    for ex in range(NE):
        if ex + 2 < NE:
            scatter_expert(ex + 2)
        if ex == 0:
            prep_expert(0)
        for fc in range(8):
            for bk in range(2):
                hf = fc // 4
                pg_, pgr_ = psb(); pu_, pur_ = psb()
                for k in range(8):
                    op("pe", lambda e, k=k, fc=fc, pg_=pg_: e.matmul(pg_[:], lhsT=WGE[:, k, fc * 128:(fc + 1) * 128], rhs=XST[:, k, bk * 512:(bk + 1) * 512], start=(k == 0), stop=(k == 7)),
                       [XSTr[bk], rg(f"wge{hf}")], [pgr_], inc=(k == 7))
                for k in range(8):
                    op("pe", lambda e, k=k, fc=fc, pu_=pu_: e.matmul(pu_[:], lhsT=WUE[:, k, fc * 128:(fc + 1) * 128], rhs=XST[:, k, bk * 512:(bk + 1) * 512], start=(k == 0), stop=(k == 7)),
                       [XSTr[bk], rg(f"wue{hf}")], [pur_], inc=(k == 7))
                si = (bk * 8 + fc) % 2
                op("act", lambda e, pg_=pg_, si=si: e.activation(out=SGT[si], in_=pg_[:], func=AF.Silu), [pgr_], [SGTr[si]])
                op("dve", lambda e, pu_=pu_, si=si, fc=fc, bk=bk: e.tensor_tensor(out=HTE[:, fc, bk * 512:(bk + 1) * 512], in0=pu_[:], in1=SGT[si], op=ALU.mult),
                   [pur_, SGTr[si]], [HTEr[bk]])
        if ex + 1 < NE:
            load_gu(ex + 1)
            prep_expert(ex + 1)
        for st in range(8):
            i = st % 2
            for half in range(2):
                py, pyr = psb()
                for fc in range(8):
                    op("pe", lambda e, fc=fc, st=st, half=half, py=py: e.matmul(py[:], lhsT=HTE[:, fc, st * 128:(st + 1) * 128], rhs=WDE[:, fc, half * 512:(half + 1) * 512],
                                                                              start=(fc == 0), stop=(fc == 7)), [HTEr[st // 4], rg("wde")], [pyr], inc=(fc == 7))
                op("act", lambda e, i=i, half=half, py=py: e.copy(out=YS[i][:, half * 512:(half + 1) * 512], in_=py[:]), [pyr], [YSr[i]])
            load("sp", ysel[ex][st * 128:(st + 1) * 128, :], YS[i], [YSELr[ex]], [YSr[i]])
        if ex > 0:
            combine(ex - 1)
        if ex + 1 < NE:
            load_d(ex + 1)
    S.barrier()
    if STOP == "E":
        return finish()
    FGT = view(96 * KB, [D]); XF = [view(100 * KB + i * 4 * KB, [D]) for i in range(4)]; OT = [view(116 * KB + i * 4 * KB, [D]) for i in range(4)]
    JK = view(132 * KB, [D])
    XFr = S.regions("xf", 4); OTr = S.regions("ot", 4)
    load("sp", FGT, fin_g.partition_broadcast(128), [rg("fgt")])
    ost = [S.dma_stream(f"o{i}") for i in range(4)]
    for j in range(NT):
        load("sp", XF[j % 4], xmid[j * 128:(j + 1) * 128, :], [XFr[j % 4]], [rg("xmid")]) if j < 4 else None
    ex = NE - 1
    for j in range(NT):
        i = j % 4
        g_ = j % 2
        pdma(lambda e, j=j, g_=g_: e.indirect_dma_start(
            out=GTL[g_], out_offset=None, in_=ysel[ex],
            in_offset=bass.IndirectOffsetOnAxis(SLI[:, j * 16 + ex:j * 16 + ex + 1], 0), bounds_check=CAP - 1, oob_is_err=False),
            [YSELr[ex], rg("sli")], [GTLr[g_]])
        op("dve", lambda e, j=j, g_=g_: e.scalar_tensor_tensor(out=XACC[:, j, :], in0=GTL[g_], scalar=WGT[:, j, ex:ex + 1], in1=XACC[:, j, :],
                                                               op0=ALU.mult, op1=ALU.add), [GTLr[g_], rg("wgt"), XAr[j]], [XAr[j]])
        if j >= 4:
            load("sp", XF[i], xmid[j * 128:(j + 1) * 128, :], [XFr[i]], [rg("xmid")])
        op("dve", lambda e, j=j: e.tensor_tensor(out=XACC[:, j, :], in0=XACC[:, j, :], in1=GT2[:], op=ALU.mult), [XAr[j], rg("gt2")], [XAr[j]])
        op("dve", lambda e, j=j, i=i: e.tensor_tensor(out=XF[i], in0=XF[i], in1=XACC[:, j, :], op=ALU.add), [XAr[j], XFr[i]], [XFr[i]])
        ssq, ssqr = small(); sd, sdr = small(); rs, rsr = small()
        op("act", lambda e, i=i: e.activation(out=JK, in_=XF[i], func=AF.Square, accum_out=ssq), [XFr[i]], [rg("jk"), ssqr])
        op("act", lambda e: e.activation(out=sd, in_=ssq, func=AF.Sqrt, scale=1.0 / D, bias=EPS), [ssqr], [sdr])
        op("dve", lambda e: e.reciprocal(out=rs, in_=sd), [sdr], [rsr])
        op("dve", lambda e, i=i: e.scalar_tensor_tensor(out=OT[i], in0=XF[i], scalar=rs, in1=FGT, op0=ALU.mult, op1=ALU.mult), [XFr[i], rsr, rg("fgt")], [OTr[i]])
        S.dma("sp", ost[i], lambda e, j=j, i=i: e.dma_start(out=out[j * 128:(j + 1) * 128, :], in_=OT[i]), reads=[OTr[i]], writes=[rg("outd")])
    S.wait_all("sp", [(o_s, S.dma_count[o_s]) for o_s in ost])
    S.emit()
    return nc


_NC = [None]


def _NEW():
    import os
    return 1 if os.environ.get("KSTOP", "") else NE


def _consts():
    j = np.arange(128)[:, None]; i = np.arange(128)[None, :]
    same = (j // 64) == (i // 64)
    lj = j % 64
    sc = -1.0 / 16.0
    C = np.zeros((128, 9, 128), np.float32)
    C[:, 0] = sc * (same & (j <= i)); C[:, 1] = sc * (same & (j >= i))
    C[:, 2] = sc * (same & (j > i)); C[:, 3] = sc * (same & (j < i))
    C[:, 4] = sc * same * ((j <= i).astype(np.float32) - (lj <= 31).astype(np.float32))
    C[:, 5] = sc * same * ((j >= i).astype(np.float32) - (lj >= 32).astype(np.float32))
    C[:, 6] = (same & (i >= j)); C[:, 7] = (same & (j >= i))
    C[:, 8, 0] = sc * (np.arange(128) // 64 == 0); C[:, 8, 1] = sc * (np.arange(128) // 64 == 1); C[:, 8, 2] = sc
    return C.reshape(128, 9 * 128)


def kernel(x, c, ctx, c_ctx, ada_w, ada_b, norm1_g, norm2_g, w_in, w_decay_up, b_decay,
           gla_norm_g, w_gla_proj, pool_w, pool_scale, w_pool_proj, w_out, w_router,
           w_gate_e, w_up_e, w_down_e, final_norm_g):
    f = lambda a: np.ascontiguousarray(np.asarray(a, dtype=np.float32))
    x = f(x); c = f(c); ctx = f(ctx); c_ctx = f(c_ctx)
    fm = lambda v: f(v).reshape(-1, 128).T
    if _NC[0] is None:
        _NC[0] = build_program()
    nc = _NC[0]
    vecs = np.zeros((128, 32), np.float32)
    vecs[:, 0:8] = fm(norm1_g[0]); vecs[:, 8:16] = fm(norm2_g[0]); vecs[:, 16:20] = fm(pool_scale[0]); vecs[:, 20] = f(gla_norm_g[0])
    w_upP = np.zeros((32, 512), np.float32)
    w_upP[0:16, 0:256] = f(w_decay_up[0, 0]); w_upP[16:32, 256:512] = f(w_decay_up[0, 1])
    consts = _consts()
    pp = np.arange(128)
    consts2 = np.concatenate([(pp[:, None] < pp[None, :]).astype(np.float32), np.ones((128, 128), np.float32)], axis=1)
    gmat = np.zeros((64, 64), np.float32)
    for r in range(4):
        for r2 in range(4):
            gmat[r * 16:(r + 1) * 16, r2 * 16:(r2 + 1) * 16] = np.eye(16, dtype=np.float32)
    shared = {
        "ada_w": f(ada_w[0]), "ada_bT": fm(ada_b[0]), "ada_b": f(ada_b[0]).reshape(1, -1), "vecs": vecs,
        "b_decay": f(b_decay[0]).reshape(1, 512), "fin_g": f(final_norm_g).reshape(1, D), "w_in": f(w_in[0]), "w_upP": w_upP,
        "w_glap": f(w_gla_proj[0]), "pool_w": f(pool_w[0]), "w_poolp": f(w_pool_proj[0]), "w_out": f(w_out[0]), "w_router": f(w_router[0]),
        "w_gate": f(w_gate_e[0])[:_NEW()], "w_up": f(w_up_e[0])[:_NEW()], "w_down": f(w_down_e[0])[:_NEW()], "consts": consts, "consts2": consts2, "gmat": gmat,
        "pmask": np.zeros((1, NEXT), np.float32),
    }
    in_maps = []
    for core in range(8):
        b, s_ = core // 4, core % 4
        t0 = 2048 * s_
        xe = np.zeros((NEXT, D), np.float32)
        lo, hi = max(t0 - 512, 0), min(t0 + 2560, 8192)
        xe[lo - (t0 - 512):hi - (t0 - 512)] = x[b, lo:hi]
        cv = np.zeros((128, 16), np.float32)
        cv[:, 0:8] = fm(c[b]); cv[:, 8:16] = fm(c_ctx)
        pcz = np.zeros((128, 16), np.float32)
        pcz[:, 2] = 1.0 if s_ > 0 else 0.0; pcz[:, 3] = 1.0 if s_ < 3 else 0.0; pcz[:, 12] = 1.0
        for r in range(4):
            pcz[:, 4 + r] = 1.0 if r < s_ else 0.0
            pcz[:, 8 + r] = 1.0 if r > s_ else 0.0
        inv = np.zeros((4, 32, 64), np.float32)
        for g, w in enumerate(WINS):
            rows = 32 * s_ + np.arange(32)
            cr = np.minimum(rows + w // 2, 128) - np.maximum(rows - w // 2, 0)
            cols = np.arange(64)
            cc_ = np.minimum(cols + w // 2, 64) - np.maximum(cols - w // 2, 0)
            inv[g] = 1.0 / (cr[:, None] * cc_[None, :]).astype(np.float32)
        selm = np.zeros((64, 16), np.float32)
        selm[s_ * 16:(s_ + 1) * 16] = np.eye(16, dtype=np.float32)
        m = dict(shared)
        m.update({"xext": xe, "ctx": f(ctx[b]), "cvec": cv, "percore": pcz, "invcnt": inv.reshape(1, -1), "sel": selm})
        in_maps.append(m)
    res = run_bass_kernel_spmd(nc, in_maps, core_ids=list(range(8)))
    outp = np.zeros((2, 8192, D), np.float32)
    for core in range(8):
        b, s_ = core // 4, core % 4
        outp[b, 2048 * s_:2048 * (s_ + 1)] = res.results[core]["out"]
    return outp
```

```python
import numpy as np
import concourse.bass as bass
import concourse.mybir as mybir
from concourse.bass_utils import run_bass_kernel_spmd

F32 = mybir.dt.float32
BF16 = mybir.dt.bfloat16
AF = mybir.ActivationFunctionType
ALU = mybir.AluOpType
AX = mybir.AxisListType

ENGS = ("pe", "act", "dve", "pool", "sp")


class Region:
    __slots__ = ("name", "last_write", "readers", "excl")

    def __init__(self, name, excl=False):
        self.name = name
        self.last_write = None
        self.readers = []
        self.excl = excl


class _Rec:
    def __getattr__(self, name):
        return lambda *a, **k: (name, a, k)


_REC = _Rec()


_BCREG = {}


def _replay(rec):
    name, a, k = rec
    if name != "indirect_dma_start":
        return lambda engine: getattr(engine, name)(*a, **k)

    def f(engine):
        key = id(engine)
        if key not in _BCREG:
            _BCREG[key] = engine.to_reg(k["bounds_check"])
        return getattr(engine, name)(*a, **dict(k, bounds_check=_BCREG[key]))
    return f


class Sched:
    def __init__(self, nc):
        self.nc = nc
        self.eng = {"pe": nc.tensor, "act": nc.scalar, "dve": nc.vector,
                    "pool": nc.gpsimd, "sp": nc.sync}
        self.items = {e: [] for e in ENGS}
        self.count = {e: 0 for e in ENGS}
        self.pending = {e: False for e in ENGS}
        self.seen = {e: {} for e in ENGS}
        self.clk = {}
        self.nsem = 0
        self.dma_count = {}
        self.streams = list(ENGS)

    def region(self, name):
        return Region(name)

    def regions(self, name, n, excl=False):
        return [Region(f"{name}{i}", excl) for i in range(n)]

    def dma_stream(self, name):
        s = f"dma:{name}:{len(self.streams)}"
        self.streams.append(s)
        self.dma_count[s] = 0
        return s

    def _need(self, e, dep):
        st, val = dep
        if self.seen[e].get(st, 0) >= val:
            return
        if st == e and val > self.count[e]:
            return
        self.items[e].append(("wait", st, val))
        ck = self.clk.get(dep)
        se = self.seen[e]
        if ck:
            for k, v in ck.items():
                if se.get(k, 0) < v:
                    se[k] = v
        if se.get(st, 0) < val:
            se[st] = val

    def _deps(self, e, reads, writes):
        for r in reads:
            if r.last_write is not None:
                self._need(e, r.last_write)
            if r.excl:
                for rd in r.readers:
                    if rd[0] != e:
                        self._need(e, rd)
        for w in writes:
            if w.last_write is not None:
                self._need(e, w.last_write)
            for rd in w.readers:
                self._need(e, rd)

    def _record(self, tok, reads, writes):
        for r in reads:
            r.readers.append(tok)
        for w in writes:
            w.last_write = tok
            w.readers = []

    def op(self, e, fn, reads=(), writes=(), inc=True):
        fn = _replay(fn(_REC))
        self._deps(e, reads, writes)
        val = self.count[e] + 1
        tok = (e, val)
        if inc:
            self.count[e] = val
            self.items[e].append(("op", fn, e, 1))
            ck = dict(self.seen[e])
            ck[e] = val
            self.clk[tok] = ck
            self.pending[e] = False
        else:
            self.items[e].append(("op", fn, None, 0))
            self.pending[e] = True
        self._record(tok, reads, writes)
        return tok

    def dma(self, q, stream, fn, reads=(), writes=()):
        fn = _replay(fn(_REC))
        self._deps(q, reads, writes)
        if self.dma_count[stream] > 0:
            self._need(q, (stream, self.dma_count[stream]))
        self.dma_count[stream] += 16
        val = self.dma_count[stream]
        tok = (stream, val)
        self.items[q].append(("op", fn, stream, 16))
        self.clk[tok] = dict(self.seen[q])
        self._record(tok, reads, writes)
        return tok

    def wait_all(self, e, toks):
        for t in toks:
            self._need(e, t)

    def emit(self):
        nc = self.nc
        for e in ENGS:
            assert not self.pending[e], f"engine {e} ends with a non-inc op"
        sems = {}
        import contextlib
        with contextlib.ExitStack() as es:
            for st in self.streams:
                nm = st.replace(":", "_")
                sems[st] = es.enter_context(nc.semaphore("s_" + nm))
            block = es.enter_context(nc.Block())

            def run(e, engine):
                for it in self.items[e]:
                    if it[0] == "wait":
                        engine.wait_ge(sems[it[1]], it[2])
                    else:
                        _, fn, st, iv = it
                        ins = fn(engine)
                        if st is not None:
                            ins.then_inc(sems[st], iv)

            @block.tensor
            def _(t):
                run("pe", t)

            @block.scalar
            def _(t):
                run("act", t)

            @block.vector
            def _(t):
                run("dve", t)

            @block.gpsimd
            def _(t):
                run("pool", t)

            @block.sync
            def _(t):
                run("sp", t)

    def barrier(self):
        toks = [(e, self.count[e]) for e in ENGS if self.count[e] > 0]
        toks += [(s, v) for s, v in self.dma_count.items() if v > 0]
        for e in ENGS:
            for t in toks:
                if t[0] != e:
                    self._need(e, t)


D = 1024
NTOK = 2048
NEXT = 3072
NT = 16
CH = 64
WINS = (2, 4, 8, 16)
NE = 16
CAP = 1024
EPS = 1e-6
U8 = mybir.dt.uint8
C_K, C_V, C_R, C_Q, C_G, C_P, C_M = 0, 256, 768, 800, 1056, 1568, 2080


def build_program():
    nc = bass.Bass("TRN2", target_bir_lowering=False)
    S = Sched(nc)

    def din(name, shape):
        return nc.dram_tensor(name, list(shape), F32, kind="ExternalInput").ap()

    xext = din("xext", [NEXT, D]); ctx = din("ctx", [256, D]); cvec = din("cvec", [128, 16])
    ada_w = din("ada_w", [D, 6 * D]); ada_bT = din("ada_bT", [128, 48]); ada_b = din("ada_b", [1, 6 * D])
    vecs = din("vecs", [128, 32]); b_decay = din("b_decay", [1, 512]); fin_g = din("fin_g", [1, D])
    w_in = din("w_in", [D, 4128]); w_upP = din("w_upP", [32, 512])
    w_glap = din("w_glap", [512, D]); pool_w = din("pool_w", [4, 128, 128]); w_poolp = din("w_poolp", [512, D])
    w_out = din("w_out", [D, D]); w_router = din("w_router", [D, 16])
    import os as _os
    NEW = 1 if _os.environ.get("KSTOP", "") else NE
    w_gate = din("w_gate", [NEW, D, D]); w_up = din("w_up", [NEW, D, D]); w_down = din("w_down", [NEW, D, D])
    consts = din("consts", [128, 9 * 128]); percore = din("percore", [128, 16])
    pmask = din("pmask", [1, NEXT]); invcnt = din("invcnt", [1, 4 * NTOK])
    sel = din("sel", [64, 16]); gmat = din("gmat", [64, 64]); consts2 = din("consts2", [128, 256])
    out = nc.dram_tensor("out", [NTOK, D], F32, kind="ExternalOutput").ap()
    xmid = nc.dram_tensor("xmid", [NTOK, D], F32).ap()
    h2rows = nc.dram_tensor("h2rows", [NTOK, D], BF16).ap()
    xsel = [nc.dram_tensor(f"xsel{i}", [CAP, D], BF16).ap() for i in range(NE)]
    ysel = [nc.dram_tensor(f"ysel{i}", [CAP, D], F32).ap() for i in range(NE)]
    U32 = mybir.dt.uint32
    ag1_in = nc.dram_tensor("ag1_in", [64, 1032], F32); ag1_out = nc.dram_tensor("ag1_out", [256, 1032], F32)
    ag2_in = nc.dram_tensor("ag2_in", [16, NTOK], F32); ag2_out = nc.dram_tensor("ag2_out", [64, NTOK], F32)
    GROUPS = [[0, 1, 2, 3], [4, 5, 6, 7]]

    def sb(name, shape, dt=F32):
        return nc.alloc_sbuf_tensor(name, list(shape), dt)

    CONS = sb("cons", [128, 9, 128])
    IDB = sb("idb", [128, 128], BF16); IDF = sb("idf", [128, 128])
    PC = sb("pc", [128, 16]); VEC = sb("vec", [128, 32]); CV = sb("cv", [128, 16]); SC = sb("scv", [128, 16])
    ABT = sb("abt", [128, 48]); MODF = sb("modf", [128, 64]); AB = sb("ab", [128, 48])
    GT1 = sb("gt1", [128, D]); GT2 = sb("gt2", [128, D]); BDEC = sb("bdec", [128, 512])
    WUP = sb("wup", [32, 512]); WRT = sb("wrt", [128, 8, 16], BF16)
    AFF = sb("aff", [128, NT, 16]); WGT = sb("wgt", [128, NT, 16])
    SEL = sb("selm", [64, 16]); GM = sb("gm", [64, 64])
    HT = sb("ht", [128, 8, NTOK], BF16)
    SMALL = sb("small", [128, 64])
    HF = sb("hf", [64, 2, 4, 128]); SINIT = sb("sinit", [64, 2, 4, 128])
    ARENA_BYTES = 146 * 1024
    ARENA = sb("arena", [128, ARENA_BYTES], U8)
    print("sbuf remaining", nc.sbuf_bytes_remaining)

    def view(off, shape, dt=F32, parts=128):
        esz = 4 if dt == F32 else 2
        n = 1
        for s_ in shape:
            n *= s_
        assert off % 32 == 0 and off + n * esz <= ARENA_BYTES, (off, n * esz)
        v = ARENA[0:parts, off:off + n * esz].bitcast(dt)
        if len(shape) == 2:
            return v.rearrange("p (a b) -> p a b", b=shape[1])
        if len(shape) == 3:
            return v.rearrange("p (a b c) -> p a b c", b=shape[1], c=shape[2])
        if len(shape) == 4:
            return v.rearrange("p (a b c d) -> p a b c d", b=shape[1], c=shape[2], d=shape[3])
        return v

    KB = 1024
    PTB = [nc.alloc_psum_tensor(f"ptb{i}", [128, 8, 128], BF16) for i in range(2)]
    PTBr = S.regions("ptb", 2, True)
    PSB = [nc.alloc_psum_tensor(f"psb{i}", [128, 512], F32) for i in range(6)]
    PSBr = S.regions("psb", 6, True)
    cnt = {"pt": 0, "ps": 0, "sm": 0}

    def ptb():
        i = cnt["pt"] % 2; cnt["pt"] += 1
        return PTB[i], PTBr[i]

    def psb():
        i = cnt["ps"] % 6; cnt["ps"] += 1
        return PSB[i], PSBr[i]

    SMr = S.regions("sm", 64)

    def small():
        i = cnt["sm"] % 64; cnt["sm"] += 1
        return SMALL[:, i:i + 1], SMr[i]

    R = {}

    def rg(name):
        if name not in R:
            R[name] = S.region(name)
        return R[name]

    dcount = [0]

    lstreams = {"sp": [S.dma_stream(f"lsp{i}") for i in range(40)], "pool": [S.dma_stream(f"lpl{i}") for i in range(24)]}
    lidx = {"sp": 0, "pool": 0}

    def load(q, dst, src, wr, rd=()):
        st = lstreams[q][lidx[q] % len(lstreams[q])]; lidx[q] += 1
        return S.dma(q, st, lambda e: e.dma_start(out=dst, in_=src), reads=list(rd), writes=list(wr))

    def op(e, fn, reads=(), writes=(), inc=True):
        return S.op(e, fn, reads=list(reads), writes=list(writes), inc=inc)

    import os
    STOP = os.environ.get("KSTOP", "")
    KSUB = int(os.environ.get("KSUB", "0"))

    def finish():
        S.barrier()
        S.emit()
        return nc

    load("sp", CONS[:], consts.rearrange("p (a b) -> p a b", b=128), [rg("cons")])
    load("sp", PC[:], percore, [rg("pc")]); load("sp", VEC[:], vecs, [rg("vec")]); load("sp", CV[:], cvec, [rg("cv")])
    load("sp", ABT[:], ada_bT, [rg("abt")]); load("sp", WUP[:], w_upP, [rg("wup")])
    load("sp", SEL[:], sel, [rg("sel")]); load("sp", GM[:], gmat, [rg("gm")])
    load("sp", BDEC[:], b_decay.partition_broadcast(128), [rg("bdec")])
    load("sp", GT1[:], ada_b[:, 2 * D:3 * D].partition_broadcast(128), [rg("gt1")])
    load("sp", GT2[:], ada_b[:, 5 * D:6 * D].partition_broadcast(128), [rg("gt2")])
    load("pool", WRT[:], w_router.rearrange("(k p) n -> p k n", p=128), [rg("wrt")])
    Uf, Ub, Lf, Lb, Mf, Mb, MKF, MKB = [CONS[:, i, :] for i in range(8)]
    BLK = CONS[:, 8, 0:2]; ONEC = CONS[:, 8, 2:3]
    op("pool", lambda e: e.memset(IDF[:], 0.0), [], [rg("idf")])
    op("pool", lambda e: e.affine_select(out=IDF[:], in_=IDF[:], pattern=[[-1, 128]], compare_op=ALU.not_equal,
                                         fill=1.0, base=0, channel_multiplier=1), [rg("idf")], [rg("idf")])
    op("dve", lambda e: e.tensor_copy(out=IDB[:], in_=IDF[:]), [rg("idf")], [rg("idb")])
    op("act", lambda e: e.activation(out=SC[:], in_=CV[:], func=AF.Silu), [rg("cv")], [rg("sc")])
    SCREP = view(0, [8, 128])
    op("dve", lambda e: e.memset(SCREP[:], 1.0), [], [rg("screp")])
    for k in range(8):
        op("dve", lambda e, k=k: e.tensor_scalar(out=SCREP[:, k, :], in0=SCREP[:, k, :], scalar1=SC[:, k:k + 1], scalar2=None, op0=ALU.mult),
           [rg("sc"), rg("screp")], [rg("screp")])
    ADW = [view(8 * KB + i * 32 * KB, [8, D]) for i in range(2)]
    ADWr = S.regions("adw", 2)
    modp, modpr = psb()
    grp_fm = [0, 1, 3, 4]
    modp_v = modp[:, 0:64].rearrange("p (a b) -> p a b", b=2)
    order = [0, 1, 3, 4, 2, 5]
    for n_, g in enumerate(order):
        slot = n_ % 2
        load("sp", ADW[slot][:], ada_w[:, g * D:(g + 1) * D].rearrange("(k p) n -> p k n", p=128), [ADWr[slot]])
        if g in grp_fm:
            gi = grp_fm.index(g)
            for cc in range(8):
                for k in range(8):
                    op("pe", lambda e, gi=gi, cc=cc, k=k, slot=slot: e.matmul(
                        modp_v[:, gi * 8 + cc, :], lhsT=ADW[slot][:, k, cc * 128:(cc + 1) * 128], rhs=SC[:, k:16:8],
                        start=(k == 0), stop=(k == 7)), [ADWr[slot], rg("sc")], [modpr], inc=(k == 7 and cc == 7))
        else:
            GT = GT1 if g == 2 else GT2
            gr = rg("gt1") if g == 2 else rg("gt2")
            for half in range(2):
                ps, psr = psb()
                for k in range(8):
                    op("pe", lambda e, k=k, slot=slot, half=half, ps=ps: e.matmul(
                        ps[:], lhsT=SCREP[:, k, :], rhs=ADW[slot][:, k, half * 512:(half + 1) * 512],
                        start=(k == 0), stop=(k == 7)), [ADWr[slot], rg("screp")], [psr], inc=(k == 7))
                op("dve", lambda e, ps=ps, GT=GT, half=half: e.tensor_tensor(
                    out=GT[:, half * 512:(half + 1) * 512], in0=ps[:], in1=GT[:, half * 512:(half + 1) * 512], op=ALU.add),
                   [psr, gr], [gr])
    MODFv = MODF[:].rearrange("p (a b) -> p a b", b=2)
    for gi, g in enumerate(grp_fm):
        for j in range(2):
            op("dve", lambda e, gi=gi, g=g, j=j: e.tensor_tensor(
                out=MODFv[:, gi * 8:(gi + 1) * 8, j], in0=modp_v[:, gi * 8:(gi + 1) * 8, j], in1=ABT[:, g * 8:(g + 1) * 8], op=ALU.add),
               [modpr, rg("abt")], [rg("modf")])
    A1, B1, ACX, BCX, A2, B2 = [AB[:, i * 8:(i + 1) * 8] for i in range(6)]

    def mkAB(A, B, gi_sh, gi_sc, j, gvec):
        op("dve", lambda e: e.scalar_tensor_tensor(out=A, in0=MODFv[:, gi_sc * 8:(gi_sc + 1) * 8, j], scalar=1.0, in1=gvec,
                                                    op0=ALU.add, op1=ALU.mult), [rg("modf"), rg("vec")], [rg("ab")])
        op("dve", lambda e: e.tensor_copy(out=B, in_=MODFv[:, gi_sh * 8:(gi_sh + 1) * 8, j]), [rg("modf")], [rg("ab")])

    mkAB(A1, B1, 0, 1, 0, VEC[:, 0:8]); mkAB(ACX, BCX, 0, 1, 1, VEC[:, 0:8]); mkAB(A2, B2, 2, 3, 0, VEC[:, 8:16])
    S.barrier()

    if STOP == "0":
        return finish()
    XT = [view(0 * KB + i * 4 * KB, [D]) for i in range(2)]
    XS = [view(8 * KB + i * 2 * KB, [D], BF16) for i in range(2)]
    JUNK = view(12 * KB, [D], BF16)
    XTr = S.regions("xt", 2); XSr = S.regions("xs", 2); JUNKr = rg("junk")
    tcount = [0]

    def norm_tile(src_dram, A, B, dst, dst_r, xt_pre=None):
        i = tcount[0] % 2; tcount[0] += 1
        xt, xs = XT[i], XS[i]
        if xt_pre is None:
            load("sp", xt, src_dram, [XTr[i]])
            xr = XTr[i]
        else:
            xt, xr = xt_pre
        ssq, ssqr = small(); sd, sdr = small(); rs, rsr = small()
        op("act", lambda e: e.activation(out=JUNK, in_=xt, func=AF.Square, accum_out=ssq), [xr], [JUNKr, ssqr])
        op("act", lambda e: e.activation(out=sd, in_=ssq, func=AF.Sqrt, scale=1.0 / D, bias=EPS), [ssqr], [sdr])
        op("dve", lambda e: e.reciprocal(out=rs, in_=sd), [sdr], [rsr])
        op("dve", lambda e: e.tensor_scalar(out=xs, in0=xt, scalar1=rs, scalar2=None, op0=ALU.mult), [xr, rsr], [XSr[i]])
        pt, ptr = ptb()
        for k in range(8):
            op("pe", lambda e, k=k: e.transpose(out=pt[:, k, :], in_=xs[:, k * 128:(k + 1) * 128], identity=IDB[:]),
               [XSr[i], rg("idb")], [ptr], inc=(k == 7))
        for k in range(8):
            if i == 0:
                op("act", lambda e, k=k: e.activation(out=dst[:, k, :], in_=pt[:, k, :], func=AF.Identity,
                                                      scale=A[:, k:k + 1], bias=B[:, k:k + 1]), [ptr, rg("ab")], [dst_r])
            else:
                op("dve", lambda e, k=k: e.tensor_scalar(out=dst[:, k, :], in0=pt[:, k, :], scalar1=A[:, k:k + 1], scalar2=B[:, k:k + 1],
                                                         op0=ALU.mult, op1=ALU.add), [ptr, rg("ab")], [dst_r])

    def wload(dst, cols, ncols, r):
        load("pool", dst, w_in[:, cols:cols + ncols].rearrange("(k p) n -> p k n", p=128), [r])

    HTr = S.regions("ht", NT)
    o_ = 14 * KB + 24 * KB + 512
    HTH = view(o_, [8, 1024], BF16); o_ += 16 * KB
    WP = view(o_, [8, 512], BF16); o_ += 8 * KB
    PD = view(o_, [48, 80]); o_ += 15 * KB
    TA = view(o_, [48, 80]); o_ += 15 * KB
    TB = view(o_, [48, 80]); o_ += 15 * KB
    INV = view(o_, [32, 64]); INVF = view(o_, [NTOK]); o_ += 8 * KB
    POOLED = view(o_, [NTOK], BF16); o_ += 4 * KB
    PW = view(o_, [4, 128], BF16); o_ += 1 * KB
    MIXT = view(130 * KB, [4, NTOK], BF16)
    assert o_ <= 130 * KB, o_
    HTHr = S.regions("hth", 8)
    wload(WP[:], C_P, 512, rg("wp"))
    load("pool", PW[:], pool_w.rearrange("g p c -> p g c"), [rg("pw")])
    go_ = 14 * KB
    WK = view(go_, [8, 256], BF16); go_ += 4 * KB
    WV = view(go_, [8, 512], BF16); go_ += 8 * KB
    WR = view(go_, [8, 32], BF16); go_ += 512
    WQ = view(go_, [8, 256], BF16); go_ += 4 * KB
    WG = view(go_, [8, 512], BF16); go_ += 8 * KB
    wload(WK[:], C_K, 256, rg("wk")); wload(WV[:], C_V, 512, rg("wv")); wload(WR[:], C_R, 32, rg("wr"))
    wload(WQ[:], C_Q, 256, rg("wq")); wload(WG[:], C_G, 512, rg("wg"))
    op("pool", lambda e: e.memset(PD[:], 0.0), [], [rg("pd")])
    for t in range(24):
        if t < 4:
            dst, dr = HTH[:, :, t * 128:(t + 1) * 128], HTHr[t]
        elif t < 20:
            dst, dr = HT[:, :, (t - 4) * 128:(t - 3) * 128], HTr[t - 4]
        else:
            dst, dr = HTH[:, :, (t - 16) * 128:(t - 15) * 128], HTHr[t - 16]
        norm_tile(xext[t * 128:(t + 1) * 128, :], A1, B1, dst, dr)

    ZT0 = view(122 * KB, [D], BF16)
    op("pool", lambda e: e.memset(ZT0, 0.0), [], [rg("zt0")])
    XSELr = S.regions("xsel", NE)
    for ex in range(NE):
        for st in range(8):
            load("sp", xsel[ex][st * 128:(st + 1) * 128, :], ZT0, [XSELr[ex]], [rg("zt0")])

    def ht_ext_block(bk):
        if bk == 0:
            return HTH[:, :, 0:512], HTHr[0:4]
        if bk == 5:
            return HTH[:, :, 512:1024], HTHr[4:8]
        return HT[:, :, (bk - 1) * 512:bk * 512], HTr[(bk - 1) * 4:bk * 4]

    for g in range(4):
        w = WINS[g]
        load("sp", INVF, invcnt[:, g * NTOK:(g + 1) * NTOK].partition_broadcast(128), [rg("inv")])
        for bk in range(6):
            ps, psr = psb()
            hsrc, hr = ht_ext_block(bk)
            for k in range(8):
                op("pe", lambda e, k=k, ps=ps, hsrc=hsrc: e.matmul(ps[:], lhsT=WP[:, k, g * 128:(g + 1) * 128], rhs=hsrc[:, k, :],
                                                              start=(k == 0), stop=(k == 7)), [rg("wp")] + hr, [psr], inc=(k == 7))
            vcol = 2 if bk == 0 else (3 if bk == 5 else 12)
            op("dve", lambda e, ps=ps, bk=bk, vcol=vcol: e.tensor_scalar(
                out=PD[:, bk * 8:(bk + 1) * 8, 8:72], in0=ps[:].rearrange("p (r c) -> p r c", c=64),
                scalar1=PC[:, vcol:vcol + 1], scalar2=None, op0=ALU.mult), [psr, rg("pc")], [rg("pd")])
        src, srr, m = PD, rg("pd"), 1
        bufs = [(TA, rg("ta")), (TB, rg("tb"))]
        bi = 0
        while m < w:
            dstb, dbr = bufs[bi]; bi ^= 1
            wd = 80 - 2 * m + 1
            op("dve", lambda e, src=src, dstb=dstb, m=m, wd=wd: e.tensor_tensor(
                out=dstb[:, :, 0:wd], in0=src[:, :, 0:wd], in1=src[:, :, m:m + wd], op=ALU.add), [srr], [dbr])
            src, srr, m = dstb, dbr, 2 * m
        coff = 8 - w // 2
        m = 1
        first = True
        while m < w:
            dstb, dbr = bufs[bi]; bi ^= 1
            nr = 48 - 2 * m + 1
            c0 = coff if first else 0
            op("dve", lambda e, src=src, dstb=dstb, m=m, nr=nr, c0=c0: e.tensor_tensor(
                out=dstb[:, 0:nr, 0:64], in0=src[:, 0:nr, c0:c0 + 64], in1=src[:, m:m + nr, c0:c0 + 64], op=ALU.add), [srr], [dbr])
            src, srr, m, first = dstb, dbr, 2 * m, False
        roff = 8 - w // 2
        dstb, dbr = bufs[bi]; bi ^= 1
        op("dve", lambda e, src=src, dstb=dstb: e.tensor_tensor(
            out=dstb[:, 0:32, 0:64], in0=src[:, roff:roff + 32, 0:64], in1=INV[:, :, :], op=ALU.mult), [srr, rg("inv")], [dbr])
        op("dve", lambda e, dstb=dstb: e.tensor_tensor(
            out=POOLED.rearrange("p (r c) -> p r c", c=64), in0=dstb[:, 0:32, 0:64], in1=PD[:, 8:40, 8:72], op=ALU.subtract),
           [dbr, rg("pd")], [rg("pooled")])
        for bk in range(4):
            ps, psr = psb()
            op("pe", lambda e, ps=ps, bk=bk: e.matmul(ps[:], lhsT=PW[:, g, :], rhs=POOLED[:, bk * 512:(bk + 1) * 512], start=True, stop=True),
               [rg("pw"), rg("pooled")], [psr])
            op("act", lambda e, ps=ps, bk=bk: e.activation(out=MIXT[:, g, bk * 512:(bk + 1) * 512], in_=ps[:], func=AF.Copy,
                                                           scale=VEC[:, 16 + g:17 + g]), [psr, rg("vec")], [rg("mixt")])
    S.barrier()

    if STOP == "P":
        return finish()
    o_ = 14 * KB
    o_ = go_
    KVB = view(o_, [32, 4, 128], BF16); o_ += 32 * KB
    KVC = view(o_, [4, 4, 128], BF16); o_ += 4 * KB
    DEC = view(o_, [4, 2, 32]); o_ += 1 * KB
    DECC = view(o_, [4, 2, 4]); o_ += 128
    RTS = view(o_, [128]); o_ += 512
    Z = view(o_, [512]); o_ += 2 * KB
    LL = view(o_, [512]); o_ += 2 * KB
    EKS = view(o_, [512]); o_ += 2 * KB
    KD = view(o_, [2, 256], BF16); o_ += 1 * KB
    VT = view(o_, [512], BF16); o_ += 1 * KB
    EBS2 = [view(o_ + i * KB, [256]) for i in range(2)]; o_ += 2 * KB
    EMS2 = [view(o_ + i * KB, [256]) for i in range(2)]; o_ += 2 * KB
    EMN2 = [view(o_ + i * KB, [256]) for i in range(2)]; o_ += 2 * KB
    QDH2 = [view(o_ + i * 512, [2, 128], BF16) for i in range(2)]; o_ += 1 * KB
    QM2 = [view(o_ + i * 512, [2, 128], BF16) for i in range(2)]; o_ += 1 * KB
    KM2 = [view(o_ + i * 512, [2, 128], BF16) for i in range(2)]; o_ += 1 * KB
    T1 = view(o_, [128]); o_ += 512
    T2 = view(o_, [128]); o_ += 512
    PTH = view(o_, [128], BF16); o_ += 256
    SF = view(o_, [4, 128]); o_ += 2 * KB
    SB2 = [view(o_ + i * 2 * KB, [4, 128]) for i in range(2)]; o_ += 4 * KB
    SFB = view(o_, [4, 128], BF16); o_ += 1 * KB
    TMPB = view(o_, [4, 128], BF16); o_ += 1 * KB
    ON = view(o_, [512], BF16); o_ += 1 * KB
    SG = view(o_, [4, 128], BF16); o_ += 1 * KB
    AGS = view(116224, [4, 1032])
    AGI_OFF = o_
    AGI = view(o_, [1032]); o_ += 4128 + 32 - 4128 % 32
    DM = view(o_, [8]); o_ += 32
    LTS = view(o_, [8]); o_ += 32
    OSUM = view(o_, [512]); o_ += 2 * KB
    KVM = view(o_, [4, 128]); o_ += 2 * KB
    OG = view(114 * KB, [4, NTOK], BF16)
    assert o_ <= 114 * KB, o_
    R['ags'] = rg('og')
    gts = ["rts", "z", "ll", "eks", "kd", "vt", "ebs", "ems", "emn", "qdh", "qm", "km", "t1", "t2", "pth", "sf", "sbk", "sfb", "tmpb",
           "on", "sg", "ags", "agi", "dm", "kvm", "dec", "decc", "kvb", "kvc", "og", "ltot"]
    psb5 = [0]
    pspool = [[0, 1, 2, 3, 4, 5]]

    def psb():
        pl = pspool[0]
        i = pl[psb5[0] % len(pl)]; psb5[0] += 1
        return PSB[i], PSBr[i]

    def gla_common(hsrc, hr, own_first, own_last, ltot_col=True):
        pv, pvr = psb()
        for k in range(8):
            op("pe", lambda e, k=k: e.matmul(pv[:], lhsT=hsrc[:, k, :], rhs=WV[:, k, :], start=(k == 0), stop=(k == 7)), hr + [rg("wv")], [pvr], inc=(k == 7))
        op("act", lambda e: e.copy(out=VT, in_=pv[:]), [pvr], [rg("vt")])
        pk, pkr = psb()
        for k in range(8):
            op("pe", lambda e, k=k: e.matmul(pk[:, 0:256], lhsT=hsrc[:, k, :], rhs=WK[:, k, :], start=(k == 0), stop=(k == 7)), hr + [rg("wk")], [pkr], inc=(k == 7))
        pr, prr = psb()
        for k in range(8):
            op("pe", lambda e, k=k: e.matmul(pr[0:32, 0:128], lhsT=WR[:, k, :], rhs=hsrc[:, k, :], start=(k == 0), stop=(k == 7)), hr + [rg("wr")], [prr], inc=(k == 7))
        op("act", lambda e: e.copy(out=RTS[0:32, :], in_=pr[0:32, 0:128]), [prr], [rg("rts")])
        op("pe", lambda e: e.matmul(pr[:, :], lhsT=RTS[0:32, :], rhs=WUP[:], start=True, stop=True), [rg("rts"), rg("wup")], [prr])
        op("dve", lambda e: e.tensor_tensor(out=Z, in0=pr[:], in1=BDEC[:], op=ALU.add), [prr, rg("bdec")], [rg("z")])
        op("act", lambda e: e.activation(out=Z, in_=Z, func=AF.Exp, scale=-1.0), [rg("z")], [rg("z")])
        op("act", lambda e: e.activation(out=LL, in_=Z, func=AF.Ln, bias=1.0), [rg("z")], [rg("ll")])
        pe_, per = psb()
        op("pe", lambda e: e.matmul(pe_[:, 0:256], lhsT=Lf, rhs=LL[:, 0:256], start=True, stop=True), [rg("cons"), rg("ll")], [per], inc=False)
        op("pe", lambda e: e.matmul(pe_[:, 256:512], lhsT=Lb, rhs=LL[:, 256:512], start=True, stop=True), [rg("cons"), rg("ll")], [per])
        op("act", lambda e: e.activation(out=EKS, in_=pe_[:], func=AF.Exp), [per], [rg("eks")])
        for d in range(2):
            op("dve", lambda e, d=d: e.tensor_tensor(out=KD[:, d, :], in0=pk[:, 0:256], in1=EKS[:, d * 256:(d + 1) * 256], op=ALU.mult),
               [pkr, rg("eks")], [rg("kd")])

    def kv_mm(h):
        res = []
        for c in range(2):
            pkv, pkvr = psb()
            for d in range(2):
                op("pe", lambda e, c=c, d=d: e.matmul(pkv[0:64, d * 128:(d + 1) * 128],
                                                      lhsT=KD[c * 64:(c + 1) * 64, d, h * 64:(h + 1) * 64],
                                                      rhs=VT[c * 64:(c + 1) * 64, h * 128:(h + 1) * 128], start=True, stop=True),
                   [rg("kd"), rg("vt")], [pkvr], inc=(d == 1))
            res.append((pkv, pkvr))
        return res

    def g1_tile(j, hsrc, hr, DECs, decr, KVs, kvr, ntl, seg_tot):
        gla_common(hsrc, hr, j == 0, j == ntl - 1)
        for h in range(4):
            pd_, pdr = psb()
            for d in range(2):
                op("pe", lambda e, d=d: e.matmul(pd_[0:64, d * 2:(d + 1) * 2], lhsT=LL[:, d * 256 + h * 64:d * 256 + (h + 1) * 64], rhs=BLK,
                                                 start=True, stop=True), [rg("ll"), rg("cons")], [pdr], inc=(d == 1))
            op("act", lambda e, h=h: e.activation(out=DECs[0:64, h, :, 2 * j:2 * j + 2], in_=pd_[0:64, 0:4].rearrange("p (d c) -> p d c", c=2),
                                                  func=AF.Exp), [pdr], [decr])
            if KSUB == 1:
                continue
            if seg_tot:
                pt_, ptr_ = psb()
                for d in range(2):
                    op("pe", lambda e, d=d: e.matmul(pt_[0:64, d:d + 1], lhsT=LL[:, d * 256 + h * 64:d * 256 + (h + 1) * 64], rhs=ONEC,
                                                     start=True, stop=True), [rg("ll"), rg("cons")], [ptr_], inc=(d == 1))
                op("dve", lambda e: e.tensor_tensor(out=LTS[0:64, h * 2:h * 2 + 2], in0=LTS[0:64, h * 2:h * 2 + 2], in1=pt_[0:64, 0:2], op=ALU.add),
                   [ptr_, rg("lts")], [rg("lts")])
            kvs = kv_mm(h)
            if KSUB == 2:
                continue
            for c in range(2):
                pkv, pkvr = kvs[c]
                if KSUB == 3 and c == 1:
                    continue
                op("dve", lambda e, c=c, h=h: e.scalar_tensor_tensor(out=SF[0:64, h, :], in0=SF[0:64, h, :], scalar=DECs[0:64, h, 0, 2 * j + c:2 * j + c + 1],
                                                                     in1=pkv[0:64, 0:128], op0=ALU.mult, op1=ALU.add),
                   [rg("sf"), decr, pkvr], [rg("sf")])
                op("act", lambda e, h=h, c=c: e.copy(out=KVs[0:64, 2 * j + c, h, :], in_=pkv[0:64, 128:256]), [pkvr], [kvr])

    SBr = S.regions("sbk", 2)

    def bwd_scan(nch, DECs, decr, KVs, kvr, store, cur=0):
        for n in range(nch - 1, -1, -1):
            nxt = cur ^ 1
            for h in range(4):
                op("dve", lambda e, h=h, n=n, cur=cur, nxt=nxt: e.scalar_tensor_tensor(out=SB2[nxt][0:64, h, :], in0=SB2[cur][0:64, h, :], scalar=DECs[0:64, h, 1, n:n + 1],
                                                                                       in1=KVs[0:64, n, h, :], op0=ALU.mult, op1=ALU.add),
                   [SBr[cur], decr, kvr], [SBr[nxt]])
            if store:
                op("act", lambda e, n=n, cur=cur: e.copy(out=KVs[0:64, n, :, :], in_=SB2[cur][0:64]), [SBr[cur]], [kvr])
            cur = nxt
        return cur

    CTXH = view(0, [8, 256], BF16)
    CTXH = AGS[:, :, :].bitcast(BF16) if False else view(AGI_OFF, [8, 256], BF16)
    CTXHr = S.regions("ctxh", 2)
    op("dve", lambda e: e.memset(SF[0:64], 0.0), [], [rg("sf")])
    op("dve", lambda e: e.memset(SB2[0][0:64], 0.0), [], [SBr[0]])
    if STOP == "W":
        return finish()
    for t in range(2):
        norm_tile(ctx[t * 128:(t + 1) * 128, :], ACX, BCX, CTXH[:, :, t * 128:(t + 1) * 128], CTXHr[t])
        if STOP == "C1":
            gla_common(CTXH[:, :, t * 128:(t + 1) * 128], [CTXHr[t]], True, True)
            return finish()
        g1_tile(t, CTXH[:, :, t * 128:(t + 1) * 128], [CTXHr[t]], DECC, rg("decc"), KVC, rg("kvc"), 2, False)
        if STOP == "C2":
            return finish()
    cb = bwd_scan(4, DECC, rg("decc"), KVC, rg("kvc"), False, 0)
    op("act", lambda e: e.copy(out=HF[:, 0], in_=SF[0:64]), [rg("sf")], [rg("hf")])
    op("act", lambda e: e.copy(out=HF[:, 1], in_=SB2[cb][0:64]), [SBr[cb]], [rg("hf")])
    if STOP == "C":
        return finish()
    op("dve", lambda e: e.memset(LTS[0:64], 0.0), [], [rg("lts")])
    op("dve", lambda e: e.memset(SF[0:64], 0.0), [rg("sf")], [rg("sf")])
    op("dve", lambda e: e.memset(SB2[0][0:64], 0.0), [SBr[0]], [SBr[0]])
    for j in range(NT):
        g1_tile(j, HT[:, :, j * 128:(j + 1) * 128], [HTr[j]], DEC, rg("dec"), KVB, rg("kvb"), NT, True)
    cb = bwd_scan(32, DEC, rg("dec"), KVB, rg("kvb"), False, 0)
    if STOP == "G1":
        return finish()
    op("act", lambda e: e.copy(out=AGI[0:64, 0:512], in_=SF[0:64].rearrange("p h v -> p (h v)")), [rg("sf")], [rg("agi")])
    op("act", lambda e: e.copy(out=AGI[0:64, 512:1024], in_=SB2[cb][0:64].rearrange("p h v -> p (h v)")), [SBr[cb]], [rg("agi")])
    op("act", lambda e: e.activation(out=AGI[0:64, 1024:1032].rearrange("p (d h) -> p d h", d=2),
                                     in_=LTS[0:64, 0:8].rearrange("p (h d) -> p d h", d=2), func=AF.Exp), [rg("lts")], [rg("agi")])
    load("pool", ag1_in.ap(), AGI[0:64, :], [rg("ag1in")], [rg("agi")])
    ccs = S.dma_stream("cc1")
    S.dma_count[ccs] = 0

    def cc_dma(stream, fn, reads, writes):
        fn = _replay(fn(_REC))
        S._deps("pool", reads, writes)
        S.dma_count[stream] += 1
        tok = (stream, S.dma_count[stream])
        S.items["pool"].append(("op", fn, stream, 1))
        S.clk[tok] = dict(S.seen["pool"])
        S._record(tok, reads, writes)

    cc_dma(ccs, lambda e: e.collective_compute("AllGather", ALU.bypass, replica_groups=GROUPS,
                                               ins=[ag1_in.ap().opt()], outs=[ag1_out.ap().opt()]), [rg("ag1in")], [rg("ag1out")])
    load("pool", AGS[0:64], ag1_out.ap().rearrange("(r p) f -> p r f", p=64), [rg("ags")], [rg("ag1out")])
    op("dve", lambda e: e.tensor_copy(out=SINIT[:], in_=HF[:]), [rg("hf")], [rg("sinit")])
    for d in range(2):
        for r in (range(4) if d == 0 else range(3, -1, -1)):
            mcol = PC[0:64, 4 + d * 4 + r:5 + d * 4 + r]
            op("dve", lambda e, r=r, d=d, mcol=mcol: e.tensor_scalar(out=DM[0:64, 0:4], in0=AGS[0:64, r, 1024 + d * 4:1028 + d * 4], scalar1=-1.0, scalar2=mcol,
                                                                     op0=ALU.add, op1=ALU.mult), [rg("ags"), rg("pc")], [rg("dm")])
            op("dve", lambda e: e.tensor_scalar(out=DM[0:64, 4:8], in0=DM[0:64, 0:4], scalar1=1.0, scalar2=None, op0=ALU.add), [rg("dm")], [rg("dm")])
            op("dve", lambda e, r=r, d=d, mcol=mcol: e.tensor_scalar(out=KVM[0:64].rearrange("p h v -> p (h v)"), in0=AGS[0:64, r, d * 512:(d + 1) * 512],
                                                                     scalar1=mcol, scalar2=None, op0=ALU.mult), [rg("ags"), rg("pc")], [rg("kvm")])
            for h in range(4):
                op("dve", lambda e, h=h, d=d: e.scalar_tensor_tensor(out=SINIT[:, d, h, :], in0=SINIT[:, d, h, :], scalar=DM[0:64, 4 + h:5 + h],
                                                                     in1=KVM[0:64, h, :], op0=ALU.mult, op1=ALU.add), [rg("sinit"), rg("dm"), rg("kvm")], [rg("sinit")])
    op("dve", lambda e: e.tensor_copy(out=SB2[0][0:64], in_=SINIT[:, 1]), [rg("sinit"), SBr[0]], [SBr[0]])
    bwd_scan(32, DEC, rg("dec"), KVB, rg("kvb"), True, 0)
    op("dve", lambda e: e.tensor_copy(out=SF[0:64], in_=SINIT[:, 0]), [rg("sinit"), rg("sf")], [rg("sf")])

    if STOP == "X":
        return finish()
    def g2_tile(j):
        hsrc, hr = HT[:, :, j * 128:(j + 1) * 128], [HTr[j]]
        gla_common(hsrc, hr, False, False)
        pO = [(PSB[3], PSBr[3]), (PSB[4], PSBr[4])]
        pI1, pI1r = PSB[5], PSBr[5]
        def H1(h):
            b_ = h % 2
            EBS, EMS, EMN, QDH, QM, KM = EBS2[b_], EMS2[b_], EMN2[b_], QDH2[b_], QM2[b_], KM2[b_]
            ebr, emr, enr, qdr, qmr, kmr = (rg(f"ebs{b_}"), rg(f"ems{b_}"), rg(f"emn{b_}"), rg(f"qdh{b_}"), rg(f"qm{b_}"), rg(f"km{b_}"))
            pf, pfr = psb()
            for qi, W_ in enumerate((WK, WQ)):
                for k in range(8):
                    op("pe", lambda e, k=k, qi=qi, W_=W_: e.matmul(pf[0:64, qi * 128:(qi + 1) * 128], lhsT=W_[:, k, h * 64:(h + 1) * 64], rhs=hsrc[:, k, :],
                                                                  start=(k == 0), stop=(k == 7)), hr + [rg("wk"), rg("wq")], [pfr], inc=(k == 7 and qi == 1))
            pb, pbr = psb()
            for ci, (d, Cm) in enumerate(((0, Uf), (1, Ub), (0, Mf), (1, Mb))):
                op("pe", lambda e, ci=ci, d=d, Cm=Cm: e.matmul(pb[0:64, ci * 128:(ci + 1) * 128], lhsT=LL[:, d * 256 + h * 64:d * 256 + (h + 1) * 64], rhs=Cm,
                                                               start=True, stop=True), [rg("ll"), rg("cons")], [pbr], inc=(ci == 3))
            op("act", lambda e: e.activation(out=EBS[0:64], in_=pb[0:64, 0:256], func=AF.Exp), [pbr], [ebr])
            op("act", lambda e: e.activation(out=EMS[0:64], in_=pb[0:64, 256:512], func=AF.Exp), [pbr], [emr])
            op("act", lambda e: e.activation(out=EMN[0:64], in_=pb[0:64, 256:512], func=AF.Exp, scale=-1.0), [pbr], [enr])
            for d in range(2):
                op("dve", lambda e, d=d: e.scalar_tensor_tensor(out=QDH[0:64, d, :], in0=pf[0:64, 128:256], scalar=0.125, in1=EBS[0:64, d * 128:(d + 1) * 128],
                                                                op0=ALU.mult, op1=ALU.mult), [pfr, ebr], [qdr])
                op("dve", lambda e, d=d: e.scalar_tensor_tensor(out=QM[0:64, d, :], in0=pf[0:64, 128:256], scalar=0.125, in1=EMS[0:64, d * 128:(d + 1) * 128],
                                                                op0=ALU.mult, op1=ALU.mult), [pfr, emr], [qmr])
                op("dve", lambda e, d=d: e.tensor_tensor(out=KM[0:64, d, :], in0=pf[0:64, 0:128], in1=EMN[0:64, d * 128:(d + 1) * 128], op=ALU.mult),
                   [pfr, enr], [kmr])

        def H2(h):
            b_ = h % 2
            EBS, QDH, QM, KM = EBS2[b_], QDH2[b_], QM2[b_], KM2[b_]
            ebr, qdr, qmr, kmr = rg(f"ebs{b_}"), rg(f"qdh{b_}"), rg(f"qm{b_}"), rg(f"km{b_}")
            psc, pscr = psb()
            for d in range(2):
                op("pe", lambda e, d=d: e.matmul(psc[:, d * 128:(d + 1) * 128], lhsT=KM[0:64, d, :], rhs=QM[0:64, d, :], start=True, stop=True),
                   [kmr, qmr], [pscr], inc=(d == 1))
            op("dve", lambda e: e.tensor_tensor(out=T1, in0=psc[:, 0:128], in1=MKF, op=ALU.mult), [pscr, rg("cons")], [rg("t1")])
            op("dve", lambda e: e.tensor_tensor(out=T2, in0=psc[:, 128:256], in1=MKB, op=ALU.mult), [pscr, rg("cons")], [rg("t2")])
            op("dve", lambda e: e.tensor_tensor(out=PTH, in0=T1, in1=T2, op=ALU.add), [rg("t1"), rg("t2")], [rg("pth")])
            kvs = kv_mm(h)
            for c in range(2):
                po, por = pO[c]
                pkv, pkvr = kvs[c]
                op("act", lambda e, h=h: e.copy(out=SFB[0:64, h, :], in_=SF[0:64, h, :]), [rg("sf")], [rg("sfb")])
                op("pe", lambda e, c=c, h=h, po=po: e.matmul(po[0:64, h * 128:(h + 1) * 128], lhsT=QDH[0:64, 0, c * 64:(c + 1) * 64], rhs=SFB[0:64, h, :],
                                                             start=True, stop=False), [qdr, rg("sfb")], [por], inc=False)
                op("pe", lambda e, c=c, h=h, po=po: e.matmul(po[0:64, h * 128:(h + 1) * 128], lhsT=QDH[0:64, 1, c * 64:(c + 1) * 64], rhs=KVB[0:64, 2 * j + c, h, :],
                                                             start=False, stop=(c == 1)), [qdr, rg("kvb")], [por], inc=(c == 1))
                pin, pinr = (po, por) if c == 0 else (pI1, pI1r)
                op("pe", lambda e, c=c, h=h, pin=pin: e.matmul(pin[0:64, h * 128:(h + 1) * 128], lhsT=PTH[c * 64:(c + 1) * 64, c * 64:(c + 1) * 64],
                                                               rhs=VT[c * 64:(c + 1) * 64, h * 128:(h + 1) * 128], start=(c == 1), stop=True),
                   [rg("pth"), rg("vt")], [pinr], inc=True)
                op("dve", lambda e, c=c, h=h: e.scalar_tensor_tensor(out=SF[0:64, h, :], in0=SF[0:64, h, :], scalar=EBS[0:64, c * 64 + 63:c * 64 + 64],
                                                                     in1=pkv[0:64, 0:128], op0=ALU.mult, op1=ALU.add),
                   [rg("sf"), ebr, pkvr], [rg("sf")])

        H1(0)
        for h in range(4):
            if h < 3:
                H1(h + 1)
            H2(h)
        ptp, ptpr = PTB[0], PTBr[0]
        for c in range(2):
            po, por = pO[c]
            if c == 0:
                op("act", lambda e, po=po: e.copy(out=OSUM[0:64, :], in_=po[0:64, :]), [por], [rg("osum")])
            else:
                op("act", lambda e: e.copy(out=OSUM[0:64, :], in_=pI1[0:64, :]), [pI1r], [rg("osum")])
                op("dve", lambda e, po=po: e.tensor_tensor(out=OSUM[0:64, :], in0=po[0:64, :], in1=OSUM[0:64, :], op=ALU.add), [por, rg("osum")], [rg("osum")])
            po, por = OSUM, rg("osum")
            for h in range(4):
                op("act", lambda e, h=h, po=po: e.activation(out=T1[0:64, :], in_=po[0:64, h * 128:(h + 1) * 128], func=AF.Square,
                                                             accum_out=DM[0:64, h:h + 1]), [por], [rg("t1"), rg("dm")])
            op("act", lambda e: e.activation(out=DM[0:64, 4:8], in_=DM[0:64, 0:4], func=AF.Sqrt, scale=1.0 / 128, bias=EPS), [rg("dm")], [rg("dm")])
            op("dve", lambda e: e.reciprocal(out=DM[0:64, 0:4], in_=DM[0:64, 4:8]), [rg("dm")], [rg("dm")])
            for h in range(4):
                op("dve", lambda e, h=h, po=po: e.tensor_scalar(out=ON[0:64, h * 128:(h + 1) * 128], in0=po[0:64, h * 128:(h + 1) * 128],
                                                                scalar1=DM[0:64, h:h + 1], scalar2=None, op0=ALU.mult), [por, rg("dm")], [rg("on")])
            for h in range(4):
                op("pe", lambda e, h=h, c=c: e.transpose(out=ptp[:, h, c * 64:(c + 1) * 64], in_=ON[0:64, h * 128:(h + 1) * 128], identity=IDB[0:64, 0:64]),
                   [rg("on"), rg("idb")], [ptpr], inc=(h == 3))
        pg, pgr = psb()
        for h in range(4):
            for k in range(8):
                op("pe", lambda e, h=h, k=k: e.matmul(pg[:, h * 128:(h + 1) * 128], lhsT=WG[:, k, h * 128:(h + 1) * 128], rhs=hsrc[:, k, :],
                                                      start=(k == 0), stop=(k == 7)), hr + [rg("wg")], [pgr], inc=(k == 7 and h == 3))
        op("act", lambda e: e.activation(out=SG[:].rearrange("p h t -> p (h t)"), in_=pg[:], func=AF.Silu), [pgr], [rg("sg")])
        op("dve", lambda e: e.scalar_tensor_tensor(out=OG[:, :, j * 128:(j + 1) * 128], in0=ptp[:, 0:4, :], scalar=VEC[:, 20:21], in1=SG[:],
                                                   op0=ALU.mult, op1=ALU.mult), [ptpr, rg("vec"), rg("sg")], [rg("og")])

    PSB.append(PTB[1][:].rearrange("p a b -> p (a b)").bitcast(F32)); PSBr.append(PTBr[1])
    pspool[0] = [0, 1, 2, 6]
    for j in range(NT):
        g2_tile(j)
    pspool[0] = [0, 1, 2, 3, 4, 5]
    S.barrier()

    if STOP == "G2":
        return finish()
    o_ = 14 * KB
    WGP = view(o_, [4, D], BF16); o_ += 8 * KB
    WPP = view(o_, [4, D], BF16); o_ += 8 * KB
    WO = view(o_, [8, D], BF16); o_ += 16 * KB
    WM = view(o_, [8, 2048], BF16); o_ += 32 * KB
    ZT = view(o_, [8, 512], BF16); o_ += 8 * KB
    SGA = view(o_, [512], BF16); o_ += 1 * KB
    SGB = view(o_, [512], BF16); o_ += 1 * KB
    TM1 = view(o_, [512]); o_ += 2 * KB
    TM2 = view(o_, [512]); o_ += 2 * KB
    XMs = [view(o_ + i * 4 * KB, [D]) for i in range(2)]; o_ += 8 * KB
    XRs = XT; XRr = XTr
    H2TK = [view(o_ + i * 2 * KB, [D], BF16) for i in range(2)]; o_ += 4 * KB
    H2TKr = S.regions("h2tk", 2)
    XMr = S.regions("xm", 2)
    EXPT = view(o_, [16]); o_ += 64
    AFTS = view(o_, [NTOK]); o_ += 8 * KB
    assert o_ <= 114 * KB, o_
    load("pool", WGP[:], w_glap.rearrange("(k p) n -> p k n", p=128), [rg("wgp")])
    load("pool", WPP[:], w_poolp.rearrange("(k p) n -> p k n", p=128), [rg("wpp")])
    load("pool", WO[:], w_out.rearrange("(k p) n -> p k n", p=128), [rg("wo")])
    wload(WM[:], C_M, 2048, rg("wm"))
    for bk in range(4):
        hsrc, hr = HT[:, :, bk * 512:(bk + 1) * 512], HTr[bk * 4:(bk + 1) * 4]
        for dc in range(8):
            pbg, pbgr = psb(); pbp, pbpr = psb(); pmg, pmgr = psb(); pmp, pmpr = psb()
            for h in range(4):
                op("pe", lambda e, h=h, dc=dc: e.matmul(pbg[:], lhsT=WGP[:, h, dc * 128:(dc + 1) * 128], rhs=OG[:, h, bk * 512:(bk + 1) * 512],
                                                        start=(h == 0), stop=(h == 3)), [rg("wgp"), rg("og")], [pbgr], inc=(h == 3))
            for g in range(4):
                op("pe", lambda e, g=g, dc=dc: e.matmul(pbp[:], lhsT=WPP[:, g, dc * 128:(dc + 1) * 128], rhs=MIXT[:, g, bk * 512:(bk + 1) * 512],
                                                        start=(g == 0), stop=(g == 3)), [rg("wpp"), rg("mixt")], [pbpr], inc=(g == 3))
            for k in range(8):
                op("pe", lambda e, k=k, dc=dc: e.matmul(pmg[:], lhsT=WM[:, k, dc * 128:(dc + 1) * 128], rhs=hsrc[:, k, :], start=(k == 0), stop=(k == 7)),
                   hr + [rg("wm")], [pmgr], inc=(k == 7))
            for k in range(8):
                op("pe", lambda e, k=k, dc=dc: e.matmul(pmp[:], lhsT=WM[:, k, 1024 + dc * 128:1024 + (dc + 1) * 128], rhs=hsrc[:, k, :], start=(k == 0), stop=(k == 7)),
                   hr + [rg("wm")], [pmpr], inc=(k == 7))
            op("act", lambda e: e.activation(out=SGA, in_=pmg[:], func=AF.Sigmoid), [pmgr], [rg("sga")])
            op("act", lambda e: e.activation(out=SGB, in_=pmp[:], func=AF.Sigmoid), [pmpr], [rg("sgb")])
            op("dve", lambda e: e.tensor_tensor(out=TM1, in0=pbg[:], in1=SGA, op=ALU.mult), [pbgr, rg("sga")], [rg("tm1")])
            op("dve", lambda e: e.tensor_tensor(out=TM2, in0=pbp[:], in1=SGB, op=ALU.mult), [pbpr, rg("sgb")], [rg("tm2")])
            op("dve", lambda e, dc=dc: e.tensor_tensor(out=ZT[:, dc, :], in0=TM1, in1=TM2, op=ALU.add), [rg("tm1"), rg("tm2")], [rg("zt")])
        def y_stage(tt):
            j = bk * 4 + tt
            b_ = j % 2
            load("sp", XRs[b_], xext[512 + j * 128:512 + (j + 1) * 128, :], [XRr[b_]])
            for half in range(2):
                py, pyr = psb()
                for dc in range(8):
                    op("pe", lambda e, dc=dc, half=half, tt=tt: e.matmul(py[:], lhsT=ZT[:, dc, tt * 128:(tt + 1) * 128], rhs=WO[:, dc, half * 512:(half + 1) * 512],
                                                                        start=(dc == 0), stop=(dc == 7)), [rg("zt"), rg("wo")], [pyr], inc=(dc == 7))
                op("dve", lambda e, half=half: e.tensor_tensor(out=XMs[b_][:, half * 512:(half + 1) * 512], in0=py[:], in1=GT1[:, half * 512:(half + 1) * 512], op=ALU.mult),
                   [pyr, rg("gt1")], [XMr[b_]])
            op("dve", lambda e: e.tensor_tensor(out=XMs[b_], in0=XMs[b_], in1=XRs[b_], op=ALU.add), [XMr[b_], XRr[b_]], [XMr[b_]])
            load("sp", xmid[j * 128:(j + 1) * 128, :], XMs[b_], [rg("xmid")], [XMr[b_]])

        def n_stage(tt):
            j = bk * 4 + tt
            b_ = j % 2
            norm_tile(None, A2, B2, HT[:, :, j * 128:(j + 1) * 128], HTr[j], xt_pre=(XMs[b_], XMr[b_]))
            pt2, pt2r = ptb()
            for k in range(8):
                op("pe", lambda e, k=k, j=j: e.transpose(out=pt2[:, k, :], in_=HT[:, k, j * 128:(j + 1) * 128], identity=IDB[:]),
                   [HTr[j], rg("idb")], [pt2r], inc=(k == 7))
            op("act", lambda e: e.copy(out=H2TK[b_].rearrange("p (k d) -> p k d", k=8), in_=pt2[:]), [pt2r], [H2TKr[b_]])
            load("sp", h2rows[j * 128:(j + 1) * 128, :], H2TK[b_], [rg("h2rows")], [H2TKr[b_]])
            pl, plr = psb()
            for k in range(8):
                op("pe", lambda e, k=k, j=j: e.matmul(pl[:, 0:16], lhsT=HT[:, k, j * 128:(j + 1) * 128], rhs=WRT[:, k, :], start=(k == 0), stop=(k == 7)),
                   [HTr[j], rg("wrt")], [plr], inc=(k == 7))
            mx, mxr = small(); sm, smr = small(); rsm, rsmr = small()
            op("dve", lambda e: e.reduce_max(out=mx, in_=pl[:, 0:16], axis=AX.X), [plr], [mxr])
            op("dve", lambda e: e.tensor_scalar(out=mx, in0=mx, scalar1=-1.0, scalar2=None, op0=ALU.mult), [mxr], [mxr])
            op("act", lambda e: e.activation(out=EXPT, in_=pl[:, 0:16], func=AF.Exp, bias=mx, accum_out=sm), [plr, mxr], [rg("expt"), smr])
            op("dve", lambda e: e.reciprocal(out=rsm, in_=sm), [smr], [rsmr])
            op("dve", lambda e, j=j: e.tensor_scalar(out=AFF[:, j, :], in0=EXPT, scalar1=rsm, scalar2=None, op0=ALU.mult), [rg("expt"), rsmr], [rg("aff")])
            pa, par = psb()
            op("pe", lambda e, j=j: e.transpose(out=pa[0:16, 0:128], in_=AFF[:, j, :], identity=IDF[:]), [rg("aff"), rg("idf")], [par])
            op("act", lambda e, j=j: e.copy(out=AFTS[0:16, j * 128:(j + 1) * 128], in_=pa[0:16, 0:128]), [par], [rg("afts")])

        y_stage(0)
        for tt in range(4):
            if tt + 1 < 4:
                y_stage(tt + 1)
            n_stage(tt)
    load("pool", ag2_in.ap(), AFTS[0:16, :], [rg("ag2in")], [rg("afts")])
    cc2 = S.dma_stream("cc2")
    cc_dma(cc2, lambda e: e.collective_compute("AllGather", ALU.bypass, replica_groups=GROUPS,
                                               ins=[ag2_in.ap().opt()], outs=[ag2_out.ap().opt()]), [rg("ag2in")], [rg("ag2out")])
    S.barrier()
    AG = view(14 * KB, [NTOK]); CMP = view(22 * KB, [NTOK]); o_ = 30 * KB
    WGE = view(96 * KB, [8, D], BF16); WUE = view(112 * KB, [8, D], BF16); WDE = view(128 * KB, [8, D], BF16)
    wst = [S.dma_stream(f"we{i}") for i in range(3)]

    wst += [S.dma_stream(f"we{i}") for i in range(3, 5)]

    def load_expert(ex):
        load_gu(ex); load_d(ex)

    def load_d(ex):
        S.dma("pool", wst[4], lambda e: e.dma_start(out=WDE[:], in_=w_down[ex].rearrange("(k p) n -> p k n", p=128)), writes=[rg("wde")])

    def load_gu(ex):
        for hf in range(2):
            cs = slice(hf * 512, (hf + 1) * 512)
            S.dma("pool", wst[hf * 2], lambda e, cs=cs: e.dma_start(out=WGE[:, :, cs], in_=w_gate[ex][:, cs].rearrange("(k p) n -> p k n", p=128)), writes=[rg(f"wge{hf}")])
            S.dma("pool", wst[hf * 2 + 1], lambda e, cs=cs: e.dma_start(out=WUE[:, :, cs], in_=w_up[ex][:, cs].rearrange("(k p) n -> p k n", p=128)), writes=[rg(f"wue{hf}")])
    LO = view(o_, [4]); o_ += 32
    load("pool", AG[0:64, :], ag2_out.ap(), [rg("ag")], [rg("ag2out")])
    load_expert(0)
    op("dve", lambda e: e.memset(LO[0:64, 0:1], 0.0), [], [rg("lo")])
    for it in range(27):
        wdt = 2.0 ** -(it + 1)
        op("dve", lambda e, wdt=wdt: e.tensor_scalar(out=LO[0:64, 1:2], in0=LO[0:64, 0:1], scalar1=wdt, scalar2=None, op0=ALU.add), [rg("lo")], [rg("lo")])
        op("dve", lambda e: e.tensor_scalar(out=CMP[0:64, :], in0=AG[0:64, :], scalar1=LO[0:64, 1:2], scalar2=None, op0=ALU.is_gt), [rg("ag"), rg("lo")], [rg("cmp")])
        op("dve", lambda e: e.reduce_sum(out=LO[0:64, 2:3], in_=CMP[0:64, :], axis=AX.X), [rg("cmp")], [rg("lo")])
        pc_, pcr = psb()
        op("pe", lambda e, pc_=pc_: e.matmul(pc_[0:64, 0:1], lhsT=GM[:], rhs=LO[0:64, 2:3], start=True, stop=True), [rg("gm"), rg("lo")], [pcr])
        op("dve", lambda e, pc_=pc_: e.tensor_scalar(out=LO[0:64, 3:4], in0=pc_[0:64, 0:1], scalar1=CAP - 0.5, scalar2=None, op0=ALU.is_ge), [pcr], [rg("lo")])
        op("dve", lambda e, wdt=wdt: e.scalar_tensor_tensor(out=LO[0:64, 0:1], in0=LO[0:64, 3:4], scalar=wdt, in1=LO[0:64, 0:1], op0=ALU.mult, op1=ALU.add),
           [rg("lo")], [rg("lo")])
    op("dve", lambda e: e.scalar_tensor_tensor(out=CMP[0:64, :], in0=AG[0:64, :], scalar=LO[0:64, 0:1], in1=AG[0:64, :], op0=ALU.is_gt, op1=ALU.mult),
       [rg("ag"), rg("lo")], [rg("cmp")])
    for j in range(NT):
        pw_, pwr = psb()
        op("pe", lambda e, j=j, pw_=pw_: e.matmul(pw_[:, 0:16], lhsT=CMP[0:64, j * 128:(j + 1) * 128], rhs=SEL[:], start=True, stop=True), [rg("cmp"), rg("sel")], [pwr])
        op("act", lambda e, j=j, pw_=pw_: e.copy(out=WGT[:, j, :], in_=pw_[:, 0:16]), [pwr], [rg("wgt")])
    o_ = 0
    C2 = view(o_, [2, 128]); o_ += 1 * KB
    MK = view(o_, [256]); o_ += 1 * KB
    CSM = view(o_, [NT, 16]); o_ += 1 * KB
    SLF = view(o_, [256]); o_ += 1 * KB
    SLI = ARENA[:, 94 * KB:95 * KB].bitcast(U32)
    RST = [view(64 * KB + (4 + i) * 2 * KB, [D], BF16) for i in range(3)]
    RSTr = S.regions("rst", 3)
    rs_cnt = [0]
    load("sp", C2[:], consts2.rearrange("p (a b) -> p a b", b=128), [rg("c2")])
    op("dve", lambda e: e.tensor_scalar(out=MK, in0=WGT[:].rearrange("p j e -> p (j e)"), scalar1=0.0, scalar2=None, op0=ALU.is_gt), [rg("wgt")], [rg("mk")])
    op("dve", lambda e: e.memset(CSM[:, 0, :], 0.0), [], [rg("csm")])
    for j in range(1, NT):
        op("dve", lambda e, j=j: e.tensor_tensor(out=CSM[:, j, :], in0=CSM[:, j - 1, :], in1=MK[:, (j - 1) * 16:j * 16], op=ALU.add), [rg("csm"), rg("mk")], [rg("csm")])
    prk, prkr = psb()
    op("pe", lambda e: e.matmul(prk[:, 0:256], lhsT=C2[:, 0, :], rhs=MK, start=True, stop=False), [rg("c2"), rg("mk")], [prkr], inc=False)
    op("pe", lambda e: e.matmul(prk[:, 0:256], lhsT=C2[:, 1, :], rhs=CSM[:].rearrange("p j e -> p (j e)"), start=False, stop=True), [rg("c2"), rg("csm")], [prkr])
    op("dve", lambda e: e.scalar_tensor_tensor(out=SLF, in0=prk[:, 0:256], scalar=-4096.0, in1=MK, op0=ALU.add, op1=ALU.mult), [prkr, rg("mk")], [rg("slf")])
    op("dve", lambda e: e.tensor_scalar(out=SLI, in0=SLF, scalar1=4096.0, scalar2=None, op0=ALU.add), [rg("slf")], [rg("sli")])
    YSELr = S.regions("ysel", NE)

    def pdma(fn, reads, writes):
        st = lstreams["pool"][lidx["pool"] % len(lstreams["pool"])]; lidx["pool"] += 1
        return S.dma("pool", st, fn, reads=list(reads), writes=list(writes))

    SXr = [S.regions(f"sx{ex}_", NT) for ex in range(NE)]

    def scatter_expert(ex):
        for j in range(NT):
            i = rs_cnt[0] % 3; rs_cnt[0] += 1
            load("sp", RST[i], h2rows[j * 128:(j + 1) * 128, :], [RSTr[i]], [rg("h2rows")])
            pdma(lambda e, j=j, i=i: e.indirect_dma_start(
                out=xsel[ex], out_offset=bass.IndirectOffsetOnAxis(SLI[:, j * 16 + ex:j * 16 + ex + 1], 0),
                in_=RST[i], in_offset=None, bounds_check=CAP - 1, oob_is_err=False), [RSTr[i], rg("sli"), XSELr[ex]], [SXr[ex][j]])

    scatter_expert(0)
    scatter_expert(1)

    if STOP == "R":
        return finish()
    XACC = view(0, [NT, D]); o_ = 64 * KB
    NXS = 4
    XSE = [view(o_ + i * 2 * KB, [D], BF16) for i in range(NXS)]; o_ += 7 * 2 * KB
    YS = [view(o_ + i * 4 * KB, [D]) for i in range(2)]; o_ += 8 * KB
    GTL = [view(o_ + i * 4 * KB, [D]) for i in range(2)]; o_ += 8 * KB
    assert o_ <= 94 * KB
    o_ = 144 * KB
    SGT = [view(o_ + i * KB, [512], BF16) for i in range(2)]; o_ += 2 * KB
    assert o_ <= ARENA_BYTES, o_
    SGTr = S.regions("sgt", 2); XSEr = S.regions("xse", NXS); YSr = S.regions("ys", 2); GTLr = S.regions("gtl", 2)
    XST = HT[:, :, 0:CAP]; HTE = HT[:, :, CAP:2 * CAP]
    XSTr = S.regions("xst", 2); HTEr = S.regions("hte", 2)
    XAr = S.regions("xacc", NT)
    for i in range(2):
        op("dve", lambda e, i=i: e.memset(GTL[i], 0.0), [], [GTLr[i]])
    xs_cnt = [0]

    def prep_expert(ex):
        for st in range(8):
            i = xs_cnt[0] % NXS; xs_cnt[0] += 1
            extra = []
            S.dma("sp", lstreams["sp"][lidx["sp"] % len(lstreams["sp"])], lambda e, i=i, st=st: e.dma_start(out=XSE[i], in_=xsel[ex][st * 128:(st + 1) * 128, :]),
                  reads=list(SXr[ex]), writes=[XSEr[i]] + extra)
            lidx["sp"] += 1
            pt3, pt3r = ptb()
            for k in range(8):
                op("pe", lambda e, k=k, i=i: e.transpose(out=pt3[:, k, :], in_=XSE[i][:, k * 128:(k + 1) * 128], identity=IDB[:]),
                   [XSEr[i], rg("idb")], [pt3r], inc=(k == 7))
            if st % 2 == 0:
                op("act", lambda e, st=st: e.copy(out=XST[:, :, st * 128:(st + 1) * 128], in_=pt3[:]), [pt3r], [XSTr[st // 4]])
            else:
                op("dve", lambda e, st=st: e.tensor_copy(out=XST[:, :, st * 128:(st + 1) * 128], in_=pt3[:]), [pt3r], [XSTr[st // 4]])

    def combine(ex):
        for j in range(NT):
            i = j % 2
            pdma(lambda e, j=j, i=i: e.indirect_dma_start(
                out=GTL[i], out_offset=None, in_=ysel[ex],
                in_offset=bass.IndirectOffsetOnAxis(SLI[:, j * 16 + ex:j * 16 + ex + 1], 0), bounds_check=CAP - 1, oob_is_err=False),
                [YSELr[ex], rg("sli")], [GTLr[i]])
            if ex == 0:
                op("dve", lambda e, j=j, i=i: e.tensor_scalar(out=XACC[:, j, :], in0=GTL[i], scalar1=WGT[:, j, ex:ex + 1], scalar2=None, op0=ALU.mult),
                   [GTLr[i], rg("wgt")], [XAr[j]])
            else:
                op("dve", lambda e, j=j, i=i: e.scalar_tensor_tensor(out=XACC[:, j, :], in0=GTL[i], scalar=WGT[:, j, ex:ex + 1], in1=XACC[:, j, :],
                                                                     op0=ALU.mult, op1=ALU.add), [GTLr[i], rg("wgt"), XAr[j]], [XAr[j]])

    for ex in range(NE):
        if ex + 2 < NE:
            scatter_expert(ex + 2)
        if ex == 0:
            prep_expert(0)
        for fc in range(8):
            for bk in range(2):
                hf = fc // 4
                pg_, pgr_ = psb(); pu_, pur_ = psb()
                for k in range(8):
                    op("pe", lambda e, k=k, fc=fc, pg_=pg_: e.matmul(pg_[:], lhsT=WGE[:, k, fc * 128:(fc + 1) * 128], rhs=XST[:, k, bk * 512:(bk + 1) * 512], start=(k == 0), stop=(k == 7)),
                       [XSTr[bk], rg(f"wge{hf}")], [pgr_], inc=(k == 7))
                for k in range(8):
                    op("pe", lambda e, k=k, fc=fc, pu_=pu_: e.matmul(pu_[:], lhsT=WUE[:, k, fc * 128:(fc + 1) * 128], rhs=XST[:, k, bk * 512:(bk + 1) * 512], start=(k == 0), stop=(k == 7)),
                       [XSTr[bk], rg(f"wue{hf}")], [pur_], inc=(k == 7))
                si = (bk * 8 + fc) % 2
                op("act", lambda e, pg_=pg_, si=si: e.activation(out=SGT[si], in_=pg_[:], func=AF.Silu), [pgr_], [SGTr[si]])
                op("dve", lambda e, pu_=pu_, si=si, fc=fc, bk=bk: e.tensor_tensor(out=HTE[:, fc, bk * 512:(bk + 1) * 512], in0=pu_[:], in1=SGT[si], op=ALU.mult),
                   [pur_, SGTr[si]], [HTEr[bk]])
        if ex + 1 < NE:
            load_gu(ex + 1)
            prep_expert(ex + 1)
        for st in range(8):
            i = st % 2
            for half in range(2):
                py, pyr = psb()
                for fc in range(8):
                    op("pe", lambda e, fc=fc, st=st, half=half, py=py: e.matmul(py[:], lhsT=HTE[:, fc, st * 128:(st + 1) * 128], rhs=WDE[:, fc, half * 512:(half + 1) * 512],
                                                                              start=(fc == 0), stop=(fc == 7)), [HTEr[st // 4], rg("wde")], [pyr], inc=(fc == 7))
                op("act", lambda e, i=i, half=half, py=py: e.copy(out=YS[i][:, half * 512:(half + 1) * 512], in_=py[:]), [pyr], [YSr[i]])
            load("sp", ysel[ex][st * 128:(st + 1) * 128, :], YS[i], [YSELr[ex]], [YSr[i]])
        if ex > 0:
            combine(ex - 1)
        if ex + 1 < NE:
            load_d(ex + 1)
    S.barrier()
    if STOP == "E":
        return finish()
    FGT = view(96 * KB, [D]); XF = [view(100 * KB + i * 4 * KB, [D]) for i in range(4)]; OT = [view(116 * KB + i * 4 * KB, [D]) for i in range(4)]
    JK = view(132 * KB, [D])
    XFr = S.regions("xf", 4); OTr = S.regions("ot", 4)
    load("sp", FGT, fin_g.partition_broadcast(128), [rg("fgt")])
    ost = [S.dma_stream(f"o{i}") for i in range(4)]
    for j in range(NT):
        load("sp", XF[j % 4], xmid[j * 128:(j + 1) * 128, :], [XFr[j % 4]], [rg("xmid")]) if j < 4 else None
    ex = NE - 1
    for j in range(NT):
        i = j % 4
        g_ = j % 2
        pdma(lambda e, j=j, g_=g_: e.indirect_dma_start(
            out=GTL[g_], out_offset=None, in_=ysel[ex],
            in_offset=bass.IndirectOffsetOnAxis(SLI[:, j * 16 + ex:j * 16 + ex + 1], 0), bounds_check=CAP - 1, oob_is_err=False),
            [YSELr[ex], rg("sli")], [GTLr[g_]])
        op("dve", lambda e, j=j, g_=g_: e.scalar_tensor_tensor(out=XACC[:, j, :], in0=GTL[g_], scalar=WGT[:, j, ex:ex + 1], in1=XACC[:, j, :],
                                                               op0=ALU.mult, op1=ALU.add), [GTLr[g_], rg("wgt"), XAr[j]], [XAr[j]])
        if j >= 4:
            load("sp", XF[i], xmid[j * 128:(j + 1) * 128, :], [XFr[i]], [rg("xmid")])
        op("dve", lambda e, j=j: e.tensor_tensor(out=XACC[:, j, :], in0=XACC[:, j, :], in1=GT2[:], op=ALU.mult), [XAr[j], rg("gt2")], [XAr[j]])
        op("dve", lambda e, j=j, i=i: e.tensor_tensor(out=XF[i], in0=XF[i], in1=XACC[:, j, :], op=ALU.add), [XAr[j], XFr[i]], [XFr[i]])
        ssq, ssqr = small(); sd, sdr = small(); rs, rsr = small()
        op("act", lambda e, i=i: e.activation(out=JK, in_=XF[i], func=AF.Square, accum_out=ssq), [XFr[i]], [rg("jk"), ssqr])
        op("act", lambda e: e.activation(out=sd, in_=ssq, func=AF.Sqrt, scale=1.0 / D, bias=EPS), [ssqr], [sdr])
        op("dve", lambda e: e.reciprocal(out=rs, in_=sd), [sdr], [rsr])
        op("dve", lambda e, i=i: e.scalar_tensor_tensor(out=OT[i], in0=XF[i], scalar=rs, in1=FGT, op0=ALU.mult, op1=ALU.mult), [XFr[i], rsr, rg("fgt")], [OTr[i]])
        S.dma("sp", ost[i], lambda e, j=j, i=i: e.dma_start(out=out[j * 128:(j + 1) * 128, :], in_=OT[i]), reads=[OTr[i]], writes=[rg("outd")])
    S.wait_all("sp", [(o_s, S.dma_count[o_s]) for o_s in ost])
    S.emit()
    return nc


_NC = [None]


def _NEW():
    import os
    return 1 if os.environ.get("KSTOP", "") else NE


def _consts():
    j = np.arange(128)[:, None]; i = np.arange(128)[None, :]
    same = (j // 64) == (i // 64)
    lj = j % 64
    sc = -1.0 / 16.0
    C = np.zeros((128, 9, 128), np.float32)
    C[:, 0] = sc * (same & (j <= i)); C[:, 1] = sc * (same & (j >= i))
    C[:, 2] = sc * (same & (j > i)); C[:, 3] = sc * (same & (j < i))
    C[:, 4] = sc * same * ((j <= i).astype(np.float32) - (lj <= 31).astype(np.float32))
    C[:, 5] = sc * same * ((j >= i).astype(np.float32) - (lj >= 32).astype(np.float32))
    C[:, 6] = (same & (i >= j)); C[:, 7] = (same & (j >= i))
    C[:, 8, 0] = sc * (np.arange(128) // 64 == 0); C[:, 8, 1] = sc * (np.arange(128) // 64 == 1); C[:, 8, 2] = sc
    return C.reshape(128, 9 * 128)


def kernel(x, c, ctx, c_ctx, ada_w, ada_b, norm1_g, norm2_g, w_in, w_decay_up, b_decay,
           gla_norm_g, w_gla_proj, pool_w, pool_scale, w_pool_proj, w_out, w_router,
           w_gate_e, w_up_e, w_down_e, final_norm_g):
    f = lambda a: np.ascontiguousarray(np.asarray(a, dtype=np.float32))
    x = f(x); c = f(c); ctx = f(ctx); c_ctx = f(c_ctx)
    fm = lambda v: f(v).reshape(-1, 128).T
    if _NC[0] is None:
        _NC[0] = build_program()
    nc = _NC[0]
    vecs = np.zeros((128, 32), np.float32)
    vecs[:, 0:8] = fm(norm1_g[0]); vecs[:, 8:16] = fm(norm2_g[0]); vecs[:, 16:20] = fm(pool_scale[0]); vecs[:, 20] = f(gla_norm_g[0])
    w_upP = np.zeros((32, 512), np.float32)
    w_upP[0:16, 0:256] = f(w_decay_up[0, 0]); w_upP[16:32, 256:512] = f(w_decay_up[0, 1])
    consts = _consts()
    pp = np.arange(128)
    consts2 = np.concatenate([(pp[:, None] < pp[None, :]).astype(np.float32), np.ones((128, 128), np.float32)], axis=1)
    gmat = np.zeros((64, 64), np.float32)
    for r in range(4):
        for r2 in range(4):
            gmat[r * 16:(r + 1) * 16, r2 * 16:(r2 + 1) * 16] = np.eye(16, dtype=np.float32)
    shared = {
        "ada_w": f(ada_w[0]), "ada_bT": fm(ada_b[0]), "ada_b": f(ada_b[0]).reshape(1, -1), "vecs": vecs,
        "b_decay": f(b_decay[0]).reshape(1, 512), "fin_g": f(final_norm_g).reshape(1, D), "w_in": f(w_in[0]), "w_upP": w_upP,
        "w_glap": f(w_gla_proj[0]), "pool_w": f(pool_w[0]), "w_poolp": f(w_pool_proj[0]), "w_out": f(w_out[0]), "w_router": f(w_router[0]),
        "w_gate": f(w_gate_e[0])[:_NEW()], "w_up": f(w_up_e[0])[:_NEW()], "w_down": f(w_down_e[0])[:_NEW()], "consts": consts, "consts2": consts2, "gmat": gmat,
        "pmask": np.zeros((1, NEXT), np.float32),
    }
    in_maps = []
    for core in range(8):
        b, s_ = core // 4, core % 4
        t0 = 2048 * s_
        xe = np.zeros((NEXT, D), np.float32)
        lo, hi = max(t0 - 512, 0), min(t0 + 2560, 8192)
        xe[lo - (t0 - 512):hi - (t0 - 512)] = x[b, lo:hi]
        cv = np.zeros((128, 16), np.float32)
        cv[:, 0:8] = fm(c[b]); cv[:, 8:16] = fm(c_ctx)
        pcz = np.zeros((128, 16), np.float32)
        pcz[:, 2] = 1.0 if s_ > 0 else 0.0; pcz[:, 3] = 1.0 if s_ < 3 else 0.0; pcz[:, 12] = 1.0
        for r in range(4):
            pcz[:, 4 + r] = 1.0 if r < s_ else 0.0
            pcz[:, 8 + r] = 1.0 if r > s_ else 0.0
        inv = np.zeros((4, 32, 64), np.float32)
        for g, w in enumerate(WINS):
            rows = 32 * s_ + np.arange(32)
            cr = np.minimum(rows + w // 2, 128) - np.maximum(rows - w // 2, 0)
            cols = np.arange(64)
            cc_ = np.minimum(cols + w // 2, 64) - np.maximum(cols - w // 2, 0)
            inv[g] = 1.0 / (cr[:, None] * cc_[None, :]).astype(np.float32)
        selm = np.zeros((64, 16), np.float32)
        selm[s_ * 16:(s_ + 1) * 16] = np.eye(16, dtype=np.float32)
        m = dict(shared)
        m.update({"xext": xe, "ctx": f(ctx[b]), "cvec": cv, "percore": pcz, "invcnt": inv.reshape(1, -1), "sel": selm})
        in_maps.append(m)
    res = run_bass_kernel_spmd(nc, in_maps, core_ids=list(range(8)))
    outp = np.zeros((2, 8192, D), np.float32)
    for core in range(8):
        b, s_ = core // 4, core % 4
        outp[b, 2048 * s_:2048 * (s_ + 1)] = res.results[core]["out"]
    return outp
```
